# Optimizing a Trainium2 kernel written in Bass

```python
import jax, jax.numpy as jnp
from jax import lax
import numpy as np

D_MODEL = 1024
BATCH = 2
SEQ = 8192
DEPTH = 1

D_MIX = D_MODEL
D_CONV = 512
N_CONV_GROUPS = 8
CONV_WIDTH = 3
N_HEADS = 8
HEAD_DIM = 64
D_ATTN = N_HEADS * HEAD_DIM
D_IN = 3 * D_CONV + 3 * D_ATTN
MOBA_BLOCK = 256
MOBA_TOPK = 3
Q_CHUNK = 64
N_GROUPS = 4
EXPERTS_PER_GROUP = 8
N_EXPERTS = N_GROUPS * EXPERTS_PER_GROUP
TOP_K_INNER = 2
D_EXPERT = 512
DISPATCH_BLOCK = 256
PLE_DIM = 256
LN_EPS = 1e-5
DEEPNORM_ALPHA = (2 * DEPTH) ** 0.25
DEEPNORM_BETA = (8 * DEPTH) ** -0.25

kernel_name = "hymba_conv_moba_hmoe_deepnorm"


def layer_norm(x, g, b):
    xf = x.astype(jnp.float32)
    mu = xf.mean(-1, keepdims=True)
    var = jnp.mean(jnp.square(xf - mu), -1, keepdims=True)
    return ((xf - mu) * lax.rsqrt(var + LN_EPS)).astype(x.dtype) * g + b


def short_conv(gate_b, gate_c, h, w_conv):
    s = h.shape[1]
    u = jnp.pad(gate_c * h, ((0, 0), (CONV_WIDTH - 1, 0), (0, 0)))
    y = w_conv[0] * u[:, 0:s] + w_conv[1] * u[:, 1:s + 1] + w_conv[2] * u[:, 2:s + 2]
    return gate_b * y


def moba_attention(q, k, v):
    b, s, h, dh = q.shape
    nb = -(-s // MOBA_BLOCK)
    s_pad = nb * MOBA_BLOCK
    topk = min(MOBA_TOPK, nb)
    scale = dh ** -0.5
    q = q.transpose(0, 2, 1, 3)
    k = k.transpose(0, 2, 1, 3)
    v = v.transpose(0, 2, 1, 3)
    pad = ((0, 0), (0, 0), (0, s_pad - s), (0, 0))
    kb = jnp.pad(k, pad).reshape(b, h, nb, MOBA_BLOCK, dh)
    vb = jnp.pad(v, pad).reshape(b, h, nb, MOBA_BLOCK, dh)
    k_mean = kb.astype(jnp.float32).mean(axis=3)
    n_chunks = s // Q_CHUNK
    q_chunks = q.reshape(b, h, n_chunks, Q_CHUNK, dh).transpose(2, 0, 1, 3, 4)
    gather_blocks = jax.vmap(jax.vmap(lambda blocks, idx: blocks[idx]))

    def chunk_fn(args):
        qc, c = args
        q_pos = c * Q_CHUNK + jnp.arange(Q_CHUNK)
        q_blk = q_pos // MOBA_BLOCK
        own = (c * Q_CHUNK) // MOBA_BLOCK
        gate = jnp.einsum('bhqd,bhnd->bhqn', qc.astype(jnp.float32), k_mean)
        past = jnp.arange(nb)[None, :] < q_blk[:, None]
        gate = jnp.where(past, gate, -jnp.inf)
        _, idx = lax.top_k(gate, topk)
        sel_valid = idx < q_blk[:, None]
        k_sel = gather_blocks(kb, idx)
        v_sel = gather_blocks(vb, idx)
        s_sel = jnp.einsum('bhqd,bhqnkd->bhqnk', qc, k_sel).astype(jnp.float32) * scale
        s_sel = jnp.where(sel_valid[..., None], s_sel, -jnp.inf)
        k_own = lax.dynamic_index_in_dim(kb, own, axis=2, keepdims=False)
        v_own = lax.dynamic_index_in_dim(vb, own, axis=2, keepdims=False)
        k_pos = own * MOBA_BLOCK + jnp.arange(MOBA_BLOCK)
        s_own = jnp.einsum('bhqd,bhkd->bhqk', qc, k_own).astype(jnp.float32) * scale
        s_own = jnp.where(k_pos[None, :] <= q_pos[:, None], s_own, -jnp.inf)
        scores = jnp.concatenate([s_sel.reshape(b, h, Q_CHUNK, topk * MOBA_BLOCK), s_own], axis=-1)
        probs = jax.nn.softmax(scores, axis=-1).astype(v.dtype)
        p_sel = probs[..., :topk * MOBA_BLOCK].reshape(b, h, Q_CHUNK, topk, MOBA_BLOCK)
        p_own = probs[..., topk * MOBA_BLOCK:]
        return (jnp.einsum('bhqnk,bhqnkd->bhqd', p_sel, v_sel)
                + jnp.einsum('bhqk,bhkd->bhqd', p_own, v_own))

    out = lax.map(chunk_fn, (q_chunks, jnp.arange(n_chunks)))
    return out.transpose(1, 0, 3, 2, 4).reshape(b, s, h * dh)


def hier_moe(x, w_router_g, b_router_g, w_router_e, b_router_e, w_gate, w_up, w_down):
    bsz, s, d = x.shape
    xt = x.reshape(-1, d)
    n = xt.shape[0]
    g_prob = jax.nn.softmax((xt @ w_router_g).astype(jnp.float32) + b_router_g, axis=-1)
    g_p, g_idx = lax.top_k(g_prob, 1)
    e_logits = ((xt @ w_router_e).astype(jnp.float32) + b_router_e).reshape(n, N_GROUPS, EXPERTS_PER_GROUP)
    sel = jnp.broadcast_to(g_idx[:, :, None], (n, 1, EXPERTS_PER_GROUP))
    e_logits = jnp.take_along_axis(e_logits, sel, axis=1)[:, 0]
    e_p, e_local = lax.top_k(jax.nn.softmax(e_logits, axis=-1), TOP_K_INNER)
    e_p = e_p / e_p.sum(-1, keepdims=True)
    weights = g_p * e_p
    experts = g_idx * EXPERTS_PER_GROUP + e_local
    a = n * TOP_K_INNER
    r = DISPATCH_BLOCK
    flat_e = experts.reshape(-1)
    flat_w = weights.reshape(-1)
    flat_tok = jnp.repeat(jnp.arange(n), TOP_K_INNER)
    order = jnp.argsort(flat_e)
    se, stok, sw = flat_e[order], flat_tok[order], flat_w[order]
    counts = jnp.bincount(flat_e, length=N_EXPERTS)
    padded = (counts + r - 1) // r * r
    pad_end = jnp.cumsum(padded)
    pad_start = pad_end - padded
    start = jnp.cumsum(counts) - counts
    slot = pad_start[se] + jnp.arange(a) - start[se]
    n_blk = -(-a // r) + N_EXPERTS
    n_pad = n_blk * r
    buf = jnp.zeros((n_pad, d), x.dtype).at[slot].set(xt[stok])
    blk_expert = jnp.minimum(jnp.searchsorted(pad_end, jnp.arange(n_blk) * r, side='right'), N_EXPERTS - 1)

    def expert_block(args):
        xb, e = args
        hid = jax.nn.silu(xb @ w_gate[e]) * (xb @ w_up[e])
        return hid @ w_down[e]

    yb = lax.map(expert_block, (buf.reshape(n_blk, r, d), blk_expert)).reshape(n_pad, d)
    y_sorted = yb[slot] * sw[:, None].astype(x.dtype)
    y = jnp.zeros((n, d), x.dtype).at[stok].add(y_sorted)
    return y.reshape(bsz, s, d)


def setup_inputs(seed: int = 0) -> dict:
    key = jax.random.key(seed)
    ks = jax.random.split(key, 20)
    f32 = jnp.float32
    nrm = lambda k, shape, sc: jax.random.normal(k, shape, f32) * sc
    L = DEPTH
    return {
        "x": nrm(ks[0], (BATCH, SEQ, D_MODEL), 1.0),
        "p": nrm(ks[1], (DEPTH, BATCH, SEQ, PLE_DIM), 1.0),
        "w_in": nrm(ks[2], (L, D_MODEL, D_IN), D_MODEL ** -0.5),
        "w_conv": nrm(ks[3], (L, CONV_WIDTH, D_CONV), CONV_WIDTH ** -0.5),
        "w_out": nrm(ks[4], (L, D_MIX, D_MODEL), D_MIX ** -0.5 * DEEPNORM_BETA),
        "ln1_g": 1.0 + nrm(ks[5], (L, D_MODEL), 0.02),
        "ln1_b": nrm(ks[6], (L, D_MODEL), 0.02),
        "w_router_g": nrm(ks[7], (L, D_MODEL, N_GROUPS), D_MODEL ** -0.5),
        "b_router_g": nrm(ks[8], (L, N_GROUPS), 0.01),
        "w_router_e": nrm(ks[9], (L, D_MODEL, N_EXPERTS), D_MODEL ** -0.5),
        "b_router_e": nrm(ks[10], (L, N_EXPERTS), 0.01),
        "w_gate": nrm(ks[11], (L, N_EXPERTS, D_MODEL, D_EXPERT), D_MODEL ** -0.5),
        "w_up": nrm(ks[12], (L, N_EXPERTS, D_MODEL, D_EXPERT), D_MODEL ** -0.5),
        "w_down": nrm(ks[13], (L, N_EXPERTS, D_EXPERT, D_MODEL), D_EXPERT ** -0.5 * DEEPNORM_BETA),
        "w_ple_gate": nrm(ks[14], (L, D_MODEL, D_MODEL), D_MODEL ** -0.5),
        "w_ple_proj": nrm(ks[15], (L, PLE_DIM, D_MODEL), PLE_DIM ** -0.5 * DEEPNORM_BETA),
        "ln2_g": 1.0 + nrm(ks[16], (L, D_MODEL), 0.02),
        "ln2_b": nrm(ks[17], (L, D_MODEL), 0.02),
    }


def reference(x, p, w_in, w_conv, w_out, ln1_g, ln1_b, w_router_g, b_router_g, w_router_e,
              b_router_e, w_gate, w_up, w_down, w_ple_gate, w_ple_proj, ln2_g, ln2_b):
    b, s, _ = x.shape
    splits = [D_CONV, 2 * D_CONV, 3 * D_CONV, 3 * D_CONV + D_ATTN, 3 * D_CONV + 2 * D_ATTN]
    for i in range(DEPTH):
        proj = x @ w_in[i]
        cb, cc, ch, q, k, v = jnp.split(proj, splits, axis=-1)
        y_conv = short_conv(cb, cc, ch, w_conv[i])
        hd = (b, s, N_HEADS, HEAD_DIM)
        y_attn = moba_attention(q.reshape(hd), k.reshape(hd), v.reshape(hd))
        mix = jnp.concatenate([y_conv, y_attn], axis=-1) @ w_out[i]
        x = layer_norm(DEEPNORM_ALPHA * x + mix, ln1_g[i], ln1_b[i])
        ffn = hier_moe(x, w_router_g[i], b_router_g[i], w_router_e[i], b_router_e[i],
                       w_gate[i], w_up[i], w_down[i])
        ple = jax.nn.sigmoid(x @ w_ple_gate[i]) * (p[i] @ w_ple_proj[i])
        x = layer_norm(DEEPNORM_ALPHA * x + ffn + ple, ln2_g[i], ln2_b[i])
    return x
```

```python
import numpy as np
from contextlib import ExitStack

import concourse.bass as bass
import concourse.mybir as mybir
from concourse.bass_utils import run_bass_kernel_spmd

F32 = mybir.dt.float32
BF16 = mybir.dt.bfloat16
U8 = mybir.dt.uint8
I32 = mybir.dt.int32
AF = mybir.ActivationFunctionType
ALU = mybir.AluOpType
AX = mybir.AxisListType

D = 1024
DIN = 3072
NH = 8
HD = 64
NE = 32
DE = 512
PLE = 256
CAP = 256
ALPHA = float(2 ** 0.25)
EPS = 1e-5
NEG = -30000.0
NEGINF = -1.0e30


class Eng:
    def __init__(self, name, sem, same_wait):
        self.name = name
        self.sem = sem
        self.count = 0
        self.waited = {}
        self.ops = []
        self.same_wait = same_wait


class Buf:
    __slots__ = ("name", "w", "r", "excl")

    def __init__(self, name, excl=False):
        self.name = name
        self.w = None
        self.r = []
        self.excl = excl


class FW:
    def __init__(self, nc, stack):
        self.nc = nc
        self.stack = stack
        self.eng = {}
        for n, sw in (("pe", False), ("act", True), ("dve", True), ("pool", True), ("sp", False)):
            s = stack.enter_context(nc.semaphore("sem_" + n))
            self.eng[n] = Eng(n, s, sw)
        self.dsems = []
        self.names = {}
        self.bc_reg = None

    def dsem(self, name):
        s = [self.stack.enter_context(self.nc.semaphore(name)), 0]
        self.dsems.append(s)
        return s

    def _wait(self, e, s, v):
        if isinstance(s, list):
            s, v = s[0], s[1]
        if v <= 0:
            return
        k = id(s)
        if e.waited.get(k, 0) < v:
            e.waited[k] = v
            e.ops.append(lambda en, s=s, v=v: en.wait_ge(s, v))

    def _deps(self, e, reads, writes):
        for b in reads:
            if b.w is not None:
                s, v = b.w
                if not (s is e.sem and not e.same_wait):
                    self._wait(e, s, v)
            if b.excl:
                for (s, v) in b.r:
                    if not (s is e.sem and not e.same_wait):
                        self._wait(e, s, v)
        for b in writes:
            if b.w is not None:
                s, v = b.w
                if not (s is e.sem and not e.same_wait):
                    self._wait(e, s, v)
            for (s, v) in b.r:
                if not (s is e.sem and not e.same_wait):
                    self._wait(e, s, v)

    def _mark(self, tok, reads, writes):
        for b in reads:
            if b.excl:
                b.r = [tok]
            else:
                b.r.append(tok)
                if len(b.r) > 64:
                    b.r = b.r[-64:]
        for b in writes:
            b.w = tok
            b.r = []

    def op(self, engname, fn, reads=(), writes=()):
        e = self.eng[engname]
        self._deps(e, reads, writes)
        e.count += 1
        tok = (e.sem, e.count)
        import sys as _sys
        line = _sys._getframe(1).f_lineno

        def run(en, fn=fn, s=e.sem, line=line):
            ins = fn(en)
            try:
                self.names[ins.ins.name] = line
            except Exception:
                pass
            return ins.then_inc(s, 1)
        e.ops.append(run)
        self._mark(tok, reads, writes)
        return tok

    def dma(self, engname, fn, sem_state, reads=(), writes=()):
        e = self.eng[engname]
        saved = []
        for b in writes:
            if b.w is not None and b.w[0] is sem_state and not b.r:
                saved.append((b, b.w))
                b.w = None
        self._deps(e, reads, writes)
        for b, w in saved:
            b.w = w
        sem_state[1] += 16
        tok = (sem_state, None)
        e.ops.append(lambda en, fn=fn, s=sem_state[0]: fn(en).then_inc(s, 16))
        self._mark(tok, reads, writes)
        return tok

    def barrier(self):
        names = ["pe", "act", "dve", "pool", "sp"]
        snap = [(self.eng[n].sem, self.eng[n].count) for n in names]
        dsnap = [(s[0], s[1]) for s in self.dsems]
        for n in names:
            e = self.eng[n]
            for (s, v) in snap:
                if s is e.sem:
                    continue
                self._wait(e, s, v)
            for (s, v) in dsnap:
                self._wait(e, s, v)

    def replay(self):
        nc = self.nc
        with nc.Block() as block:
            @block.tensor
            def _(en):
                for f in self.eng["pe"].ops:
                    f(en)

            @block.scalar
            def _(en):
                for f in self.eng["act"].ops:
                    f(en)

            @block.vector
            def _(en):
                for f in self.eng["dve"].ops:
                    f(en)

            @block.gpsimd
            def _(en):
                for f in self.eng["pool"].ops:
                    f(en)

            @block.sync
            def _(en):
                for f in self.eng["sp"].ops:
                    f(en)


def build_nc(S=8192, dbg=False, stop_after=None):
    NB = S // 256
    NSLOT = NB // 4
    NOWN = NSLOT * 256
    TOT = S + NOWN
    NT = NOWN // 128
    NCH = S // 512
    NKT = TOT // 128
    NSL = NE * CAP

    nc = bass.Bass("TRN2", target_bir_lowering=False)

    def din(name, shape, dt=F32):
        return nc.dram_tensor(name, list(shape), dt, kind="ExternalInput").ap()

    xf = din("xf", [S, D])
    xo = din("xo", [NSLOT, 258, D])
    po = din("po", [NSLOT, 256, PLE])
    nm1_d = din("nm1", [NSLOT * 256])
    nm2_d = din("nm2", [NSLOT * 256])
    w_in = din("w_in", [D, DIN])
    w_conv = din("w_conv", [3, 512])
    w_out = din("w_out", [D, D])
    ln1_g = din("ln1_g", [D])
    ln1_b = din("ln1_b", [D])
    w_rg = din("w_rg", [D, 4])
    b_rg = din("b_rg", [4])
    w_re = din("w_re", [D, NE])
    b_re = din("b_re", [NE])
    w_gate = din("w_gate", [NE, D, DE])
    w_up = din("w_up", [NE, D, DE])
    w_down = din("w_down", [NE, DE, D])
    w_pg = din("w_pg", [D, D])
    w_pp = din("w_pp", [PLE, D])
    ln2_g = din("ln2_g", [D])
    ln2_b = din("ln2_b", [D])
    out = nc.dram_tensor("out", [NOWN, D], F32, kind="ExternalOutput").ap()
    if dbg:
        dbg_x1 = nc.dram_tensor("dbg_x1", [NOWN, D], F32, kind="ExternalOutput").ap()
        dbg_mix = nc.dram_tensor("dbg_mix", [NOWN, D], F32, kind="ExternalOutput").ap()

    KT = nc.dram_tensor("KT_scr", [NH * HD, TOT], BF16).ap()
    VV = nc.dram_tensor("VV_scr", [NH, 128, NKT, 65], BF16).ap()
    XS = nc.dram_tensor("XS_scr", [NSL, D], BF16).ap()
    YS = nc.dram_tensor("YS_scr", [NSL, D], F32).ap()
    ACCD = nc.dram_tensor("ACC_scr", [NOWN, D], F32).ap()

    st = ExitStack()
    with st:
        fw = FW(nc, st)
        ARENA = 98304 + 59392 + 20480 + 8192
        arena = st.enter_context(nc.sbuf_tensor("arena", [128, ARENA], U8))
        R0, R1, R2, RC = 0, 98304, 98304 + 59392, 98304 + 59392 + 20480

        class Alloc:
            def __init__(self, base, size):
                self.base, self.size, self.off = base, size, 0

            def __call__(self, nelem, dtype, pat=None, **kw):
                esz = 4 if dtype in (F32, I32) else 2
                nbytes = nelem * esz
                o = self.base + self.off
                self.off += (nbytes + 63) // 64 * 64
                assert self.off <= self.size, ("arena overflow", self.base, self.off, self.size)
                a = arena[:, o:o + nbytes].bitcast(dtype)
                if pat:
                    a = a.rearrange(pat, **kw)
                return a

        psall = st.enter_context(nc.psum_tensor("psall", [128, 4096], F32))
        psf = [psall[:, i * 512:(i + 1) * 512] for i in range(6)]
        psb = [psall[:, (6 + i) * 512:(7 + i) * 512].bitcast(BF16) for i in range(2)]
        psw = [psall[:, j * 1024:(j + 1) * 1024] for j in range(2)]
        pse = psall[:, 6 * 512:7 * 512]
        PW = [Buf("psw%d" % j, excl=True) for j in range(2)]
        PF = [Buf("psf%d" % i, excl=True) for i in range(6)]
        PB = [Buf("psb%d" % i, excl=True) for i in range(2)]

        def alt(i):
            return "act" if i % 2 == 0 else "dve"


        def MM(o, l, r, st_, sp_):
            return lambda e: e.matmul(o, lhsT=l, rhs=r, start=st_, stop=sp_)

        def TR(o, i_, idn):
            return lambda e: e.transpose(out=o, in_=i_, identity=idn)

        def ACTF(o, i_, f, **kw):
            return lambda e: e.activation(out=o, in_=i_, func=f, **kw)

        def TT(o, a, b, op):
            return lambda e: e.tensor_tensor(out=o, in0=a, in1=b, op=op)

        def TS(o, a, s1, s2, op0, op1=None):
            if op1 is None:
                return lambda e: e.tensor_scalar(out=o, in0=a, scalar1=s1, scalar2=None, op0=op0)
            return lambda e: e.tensor_scalar(out=o, in0=a, scalar1=s1, scalar2=s2, op0=op0, op1=op1)

        def STT(o, a, sc, b, op0, op1):
            return lambda e: e.scalar_tensor_tensor(out=o, in0=a, scalar=sc, in1=b, op0=op0, op1=op1)

        def MS(ap, v):
            return lambda e: e.memset(ap, v)

        def RD(o, i_, op):
            return lambda e: e.tensor_reduce(out=o, in_=i_, axis=AX.X, op=op)

        def MX(o, i_):
            return lambda e: e.max(out=o, in_=i_)

        def RCP(o, i_):
            return lambda e: e.reciprocal(out=o, in_=i_)

        def DM(o, i_):
            return lambda e: e.dma_start(out=o, in_=i_)

        def CPY(o, i_):
            return lambda e: e.tensor_copy(out=o, in_=i_)

        def ASEL(o, i_, pattern, cmp, fill, base, cm):
            return lambda e: e.affine_select(out=o, in_=i_, pattern=pattern, compare_op=cmp, fill=fill, base=base, channel_multiplier=cm)

        def BNS(o, i_):
            return lambda e: e.bn_stats(out=o, in_=i_)

        def BNA(o, i_):
            return lambda e: e.bn_aggr(out=o, in_=i_)

        def SCAT(dst, idx, src, bc):
            return lambda e: e.indirect_dma_start(out=dst, out_offset=bass.IndirectOffsetOnAxis(ap=idx, axis=0), in_=src, in_offset=None, bounds_check=bc, oob_is_err=False)

        def GATH(dst, src, idx, bc):
            return lambda e: e.indirect_dma_start(out=dst, out_offset=None, in_=src, in_offset=bass.IndirectOffsetOnAxis(ap=idx, axis=0), bounds_check=bc, oob_is_err=False)


        def _L_matmul(o, lhsT=None, rhs=None, start=None, stop=None):
            return lambda e: e.matmul(o, lhsT=lhsT, rhs=rhs, start=start, stop=stop)

        def _mk(meth):
            def f(*a, **kw):
                return lambda e: getattr(e, meth)(*a, **kw)
            return f

        _L_transpose = _mk("transpose")
        _L_activation = _mk("activation")
        _L_tensor_tensor = _mk("tensor_tensor")
        _L_tensor_scalar = _mk("tensor_scalar")
        _L_scalar_tensor_tensor = _mk("scalar_tensor_tensor")
        _L_memset = _mk("memset")
        _L_tensor_reduce = _mk("tensor_reduce")
        _L_max = _mk("max")
        _L_reciprocal = _mk("reciprocal")
        _L_dma_start = _mk("dma_start")
        _L_tensor_copy = _mk("tensor_copy")
        _L_affine_select = _mk("affine_select")
        _L_bn_stats = _mk("bn_stats")
        _L_bn_aggr = _mk("bn_aggr")
        def _L_indirect_dma_start(*a, **kw):
            def f(e):
                if fw.bc_reg is None:
                    fw.bc_reg = e.to_reg(kw["bounds_check"])
                kw2 = dict(kw)
                kw2["bounds_check"] = fw.bc_reg
                return e.indirect_dma_start(*a, **kw2)
            return f
        _L_iota = _mk("iota")

        def copy_op(eng, out_ap, in_ap):
            if eng == "act":
                return lambda e: e.activation(out=out_ap, in_=in_ap, func=AF.Copy)
            return lambda e: e.tensor_copy(out=out_ap, in_=in_ap)

        ac = Alloc(RC, 8192)
        ident_f = ac(128, F32)
        ident_b = ac(128, BF16)
        ones_f = ac(128, F32)
        CBm = ac(512, BF16, "p (t q) -> p t q", t=2)
        wc = ac(12, F32, "p (c k) -> p c k", c=4)
        km = ac(4 * 32, F32, "p (c n) -> p c n", c=4)
        kmb = ac(4 * 32, BF16, "p (c n) -> p c n", c=4)
        kmh = ac(8 * 32, BF16, "p (h n) -> p h n", h=8)
        slot_i = ac(NT * 2, I32, "p (t k) -> p t k", k=2)
        w12 = ac(NT * 2, F32, "p (t k) -> p t k", k=2)
        Lst = ac(128, F32)
        eC = ac(32, F32)
        Spre = ac(32, F32)
        bias36 = ac(36, F32)
        ztile = ac(1024, BF16)
        B_ident = Buf("ident")
        B_const = Buf("const")
        B_km = Buf("km")
        B_kmh = Buf("kmh")
        B_slot = [Buf("slot%d" % t) for t in range(NT)]
        B_w12 = [Buf("w12_%d" % t) for t in range(NT)]
        B_Spre = Buf("Spre")

        s_setup = fw.dsem("setup")

        fw.op("pool", _L_memset(ident_f, 0.0), writes=[B_ident])
        fw.op("pool", _L_affine_select(out=ident_f, in_=ident_f, pattern=[[-1, 128]], compare_op=ALU.not_equal, fill=1.0, base=0, channel_multiplier=1), reads=[B_ident], writes=[B_ident])
        fw.op("pool", _L_tensor_copy(out=ident_b, in_=ident_f), reads=[B_ident], writes=[B_const])
        fw.op("pool", _L_memset(ones_f, 1.0), writes=[B_const])
        fw.op("pool", _L_memset(CBm, 0.0), writes=[B_const])
        fw.op("pool", _L_affine_select(out=CBm, in_=CBm, pattern=[[-128, 2], [1, 256]], compare_op=ALU.is_ge, fill=NEG, base=0, channel_multiplier=-1), reads=[B_const], writes=[B_const])
        fw.op("pool", _L_memset(Lst, 1.0), writes=[B_const])
        fw.op("pool", _L_affine_select(out=Lst, in_=Lst, pattern=[[1, 128]], compare_op=ALU.is_ge, fill=0.0, base=-1, channel_multiplier=-1), reads=[B_const], writes=[B_const])
        fw.op("pool", _L_iota(eC, pattern=[[CAP, 32]], base=0, channel_multiplier=0, allow_small_or_imprecise_dtypes=True), writes=[B_const])
        fw.op("pool", _L_memset(Spre, 0.0), writes=[B_Spre])
        for c_ in range(4):
            for k_ in range(3):
                fw.dma("sp", _L_dma_start(out=wc[:, c_, k_:k_ + 1], in_=w_conv[k_, c_ * 128:(c_ + 1) * 128].rearrange("(p o) -> p o", o=1)), s_setup, writes=[B_const])
        fw.dma("sp", _L_dma_start(out=bias36[:, 0:4], in_=b_rg.partition_broadcast(128)), s_setup, writes=[B_const])
        fw.dma("sp", _L_dma_start(out=bias36[:, 4:36], in_=b_re.partition_broadcast(128)), s_setup, writes=[B_const])

        a0 = Alloc(R0, 98304)
        w_in_bf = a0(8 * DIN, BF16, "p (c n) -> p c n", c=8)
        Qaug = Alloc(R0 + 49152, 32768)(8 * NOWN, BF16, "p (h n) -> p h n", h=8)
        yconvT = Alloc(R0 + 81920, 16384)(4 * NOWN, BF16, "p (c n) -> p c n", c=4)
        B_win = [Buf("win%d" % c) for c in range(8)]
        B_Q = [Buf("Q%d" % i) for i in range(NSLOT)]
        B_yc = [Buf("yc%d" % i) for i in range(NSLOT)]

        a1 = Alloc(R1, 59392)
        wst = [a1(DIN, F32) for _ in range(2)]
        B_wst = [Buf("wst%d" % i) for i in range(2)]
        s_wst = [fw.dsem("wst%d" % i) for i in range(2)]
        B_z = Buf("ztile")
        s_z = fw.dsem("zfill")
        B_XS = Buf("XS")

        fw.op("pool", _L_memset(ztile, 0.0), writes=[B_z])
        XSv = XS.rearrange("(t p) d -> t p d", p=128)
        for c in range(8):
            sl = c % 2
            fw.dma("sp", _L_dma_start(out=wst[sl], in_=w_in[c * 128:(c + 1) * 128, :]), s_wst[sl], writes=[B_wst[sl]])
            for k, eng in enumerate(("pool", "act", "dve")):
                o, i_ = w_in_bf[:, c, k * 1024:(k + 1) * 1024], wst[sl][:, k * 1024:(k + 1) * 1024]
                fw.op(eng, copy_op(eng, o, i_), reads=[B_wst[sl]], writes=[B_win[c]])
        fw.barrier()

        a1 = Alloc(R1, 59392)
        xs = [a1(4 * D, F32, "p (t d) -> p t d", t=4) for _ in range(2)]
        xT = [a1(8 * 512, BF16, "p (c n) -> p c n", c=8) for _ in range(2)]
        KTst = [a1(4 * 512, BF16, "p (c n) -> p c n", c=4) for _ in range(2)]
        a2 = Alloc(R2, 20480)
        Vst = [a2(8 * 4 * 65, BF16, "p (h k e) -> p h k e", h=8, k=4) for _ in range(2)]
        B_xs = [Buf("xs%d" % i) for i in range(2)]
        B_xT = [Buf("xT%d" % i) for i in range(2)]
        B_KTst = [Buf("KTst%d" % i) for i in range(2)]
        B_Vst = [Buf("Vst%d" % i) for i in range(2)]
        s_xs = [fw.dsem("xs%d" % i) for i in range(2)]
        s_kst = [fw.dsem("kst%d" % i) for i in range(2)]
        s_vst = [fw.dsem("vst%d" % i) for i in range(2)]
        B_KT = Buf("KT")
        B_VV = Buf("VV")
        for i in range(2):
            fw.op("pool", _L_memset(Vst[i], 1.0), writes=[B_Vst[i]])
        fw.op("pool", _L_memset(km, 0.0), writes=[B_km])
        fw.op("pool", _L_memset(Qaug[64:128], 0.0), writes=B_Q)
        ZF_PER = (NSL // 128 + NCH - 1) // NCH

        KTv = KT.rearrange("(c p) n -> p c n", p=128)
        VVv = VV.rearrange("h p k e -> p h k e")
        xfv = xf.rearrange("(t p) d -> t p d", p=128)

        def load_chunk(t):
            sl = t % 2
            for q in range(4):
                fw.dma("sp", _L_dma_start(out=xs[sl][:, q, :], in_=xfv[4 * t + q]), s_xs[sl], writes=[B_xs[sl]])

        ev = [0]
        load_chunk(0)
        for t in range(NCH):
            sl = t % 2
            if t + 1 < NCH:
                load_chunk(t + 1)
            for zt in range(t * ZF_PER, min((t + 1) * ZF_PER, NSL // 128)):
                fw.dma("act", _L_dma_start(out=XSv[zt], in_=ztile), s_z, reads=[B_z], writes=[B_XS])
            for c in range(8):
                bk = c % 2
                for q in range(4):
                    fw.op("pe", _L_transpose(out=psf[bk][:, q * 128:(q + 1) * 128], in_=xs[sl][:, q, c * 128:(c + 1) * 128], identity=ident_f), reads=[B_xs[sl], B_ident], writes=[PF[bk]])
                eng = alt(ev[0]); ev[0] += 1
                fw.op(eng, copy_op(eng, xT[sl][:, c, :], psf[bk][:, :]), reads=[PF[bk]], writes=[B_xT[sl]])
            for pr in range(4):
                bk = 2 + pr % 2
                for c in range(8):
                    fw.op("pe", _L_matmul(psf[bk][:, :], lhsT=w_in_bf[:, c, 2048 + pr * 128:2048 + (pr + 1) * 128], rhs=xT[sl][:, c, :], start=(c == 0), stop=(c == 7)), reads=[B_xT[sl], B_win[c]], writes=[PF[bk]])
                fw.op("act", copy_op("act", KTst[sl][:, pr, :], psf[bk][:, :]), reads=[PF[bk]], writes=[B_KTst[sl]])
                fw.op("dve", _L_tensor_reduce(out=km[:, pr, 2 * t:2 * t + 2], in_=psf[bk][:, :].rearrange("p (a b) -> p a b", a=2), axis=AX.X, op=ALU.add), reads=[PF[bk]], writes=[B_km])
            fw.dma("sp", _L_dma_start(out=KTv[:, :, t * 512:(t + 1) * 512], in_=KTst[sl]), s_kst[sl], reads=[B_KTst[sl]], writes=[])
            for q in range(4):
                bk = 4 + q % 2
                for c in range(8):
                    fw.op("pe", _L_matmul(psf[bk][:, :], lhsT=xT[sl][:, c, q * 128:(q + 1) * 128], rhs=w_in_bf[:, c, 2560:3072], start=(c == 0), stop=(c == 7)), reads=[B_xT[sl], B_win[c]], writes=[PF[bk]])
                eng = alt(ev[0]); ev[0] += 1
                fw.op(eng, copy_op(eng, Vst[sl][:, :, q, 0:64], psf[bk][:, :].rearrange("p (h e) -> p h e", h=8)), reads=[PF[bk]], writes=[B_Vst[sl]])
            fw.dma("sp", _L_dma_start(out=VVv[:, :, 4 * t:4 * t + 4, :], in_=Vst[sl]), s_vst[sl], reads=[B_Vst[sl]], writes=[])
        fw.barrier()

        a1 = Alloc(R1, 59392)
        xos = [[a1(D, F32) for _ in range(3)] for _ in range(2)]
        xTo = [a1(8 * 258, BF16, "p (c n) -> p c n", c=8) for _ in range(2)]
        KTst2 = [a1(4 * 256, BF16, "p (c n) -> p c n", c=4) for _ in range(2)]
        Vst2 = [a1(8 * 2 * 65, BF16, "p (h k e) -> p h k e", h=8, k=2) for _ in range(2)]
        ccs = [a1(258, F32) for _ in range(2)]
        uu = [a1(258, F32) for _ in range(2)]
        tt_ = [a1(256, F32) for _ in range(2)]
        B_xos = [Buf("xos%d" % i) for i in range(2)]
        B_xTo = [Buf("xTo%d" % i) for i in range(2)]
        B_K2 = [Buf("K2_%d" % i) for i in range(2)]
        B_V2 = [Buf("V2_%d" % i) for i in range(2)]
        B_cc = [Buf("cc%d" % i) for i in range(2)]
        B_uu = [Buf("uu%d" % i) for i in range(2)]
        B_tt = [Buf("tt%d" % i) for i in range(2)]
        s_xos = [fw.dsem("xos%d" % i) for i in range(2)]
        s_k2 = [fw.dsem("k2_%d" % i) for i in range(2)]
        s_v2 = [fw.dsem("v2_%d" % i) for i in range(2)]
        for i in range(2):
            fw.op("pool", _L_memset(Vst2[i], 1.0), writes=[B_V2[i]])

        def load_own(i):
            sl = i % 2
            fw.dma("sp", _L_dma_start(out=xos[sl][0], in_=xo[i, 0:128, :]), s_xos[sl], writes=[B_xos[sl]])
            fw.dma("sp", _L_dma_start(out=xos[sl][1], in_=xo[i, 128:256, :]), s_xos[sl], writes=[B_xos[sl]])
            fw.dma("sp", _L_dma_start(out=xos[sl][2][0:2, :], in_=xo[i, 256:258, :]), s_xos[sl], writes=[B_xos[sl]])

        load_own(0)
        cvi = [0]
        for i in range(NSLOT):
            sl = i % 2
            if i + 1 < NSLOT:
                load_own(i + 1)
            c0 = i * 256
            for c in range(8):
                bk = c % 2
                fw.op("pe", _L_transpose(out=psf[bk][:, 0:128], in_=xos[sl][0][:, c * 128:(c + 1) * 128], identity=ident_f), reads=[B_xos[sl], B_ident], writes=[PF[bk]])
                fw.op("pe", _L_transpose(out=psf[bk][:, 128:256], in_=xos[sl][1][:, c * 128:(c + 1) * 128], identity=ident_f), reads=[B_xos[sl], B_ident], writes=[PF[bk]])
                fw.op("pe", _L_transpose(out=psf[bk][:, 256:258], in_=xos[sl][2][0:2, c * 128:(c + 1) * 128], identity=ident_f[0:2, 0:2]), reads=[B_xos[sl], B_ident], writes=[PF[bk]])
                eng = alt(ev[0]); ev[0] += 1
                fw.op(eng, copy_op(eng, xTo[sl][:, c, :], psf[bk][:, 0:258]), reads=[PF[bk]], writes=[B_xTo[sl]])
            for hp in range(4):
                bk = 2 + hp % 2
                for hh in range(2):
                    h = 2 * hp + hh
                    for c in range(8):
                        fw.op("pe", _L_matmul(psf[bk][0:64, hh * 256:(hh + 1) * 256], lhsT=w_in_bf[:, c, 1536 + h * 64:1536 + (h + 1) * 64], rhs=xTo[sl][:, c, 2:258], start=(c == 0), stop=(c == 7)), reads=[B_xTo[sl], B_win[c]], writes=[PF[bk]])
                eng = alt(ev[0]); ev[0] += 1
                fw.op(eng, copy_op(eng, Qaug[0:64, 2 * hp:2 * hp + 2, c0:c0 + 256], psf[bk][0:64, :].rearrange("p (a b) -> p a b", a=2)), reads=[PF[bk]], writes=[B_Q[i]])
            for pp in range(2):
                bk = 4 + pp % 2
                for q in range(2):
                    pr = 2 * pp + q
                    for c in range(8):
                        fw.op("pe", _L_matmul(psf[bk][:, q * 256:(q + 1) * 256], lhsT=w_in_bf[:, c, 2048 + pr * 128:2048 + (pr + 1) * 128], rhs=xTo[sl][:, c, 2:258], start=(c == 0), stop=(c == 7)), reads=[B_xTo[sl], B_win[c]], writes=[PF[bk]])
                eng = alt(ev[0]); ev[0] += 1
                fw.op(eng, copy_op(eng, KTst2[sl][:, 2 * pp:2 * pp + 2, :], psf[bk][:, :].rearrange("p (a b) -> p a b", a=2)), reads=[PF[bk]], writes=[B_K2[sl]])
            fw.dma("sp", _L_dma_start(out=KTv[:, :, S + c0:S + c0 + 256], in_=KTst2[sl]), s_k2[sl], reads=[B_K2[sl]], writes=[])
            for q in range(2):
                bk = 2 + q % 2
                for c in range(8):
                    fw.op("pe", _L_matmul(psf[bk][:, :], lhsT=xTo[sl][:, c, 2 + q * 128:2 + (q + 1) * 128], rhs=w_in_bf[:, c, 2560:3072], start=(c == 0), stop=(c == 7)), reads=[B_xTo[sl], B_win[c]], writes=[PF[bk]])
                eng = alt(ev[0]); ev[0] += 1
                fw.op(eng, copy_op(eng, Vst2[sl][:, :, q, 0:64], psf[bk][:, :].rearrange("p (h e) -> p h e", h=8)), reads=[PF[bk]], writes=[B_V2[sl]])
            fw.dma("sp", _L_dma_start(out=VVv[:, :, S // 128 + 2 * i:S // 128 + 2 * i + 2, :], in_=Vst2[sl]), s_v2[sl], reads=[B_V2[sl]], writes=[])
            for cc in range(4):
                k2 = cvi[0] % 2
                cvi[0] += 1
                banks = (0, 1, 4) if cc % 2 == 0 else (5, 2, 3)
                specs = ((banks[0], 0 + cc * 128, 2, 256), (banks[1], 512 + cc * 128, 0, 258), (banks[2], 1024 + cc * 128, 0, 258))
                for (bk, col, o0, n) in specs:
                    for c in range(8):
                        fw.op("pe", _L_matmul(psf[bk][:, 0:n], lhsT=w_in_bf[:, c, col:col + 128], rhs=xTo[sl][:, c, o0:o0 + n], start=(c == 0), stop=(c == 7)), reads=[B_xTo[sl], B_win[c]], writes=[PF[bk]])
                bcb, bcc, bch = banks
                fw.op("act", copy_op("act", ccs[k2], psf[bcc][:, 0:258]), reads=[PF[bcc]], writes=[B_cc[k2]])
                fw.op("dve", _L_tensor_tensor(out=uu[k2], in0=ccs[k2], in1=psf[bch][:, 0:258], op=ALU.mult), reads=[B_cc[k2], PF[bch]], writes=[B_uu[k2]])
                fw.op("dve", _L_tensor_scalar(out=tt_[k2], in0=uu[k2][:, 2:258], scalar1=wc[:, cc, 2:3], scalar2=None, op0=ALU.mult), reads=[B_uu[k2], B_const], writes=[B_tt[k2]])
                fw.op("dve", _L_scalar_tensor_tensor(out=tt_[k2], in0=uu[k2][:, 1:257], scalar=wc[:, cc, 1:2], in1=tt_[k2], op0=ALU.mult, op1=ALU.add), reads=[B_uu[k2], B_const, B_tt[k2]], writes=[B_tt[k2]])
                fw.op("dve", _L_scalar_tensor_tensor(out=tt_[k2], in0=uu[k2][:, 0:256], scalar=wc[:, cc, 0:1], in1=tt_[k2], op0=ALU.mult, op1=ALU.add), reads=[B_uu[k2], B_const, B_tt[k2]], writes=[B_tt[k2]])
                fw.op("dve", _L_tensor_tensor(out=yconvT[:, cc, c0:c0 + 256], in0=tt_[k2], in1=psf[bcb][:, 0:256], op=ALU.mult), reads=[B_tt[k2], PF[bcb]], writes=[B_yc[i]])
        fw.barrier()

        a2 = Alloc(R2, 20480)
        nm1 = a2(NSLOT * 256, F32, "p (i n) -> p i n", i=NSLOT)
        nm2 = a2(NSLOT * 256, F32, "p (i n) -> p i n", i=NSLOT)
        gm = [a2(256, F32, "p (h n) -> p h n", h=8) for _ in range(2)]
        m8 = [a2(64, F32, "p (h n) -> p h n", h=8) for _ in range(2)]
        a1 = Alloc(R1, 59392)
        bpad = [a1(8 * 128, BF16, "p (h n) -> p h n", h=8) for _ in range(2)]
        tA3 = [a1(256, F32, "p (h n) -> p h n", h=8) for _ in range(2)]
        tB3 = [a1(256, F32, "p (h n) -> p h n", h=8) for _ in range(2)]
        mm3 = [a1(24, F32, "p (r h) -> p r h", r=3) for _ in range(2)]
        B_nm = Buf("nm")
        B_gm = [Buf("gm%d" % i) for i in range(2)]
        B_m8 = [Buf("m8%d" % i) for i in range(2)]
        B_bp = [Buf("bp%d" % i) for i in range(2)]
        s_nm = fw.dsem("nm")
        s_kmh = fw.dsem("kmh")
        fw.dma("sp", _L_dma_start(out=nm1.rearrange("p i n -> p (i n)"), in_=nm1_d.partition_broadcast(128)), s_nm, writes=[B_nm])
        fw.dma("sp", _L_dma_start(out=nm2.rearrange("p i n -> p (i n)"), in_=nm2_d.partition_broadcast(128)), s_nm, writes=[B_nm])
        for i in range(2):
            fw.op("pool", _L_memset(bpad[i], 0.0), writes=[B_bp[i]])
        fw.op("dve", _L_tensor_copy(out=kmb, in_=km), reads=[B_km], writes=[B_km])
        kmh_v = kmh.rearrange("p (a b) n -> p a b n", b=2)
        fw.dma("sp", _L_dma_start(out=kmh_v[0:64, :, 0, :], in_=kmb[0:64, :, :]), s_kmh, reads=[B_km], writes=[B_kmh])
        fw.dma("sp", _L_dma_start(out=kmh_v[0:64, :, 1, :], in_=kmb[64:128, :, :]), s_kmh, reads=[B_km], writes=[B_kmh])
        for i in range(NSLOT):
            for t in range(2):
                k2 = (2 * i + t) % 2
                q0 = i * 256 + t * 128
                gb = k2
                for h in range(8):
                    fw.op("pe", _L_matmul(psf[gb][:, h * 32:(h + 1) * 32], lhsT=Qaug[0:64, h, q0:q0 + 128], rhs=kmh[0:64, h, :], start=True, stop=True), reads=[B_Q[i], B_kmh], writes=[PF[gb]])
                fw.op("dve", _L_tensor_tensor(out=gm[k2].rearrange("p h n -> p (h n)"), in0=psf[gb][:, 0:256], in1=nm1[:, i, :], op=ALU.add), reads=[PF[gb], B_nm], writes=[B_gm[k2]])
                G, A_, B_ = gm[k2], tA3[k2], tB3[k2]
                rd = [B_gm[k2], B_m8[k2]]
                wr_ = [B_m8[k2]]

                def bc(r):
                    return mm3[k2][:, r, :].unsqueeze(2).to_broadcast([128, 8, 32])
                fw.op("dve", _L_tensor_reduce(out=mm3[k2][:, 0, :], in_=G, axis=AX.X, op=ALU.max), reads=rd, writes=wr_)
                fw.op("dve", _L_tensor_tensor(out=A_, in0=G, in1=bc(0), op=ALU.is_ge), reads=rd, writes=wr_)
                fw.op("dve", _L_scalar_tensor_tensor(out=B_, in0=A_, scalar=-1.0e9, in1=G, op0=ALU.mult, op1=ALU.add), reads=rd, writes=wr_)
                fw.op("dve", _L_tensor_reduce(out=mm3[k2][:, 1, :], in_=B_, axis=AX.X, op=ALU.max), reads=rd, writes=wr_)
                fw.op("dve", _L_tensor_tensor(out=A_, in0=B_, in1=bc(1), op=ALU.is_ge), reads=rd, writes=wr_)
                fw.op("dve", _L_scalar_tensor_tensor(out=B_, in0=A_, scalar=-1.0e9, in1=B_, op0=ALU.mult, op1=ALU.add), reads=rd, writes=wr_)
                fw.op("dve", _L_tensor_reduce(out=mm3[k2][:, 2, :], in_=B_, axis=AX.X, op=ALU.max), reads=rd, writes=wr_)
                fw.op("dve", _L_tensor_tensor(out=A_, in0=G, in1=bc(2), op=ALU.is_lt), reads=rd, writes=wr_)
                fw.op("dve", _L_scalar_tensor_tensor(out=bpad[k2][:, :, 64:96], in0=A_, scalar=NEG, in1=nm2[:, i, :].rearrange("p (h n) -> p h n", h=8), op0=ALU.mult, op1=ALU.add), reads=rd + [B_nm], writes=[B_bp[k2]])
                pb = k2
                for h in range(8):
                    fw.op("pe", _L_transpose(out=psb[pb][:, h * 128:(h + 1) * 128], in_=bpad[k2][:, h, :], identity=ident_b), reads=[B_bp[k2], B_const], writes=[PB[pb]])
                fw.op("act", copy_op("act", Qaug[64:96, :, q0:q0 + 128], psb[pb][64:96, :].rearrange("p (h n) -> p h n", h=8)), reads=[PB[pb]], writes=[B_Q[i]])
        fw.barrier()

        if stop_after == "A":
            pass

        aB0 = Alloc(R0, 49152)
        KTaug = [aB0(TOT, BF16) for _ in range(2)]
        aB1 = Alloc(R1, 59392)
        yattnT = aB1(8 * NOWN, BF16, "p (h n) -> p h n", h=8)
        aB1.off = 32768
        Vh = [aB1(NKT * 65, BF16, "p (k e) -> p k e", e=65) for _ in range(2)]
        Pt = [aB1(1024, BF16) for _ in range(2)]
        aB2 = Alloc(R2, 20480)
        rcp = [aB2(256, F32) for _ in range(2)]
        bcs = [aB2(256, F32) for _ in range(2)]
        B_KTa = [Buf("KTa%d" % i) for i in range(2)]
        B_Vh = [Buf("Vh%d" % i) for i in range(2)]
        B_Pt = [Buf("Pt%d" % i) for i in range(2)]
        B_rcp = [Buf("rcp%d" % i) for i in range(2)]
        B_bcs = [Buf("bcs%d" % i) for i in range(2)]
        B_ya = [[Buf("ya%d_%d" % (h, i)) for i in range(NSLOT)] for h in range(8)]
        s_kta = [fw.dsem("kta%d" % i) for i in range(2)]
        s_vh = [fw.dsem("vh%d" % i) for i in range(2)]
        for s_ in range(2):
            fw.op("pool", _L_memset(KTaug[s_][64:128, :], 0.0), writes=[B_KTa[s_]])
            fw.op("pool", _L_memset(KTaug[s_][64:96, 0:S], 1.0), writes=[B_KTa[s_]])
            fw.op("pool", _L_affine_select(out=KTaug[s_][64:96, 0:S], in_=KTaug[s_][64:96, 0:S], pattern=[[1, S]], compare_op=ALU.is_ge, fill=0.0, base=0, channel_multiplier=-256), reads=[B_KTa[s_]], writes=[B_KTa[s_]])
            fw.op("pool", _L_affine_select(out=KTaug[s_][64:96, 0:S], in_=KTaug[s_][64:96, 0:S], pattern=[[-1, S]], compare_op=ALU.is_ge, fill=0.0, base=255, channel_multiplier=256), reads=[B_KTa[s_]], writes=[B_KTa[s_]])
        KTh = KT.rearrange("(h d) n -> h d n", d=64)

        def load_head(h):
            s_ = h % 2
            fw.dma("sp", _L_dma_start(out=KTaug[s_][0:64, :], in_=KTh[h]), s_kta[s_], reads=[B_KT], writes=[B_KTa[s_]])
            fw.dma("sp", _L_dma_start(out=Vh[s_], in_=VV[h]), s_vh[s_], reads=[B_VV], writes=[B_Vh[s_]])

        load_head(0)
        epi_pending = []
        sb_i = [0]
        ob_i = [0]
        pt_i = [0]
        for h in range(8):
            s_ = h % 2
            if h + 1 < 8:
                load_head(h + 1)
            for i in range(NSLOT):
                q0 = i * 256
                units = [256 * n for n in range(4 * i + 3)] + [S + q0]
                nu = len(units)
                ob = 4 + ob_i[0] % 2
                ob_i[0] += 1
                ndu = nu // 2

                def qk2(du):
                    j = sb_i[0] % 2
                    sb_i[0] += 1
                    for k in range(2):
                        u = 2 * du + k
                        kc = units[u]
                        own = (u == nu - 1)
                        for t in range(2):
                            o_ap = psw[j][:, k * 512 + t * 256:k * 512 + (t + 1) * 256]
                            fw.op("pe", _L_matmul(o_ap, lhsT=KTaug[s_][:, kc + t * 128:kc + (t + 1) * 128], rhs=Qaug[:, h, q0:q0 + 256], start=True, stop=(not own)), reads=[B_KTa[s_], B_Q[i]], writes=[PW[j]])
                            if own:
                                fw.op("pe", _L_matmul(o_ap, lhsT=ident_b, rhs=CBm[:, t, :], start=False, stop=True), reads=[B_const], writes=[PW[j]])
                    return j

                def expo2(j):
                    pi = pt_i[0] % 2
                    pt_i[0] += 1
                    fw.op("act", _L_activation(out=Pt[pi], in_=psw[j], func=AF.Exp, scale=0.125), reads=[PW[j]], writes=[B_Pt[pi]])
                    return pi

                def pv2(du, pi):
                    for k in range(2):
                        u = 2 * du + k
                        kc = units[u]
                        for t in range(2):
                            kt = kc // 128 + t
                            fw.op("pe", _L_matmul(psf[ob][0:65, 0:256], lhsT=Vh[s_][:, kt, :], rhs=Pt[pi][:, k * 512 + t * 256:k * 512 + (t + 1) * 256], start=(u == 0 and t == 0), stop=(u == nu - 1 and t == 1)), reads=[B_Vh[s_], B_Pt[pi]], writes=[PF[ob]])

                js = [qk2(0)]
                for du in range(ndu):
                    if du + 1 < ndu:
                        js.append(qk2(du + 1))
                    pi = expo2(js[du])
                    pv2(du, pi)
                    if du == 0 and epi_pending:
                        epi_pending.pop()()
                k2 = ob_i[0] % 2
                fw.op("dve", _L_reciprocal(out=rcp[k2][64:65, :], in_=psf[ob][64:65, 0:256]), reads=[PF[ob]], writes=[B_rcp[k2]])

                def epilogue(k2=k2, ob=ob, h=h, i=i, q0=q0):
                    fw.op("pe", _L_matmul(pse[0:64, 0:256], lhsT=ones_f[64:65, 0:64], rhs=rcp[k2][64:65, :], start=True, stop=True), reads=[B_rcp[k2], B_const], writes=[PB[0]])
                    fw.op("dve", copy_op("dve", bcs[k2][0:64, :], pse[0:64, 0:256]), reads=[PB[0]], writes=[B_bcs[k2]])
                    fw.op("dve", _L_tensor_tensor(out=yattnT[0:64, h, q0:q0 + 256], in0=psf[ob][0:64, 0:256], in1=bcs[k2][0:64, :], op=ALU.mult), reads=[PF[ob], B_bcs[k2]], writes=[B_ya[h][i]])
                epi_pending.append(epilogue)
        while epi_pending:
            epi_pending.pop()()
        fw.barrier()

        aC = Alloc(R0, 81920)
        wo_c = aC(4 * D, BF16, "p (c n) -> p c n", c=4)
        wo_a = aC(8 * D, BF16, "p (c n) -> p c n", c=8)
        wpg = aC(8 * D, BF16, "p (c n) -> p c n", c=8)
        wpp = aC(2 * D, BF16, "p (c n) -> p c n", c=2)
        wr = aC(8 * 36, F32, "p (c n) -> p c n", c=8)
        lng1 = aC(D, F32)
        lnb1 = aC(D, F32)
        cst = [aC(D, F32) for _ in range(4)]
        B_wC = Buf("wC")
        B_cst = [Buf("cst%d" % i) for i in range(4)]
        s_cst = [fw.dsem("cst%d" % i) for i in range(4)]
        s_wC = fw.dsem("wCs")
        fw.dma("sp", _L_dma_start(out=lng1, in_=ln1_g.partition_broadcast(128)), s_wC, writes=[B_wC])
        fw.dma("sp", _L_dma_start(out=lnb1, in_=ln1_b.partition_broadcast(128)), s_wC, writes=[B_wC])
        with nc.allow_non_contiguous_dma(reason="small router weights"):
            fw.dma("sp", _L_dma_start(out=wr[:, :, 0:4], in_=w_rg.rearrange("(c p) n -> p c n", p=128)), s_wC, writes=[B_wC])
            fw.dma("sp", _L_dma_start(out=wr[:, :, 4:36], in_=w_re.rearrange("(c p) n -> p c n", p=128)), s_wC, writes=[B_wC])
        pieces = []
        for c in range(4):
            pieces.append((w_out[c * 128:(c + 1) * 128, :], wo_c[:, c, :], 128))
        for hh in range(8):
            pieces.append((w_out[512 + hh * 64:512 + (hh + 1) * 64, :], wo_a[0:64, hh, :], 64))
        for c in range(8):
            pieces.append((w_pg[c * 128:(c + 1) * 128, :], wpg[:, c, :], 128))
        for c in range(2):
            pieces.append((w_pp[c * 128:(c + 1) * 128, :], wpp[:, c, :], 128))
        for k, (src, dst, npart) in enumerate(pieces):
            sl = k % 4
            fw.dma("sp", _L_dma_start(out=cst[sl][0:npart, :], in_=src), s_cst[sl], writes=[B_cst[sl]])
            eng = ("pool", "act", "dve")[k % 3]
            fw.op(eng, copy_op(eng, dst, cst[sl][0:npart, :]), reads=[B_cst[sl]], writes=[B_wC])

        aC1 = Alloc(R1 + 32768, 59392 - 32768)
        aC2 = Alloc(R2, 20480)
        xr = [aC1(D, F32) for _ in range(2)]
        h1 = [aC1(D, F32) for _ in range(2)]
        x1b = [aC1(D, BF16) for _ in range(2)]
        x1Tb = aC1(8 * 128, BF16, "p (c n) -> p c n", c=8)
        pTb = aC1(2 * 128, BF16, "p (c n) -> p c n", c=2)
        prow = [aC1(PLE, F32) for _ in range(2)]
        x1Tf = aC2(8 * 128, F32, "p (c n) -> p c n", c=8)
        sgt = aC2(D, F32)
        acc = [aC2(D, F32) for _ in range(2)]
        rs = aC2(512, F32)
        B_xr = [Buf("xr%d" % i) for i in range(2)]
        B_h1 = [Buf("h1_%d" % i) for i in range(2)]
        B_x1b = [Buf("x1b%d" % i) for i in range(2)]
        B_x1Tb = Buf("x1Tb")
        B_x1Tf = Buf("x1Tf")
        B_pTb = Buf("pTb")
        B_prow = [Buf("prow%d" % i) for i in range(2)]
        B_sgt = Buf("sgt")
        B_acc = [Buf("acc%d" % i) for i in range(2)]
        B_rs = Buf("rs")
        s_xr = [fw.dsem("xr%d" % i) for i in range(2)]
        s_pr = [fw.dsem("pr%d" % i) for i in range(2)]
        s_acc = [fw.dsem("accst%d" % i) for i in range(2)]
        s_sc = [fw.dsem("scat%d" % i) for i in range(2)]
        s_dbg = fw.dsem("dbg")
        B_ACCD = [Buf("ACCD%d" % t) for t in range(NT)]
        B_dbg = Buf("dbg")
        L36 = rs[:, 0:36]
        gmax = rs[:, 36:37]
        gsum = rs[:, 37:38]
        gp = rs[:, 38:39]
        oh = rs[:, 40:44]
        pen = rs[:, 44:48]
        junk4 = rs[:, 48:52]
        lem = rs[:, 64:96]
        r8 = rs[:, 96:104]
        sel1 = rs[:, 128:160]
        sel2 = rs[:, 160:192]
        selb = rs[:, 192:224]
        tmpa = rs[:, 224:256]
        tmpb = rs[:, 256:288]
        dd = rs[:, 288:289]
        rr = rs[:, 289:290]
        den = rs[:, 290:291]
        slf = rs[:, 292:294]
        stats = rs[:, 300:312]
        mv = rs[:, 312:314]
        rstd = rs[:, 314:315]

        dmy = rs[:, 320:324]
        B_dmy = Buf("dmy")
        fw.op("pool", _L_memset(rs[:, 316:324], 0.0), writes=[B_dmy])

        def prefetch_table(func):
            fw.op("act", _L_activation(out=dmy[:, 2:4], in_=dmy[:, 0:2], func=func), reads=[], writes=[B_dmy])

        def layer_norm(eng_list, h_ap, B_h, g_ap, b_ap, B_gb):
            fw.op("dve", _L_bn_stats(out=stats[:, 0:6], in_=h_ap[:, 0:512]), reads=[B_h], writes=[B_rs])
            fw.op("dve", _L_bn_stats(out=stats[:, 6:12], in_=h_ap[:, 512:1024]), reads=[B_h], writes=[B_rs])
            fw.op("dve", _L_bn_aggr(out=mv, in_=stats), reads=[B_rs], writes=[B_rs])
            fw.op("act", _L_activation(out=rstd, in_=mv[:, 1:2], func=AF.Sqrt, bias=EPS, scale=1.0), reads=[B_rs], writes=[B_rs])
            prefetch_table(AF.Exp)
            fw.op("dve", _L_reciprocal(out=rstd, in_=rstd), reads=[B_rs], writes=[B_rs])
            fw.op("dve", _L_tensor_scalar(out=h_ap, in0=h_ap, scalar1=mv[:, 0:1], scalar2=rstd, op0=ALU.subtract, op1=ALU.mult), reads=[B_h, B_rs], writes=[B_h])
            fw.op("dve", _L_tensor_tensor(out=h_ap, in0=h_ap, in1=g_ap, op=ALU.mult), reads=[B_h, B_gb], writes=[B_h])
            fw.op("dve", _L_tensor_tensor(out=h_ap, in0=h_ap, in1=b_ap, op=ALU.add), reads=[B_h, B_gb], writes=[B_h])

        def load_tok(t):
            k2 = t % 2
            i, hf = t // 2, t % 2
            fw.dma("sp", _L_dma_start(out=xr[k2], in_=xo[i, 2 + hf * 128:2 + (hf + 1) * 128, :]), s_xr[k2], writes=[B_xr[k2]])
            fw.dma("sp", _L_dma_start(out=prow[k2], in_=po[i, hf * 128:(hf + 1) * 128, :]), s_pr[k2], writes=[B_prow[k2]])

        def out_proj(t):
            i = t // 2
            tk = t * 128
            for half in range(2):
                bk = 2 + half
                n_mm = 12
                m = 0
                for cc in range(4):
                    fw.op("pe", _L_matmul(psf[bk][:, :], lhsT=yconvT[:, cc, tk:tk + 128], rhs=wo_c[:, cc, half * 512:(half + 1) * 512], start=(m == 0), stop=False), reads=[B_yc[i], B_wC], writes=[PF[bk]])
                    m += 1
                for hh in range(8):
                    fw.op("pe", _L_matmul(psf[bk][:, :], lhsT=yattnT[0:64, hh, tk:tk + 128], rhs=wo_a[0:64, hh, half * 512:(half + 1) * 512], start=False, stop=(m == n_mm - 1)), reads=[B_ya[hh][i], B_wC], writes=[PF[bk]])
                    m += 1

        load_tok(0)
        out_proj(0)
        prefetch_table(AF.Sqrt)
        for t in range(NT):
            k2 = t % 2
            i, hf = t // 2, t % 2
            tk = t * 128
            if t + 1 < NT:
                load_tok(t + 1)
            for half in range(2):
                bk = 2 + half
                fw.op("dve", _L_scalar_tensor_tensor(out=h1[k2][:, half * 512:(half + 1) * 512], in0=xr[k2][:, half * 512:(half + 1) * 512], scalar=ALPHA, in1=psf[bk][:, :], op0=ALU.mult, op1=ALU.add), reads=[B_xr[k2], PF[bk]], writes=[B_h1[k2]])
            if dbg:
                fw.dma("sp", _L_dma_start(out=dbg_mix[tk:tk + 128, :], in_=h1[k2]), s_dbg, reads=[B_h1[k2]], writes=[B_dbg])
            layer_norm(None, h1[k2], B_h1[k2], lng1, lnb1, B_wC)
            if dbg:
                fw.dma("sp", _L_dma_start(out=dbg_x1[tk:tk + 128, :], in_=h1[k2]), s_dbg, reads=[B_h1[k2]], writes=[B_dbg])
            for g in range(2):
                bk = 2 + g
                for q in range(4):
                    c = 4 * g + q
                    fw.op("pe", _L_transpose(out=psf[bk][:, q * 128:(q + 1) * 128], in_=h1[k2][:, c * 128:(c + 1) * 128], identity=ident_f), reads=[B_h1[k2], B_ident], writes=[PF[bk]])
                fw.op("act", copy_op("act", x1Tf[:, 4 * g:4 * g + 4, :], psf[bk][:, :].rearrange("p (a b) -> p a b", a=4)), reads=[PF[bk]], writes=[B_x1Tf])
                fw.op("dve", copy_op("dve", x1Tb[:, 4 * g:4 * g + 4, :], psf[bk][:, :].rearrange("p (a b) -> p a b", a=4)), reads=[PF[bk]], writes=[B_x1Tb])
            fw.op("act", copy_op("act", x1b[k2], h1[k2]), reads=[B_h1[k2]], writes=[B_x1b[k2]])
            for c in range(8):
                fw.op("pe", _L_matmul(psf[4][:, 0:36], lhsT=x1Tf[:, c, :], rhs=wr[:, c, :], start=(c == 0), stop=(c == 7)), reads=[B_x1Tf, B_wC], writes=[PF[4]])
            for half in range(2):
                bk = half
                for c in range(8):
                    fw.op("pe", _L_matmul(psf[bk][:, :], lhsT=x1Tb[:, c, :], rhs=wpg[:, c, half * 512:(half + 1) * 512], start=(c == 0), stop=(c == 7)), reads=[B_x1Tb, B_wC], writes=[PF[bk]])
            fw.op("dve", _L_tensor_tensor(out=L36, in0=psf[4][:, 0:36], in1=bias36, op=ALU.add), reads=[PF[4], B_const], writes=[B_rs])
            fw.op("dve", _L_tensor_reduce(out=gmax, in_=rs[:, 0:4], axis=AX.X, op=ALU.max), reads=[B_rs], writes=[B_rs])
            fw.op("dve", _L_tensor_scalar(out=dd, in0=gmax, scalar1=-1.0, scalar2=None, op0=ALU.mult), reads=[B_rs], writes=[B_rs])
            fw.op("act", _L_activation(out=junk4, in_=rs[:, 0:4], func=AF.Exp, bias=dd, scale=1.0, accum_out=gsum), reads=[B_rs], writes=[B_rs])
            fw.op("dve", _L_reciprocal(out=gp, in_=gsum), reads=[B_rs], writes=[B_rs])
            fw.op("dve", _L_tensor_scalar(out=oh, in0=rs[:, 0:4], scalar1=gmax, scalar2=None, op0=ALU.is_equal), reads=[B_rs], writes=[B_rs])
            fw.op("dve", _L_tensor_scalar(out=pen, in0=oh, scalar1=1.0, scalar2=1.0e30, op0=ALU.subtract, op1=ALU.mult), reads=[B_rs], writes=[B_rs])
            for g in range(4):
                fw.op("dve", _L_tensor_scalar(out=lem[:, 8 * g:8 * g + 8], in0=rs[:, 4 + 8 * g:12 + 8 * g], scalar1=pen[:, g:g + 1], scalar2=None, op0=ALU.add), reads=[B_rs], writes=[B_rs])
            fw.op("dve", _L_max(out=r8, in_=lem), reads=[B_rs], writes=[B_rs])
            fw.op("dve", _L_tensor_scalar(out=sel1, in0=lem, scalar1=r8[:, 0:1], scalar2=None, op0=ALU.is_equal), reads=[B_rs], writes=[B_rs])
            fw.op("dve", _L_tensor_scalar(out=sel2, in0=lem, scalar1=r8[:, 1:2], scalar2=None, op0=ALU.is_equal), reads=[B_rs], writes=[B_rs])
            fw.op("dve", _L_tensor_tensor(out=selb, in0=sel1, in1=sel2, op=ALU.add), reads=[B_rs], writes=[B_rs])
            fw.op("dve", _L_tensor_tensor(out=dd, in0=r8[:, 1:2], in1=r8[:, 0:1], op=ALU.subtract), reads=[B_rs], writes=[B_rs])
            fw.op("act", _L_activation(out=rr, in_=dd, func=AF.Exp), reads=[B_rs], writes=[B_rs])
            prefetch_table(AF.Sigmoid)
            fw.op("dve", _L_tensor_scalar(out=den, in0=rr, scalar1=1.0, scalar2=None, op0=ALU.add), reads=[B_rs], writes=[B_rs])
            fw.op("dve", _L_reciprocal(out=den, in_=den), reads=[B_rs], writes=[B_rs])
            fw.op("dve", _L_tensor_tensor(out=w12[:, t, 0:1], in0=gp, in1=den, op=ALU.mult), reads=[B_rs], writes=[B_w12[t]])
            fw.op("dve", _L_tensor_tensor(out=w12[:, t, 1:2], in0=w12[:, t, 0:1], in1=rr, op=ALU.mult), reads=[B_rs, B_w12[t]], writes=[B_w12[t]])
            fw.op("pe", _L_matmul(psf[5][:, 0:32], lhsT=Lst, rhs=selb, start=True, stop=False), reads=[B_rs, B_const], writes=[PF[5]])
            fw.op("pe", _L_matmul(psf[5][:, 0:32], lhsT=ones_f, rhs=Spre, start=False, stop=True), reads=[B_Spre, B_const], writes=[PF[5]])
            if t + 1 < NT:
                out_proj(t + 1)
            fw.op("dve", _L_tensor_tensor(out=tmpa, in0=psf[5][:, 0:32], in1=eC, op=ALU.add), reads=[PF[5], B_const], writes=[B_rs])
            fw.op("dve", _L_tensor_scalar(out=tmpb, in0=psf[5][:, 0:32], scalar1=float(CAP), scalar2=1.0e6, op0=ALU.is_ge, op1=ALU.mult), reads=[PF[5]], writes=[B_rs])
            fw.op("dve", _L_tensor_tensor(out=tmpa, in0=tmpa, in1=tmpb, op=ALU.add), reads=[B_rs], writes=[B_rs])
            fw.op("dve", _L_tensor_tensor(out=tmpb, in0=tmpa, in1=sel1, op=ALU.mult), reads=[B_rs], writes=[B_rs])
            fw.op("dve", _L_tensor_reduce(out=slf[:, 0:1], in_=tmpb, axis=AX.X, op=ALU.add), reads=[B_rs], writes=[B_rs])
            fw.op("dve", _L_tensor_tensor(out=tmpb, in0=tmpa, in1=sel2, op=ALU.mult), reads=[B_rs], writes=[B_rs])
            fw.op("dve", _L_tensor_reduce(out=slf[:, 1:2], in_=tmpb, axis=AX.X, op=ALU.add), reads=[B_rs], writes=[B_rs])
            fw.op("dve", _L_tensor_copy(out=slot_i[:, t, :], in_=slf), reads=[B_rs], writes=[B_slot[t]])
            fw.op("dve", _L_tensor_tensor(out=Spre, in0=Spre, in1=selb, op=ALU.add), reads=[B_rs, B_Spre], writes=[B_Spre])
            for k in range(2):
                fw.dma("pool", _L_indirect_dma_start(out=XS, out_offset=bass.IndirectOffsetOnAxis(ap=slot_i[:, t, k:k + 1], axis=0), in_=x1b[k2], in_offset=None, bounds_check=NSL - 1, oob_is_err=False), s_sc[k2], reads=[B_x1b[k2], B_slot[t], B_XS], writes=[B_XS])
            for half in range(2):
                fw.op("act", _L_activation(out=sgt[:, half * 512:(half + 1) * 512], in_=psf[half][:, :], func=AF.Sigmoid), reads=[PF[half]], writes=[B_sgt])
            prefetch_table(AF.Sqrt)
            for c in range(2):
                fw.op("pe", _L_transpose(out=psf[4][:, c * 128:(c + 1) * 128], in_=prow[k2][:, c * 128:(c + 1) * 128], identity=ident_f), reads=[B_prow[k2], B_ident], writes=[PF[4]])
            fw.op("act", copy_op("act", pTb, psf[4][:, 0:256].rearrange("p (a b) -> p a b", a=2)), reads=[PF[4]], writes=[B_pTb])
            for half in range(2):
                bk = half
                for c in range(2):
                    fw.op("pe", _L_matmul(psf[bk][:, :], lhsT=pTb[:, c, :], rhs=wpp[:, c, half * 512:(half + 1) * 512], start=(c == 0), stop=(c == 1)), reads=[B_pTb, B_wC], writes=[PF[bk]])
                fw.op("dve", _L_tensor_tensor(out=acc[k2][:, half * 512:(half + 1) * 512], in0=sgt[:, half * 512:(half + 1) * 512], in1=psf[bk][:, :], op=ALU.mult), reads=[B_sgt, PF[bk]], writes=[B_acc[k2]])
            fw.op("dve", _L_scalar_tensor_tensor(out=acc[k2], in0=h1[k2], scalar=ALPHA, in1=acc[k2], op0=ALU.mult, op1=ALU.add), reads=[B_h1[k2], B_acc[k2]], writes=[B_acc[k2]])
            fw.dma("sp", _L_dma_start(out=ACCD[tk:tk + 128, :], in_=acc[k2]), s_acc[k2], reads=[B_acc[k2]], writes=[B_ACCD[t]])
        fw.barrier()

        aD = Alloc(R0, 98304)
        wstg = [aD(8 * DE, F32) for _ in range(3)]
        wbf = [aD(8 * DE, BF16) for _ in range(6)]
        aD1 = Alloc(R1, 59392)
        Xe = [aD1(D, BF16) for _ in range(2)]
        XeT = [aD1(8 * 256, BF16, "p (c n) -> p c n", c=8) for _ in range(2)]
        hT = [aD1(4 * 256, BF16, "p (c n) -> p c n", c=4) for _ in range(2)]
        sgu = [aD1(256, F32) for _ in range(2)]
        Yst = [aD1(D, F32) for _ in range(2)]
        Y1 = [aD1(D, F32) for _ in range(2)]
        Y2 = [aD1(D, F32) for _ in range(2)]
        accr = [aD1(D, F32) for _ in range(3)]
        aD2 = Alloc(R2, 20480)
        lng2 = aD2(D, F32)
        lnb2 = aD2(D, F32)
        rs2 = aD2(64, F32)
        B_wstg = [Buf("wstg%d" % i) for i in range(3)]
        B_wbf = [Buf("wbf%d" % i) for i in range(6)]
        B_Xe = [Buf("Xe%d" % i) for i in range(2)]
        B_XeT = [Buf("XeT%d" % i) for i in range(2)]
        B_hT = [Buf("hT%d" % i) for i in range(2)]
        B_sgu = [Buf("sgu%d" % i) for i in range(2)]
        B_Yst = [Buf("Yst%d" % i) for i in range(2)]
        B_Y1 = [Buf("Y1_%d" % i) for i in range(2)]
        B_Y2 = [Buf("Y2_%d" % i) for i in range(2)]
        B_accr = [Buf("accr%d" % i) for i in range(3)]
        B_ln2 = Buf("ln2")
        B_YS = Buf("YS")
        s_wstg = [fw.dsem("wstg%d" % i) for i in range(3)]
        s_xe = [fw.dsem("xe%d" % i) for i in range(2)]
        s_yst = [fw.dsem("yst%d" % i) for i in range(2)]
        s_g = [fw.dsem("gath%d" % i) for i in range(2)]
        s_accr = [fw.dsem("accr%d" % i) for i in range(3)]
        s_out = [fw.dsem("out%d" % i) for i in range(3)]
        s_ln2 = fw.dsem("ln2")
        B_out = Buf("out")
        fw.dma("sp", _L_dma_start(out=lng2, in_=ln2_g.partition_broadcast(128)), s_ln2, writes=[B_ln2])
        fw.dma("sp", _L_dma_start(out=lnb2, in_=ln2_b.partition_broadcast(128)), s_ln2, writes=[B_ln2])

        mats = []
        for ex in range(NE):
            mats.append(("g", ex))
            mats.append(("u", ex))
            mats.append(("d", ex))

        CAST_ENG = ("pool", "act", "dve", "dve", "pool", "act", "dve", "act", "pool", "dve", "act", "dve")

        pending = {"act": [], "dve": []}

        def load_mat(mi):
            kind, ex = mats[mi]
            sl = mi % 3
            bs = mi % 6
            if kind in ("g", "u"):
                src = (w_gate if kind == "g" else w_up)[ex].rearrange("(p c) f -> p c f", c=8)
                dstv = wstg[sl].rearrange("p (c f) -> p c f", c=8)
            else:
                src = w_down[ex].rearrange("(c p) n -> p c n", p=128)
                dstv = wstg[sl].rearrange("p (c n) -> p c n", c=4)
            fw.dma("sp", _L_dma_start(out=dstv, in_=src), s_wstg[sl], writes=[B_wstg[sl]])
            for k in range(4):
                eng = CAST_ENG[(4 * mi + k) % 12]
                o, i_ = wbf[bs][:, k * 1024:(k + 1) * 1024], wstg[sl][:, k * 1024:(k + 1) * 1024]

                def emit(eng=eng, o=o, i_=i_, sl=sl, bs=bs):
                    fw.op(eng, copy_op(eng, o, i_), reads=[B_wstg[sl]], writes=[B_wbf[bs]])
                if eng == "pool":
                    emit()
                else:
                    pending[eng].append(emit)

        def drain(eng, n):
            for _ in range(n):
                if pending[eng]:
                    pending[eng].pop(0)()

        XSe = XS.rearrange("(e s p) d -> e s p d", s=2, p=128)
        YSe = YS.rearrange("(e s p) d -> e s p d", s=2, p=128)

        def load_xe(ex):
            for s2 in range(2):
                fw.dma("act", _L_dma_start(out=Xe[s2], in_=XSe[ex, s2]), s_xe[s2], reads=[B_XS], writes=[B_Xe[s2]])

        for mi in range(3):
            load_mat(mi)
        drain("act", 99)
        drain("dve", 99)
        load_xe(0)
        yi = [0]
        for ex in range(NE):
            k2 = ex % 2
            bb = (3 * ex) % 6
            wg = wbf[bb].rearrange("p (c f) -> p c f", c=8)
            wu = wbf[bb + 1].rearrange("p (c f) -> p c f", c=8)
            wd = wbf[bb + 2].rearrange("p (c n) -> p c n", c=4)
            if ex + 1 < NE:
                for q in range(3):
                    load_mat(3 * (ex + 1) + q)
            for s2 in range(2):
                pb = s2
                for c in range(8):
                    fw.op("pe", _L_transpose(out=psb[pb][:, c * 128:(c + 1) * 128], in_=Xe[s2].rearrange("t (p c) -> t c p", c=8)[:, c, :], identity=ident_b), reads=[B_Xe[s2], B_const], writes=[PB[pb]])
                eng = alt(s2)
                fw.op(eng, copy_op(eng, XeT[k2][:, :, s2 * 128:(s2 + 1) * 128], psb[pb][:, :].rearrange("p (c n) -> p c n", c=8)), reads=[PB[pb]], writes=[B_XeT[k2]])
            if ex + 1 < NE:
                load_xe(ex + 1)
            for fo in range(4):
                bg, bu = (0, 1) if fo % 2 == 0 else (2, 3)
                for (bk, wm, bi) in ((bg, wg, bb), (bu, wu, bb + 1)):
                    for c in range(8):
                        fw.op("pe", _L_matmul(psf[bk][:, 0:256], lhsT=wm[:, c, fo * 128:(fo + 1) * 128], rhs=XeT[k2][:, c, :], start=(c == 0), stop=(c == 7)), reads=[B_XeT[k2], B_wbf[bi]], writes=[PF[bk]])
                sq = fo % 2
                fw.op("act", _L_activation(out=sgu[sq], in_=psf[bg][:, 0:256], func=AF.Silu), reads=[PF[bg]], writes=[B_sgu[sq]])
                fw.op("dve", _L_tensor_tensor(out=hT[k2][:, fo, :], in0=sgu[sq], in1=psf[bu][:, 0:256], op=ALU.mult), reads=[B_sgu[sq], PF[bu]], writes=[B_hT[k2]])
                drain("act", 1)
                drain("dve", 1)
            for s2 in range(2):
                ys = yi[0] % 2
                yi[0] += 1
                for half in range(2):
                    bk = 4 + half
                    for fo in range(4):
                        fw.op("pe", _L_matmul(psf[bk][:, :], lhsT=hT[k2][:, fo, s2 * 128:(s2 + 1) * 128], rhs=wd[:, fo, half * 512:(half + 1) * 512], start=(fo == 0), stop=(fo == 3)), reads=[B_hT[k2], B_wbf[bb + 2]], writes=[PF[bk]])
                    eng = alt(half)
                    fw.op(eng, copy_op(eng, Yst[ys][:, half * 512:(half + 1) * 512], psf[bk][:, :]), reads=[PF[bk]], writes=[B_Yst[ys]])
                fw.dma("act", _L_dma_start(out=YSe[ex, s2], in_=Yst[ys]), s_yst[ys], reads=[B_Yst[ys]], writes=[])
            drain("act", 99)
            drain("dve", 99)

        fw.barrier()

        st2 = rs2[:, 0:12]
        mv2 = rs2[:, 12:14]
        rstd2 = rs2[:, 14:15]

        def fin_issue(t):
            k2, k3 = t % 2, t % 3
            tk = t * 128
            fw.op("pool", _L_memset(Y1[k2], 0.0), writes=[B_Y1[k2]])
            fw.op("pool", _L_memset(Y2[k2], 0.0), writes=[B_Y2[k2]])
            fw.dma("sp", _L_dma_start(out=accr[k3], in_=ACCD[tk:tk + 128, :]), s_accr[k3], reads=[B_ACCD[t]], writes=[B_accr[k3]])
            fw.dma("pool", _L_indirect_dma_start(out=Y1[k2], out_offset=None, in_=YS, in_offset=bass.IndirectOffsetOnAxis(ap=slot_i[:, t, 0:1], axis=0), bounds_check=NSL - 1, oob_is_err=False), s_g[k2], reads=[B_YS, B_slot[t]], writes=[B_Y1[k2]])
            fw.dma("pool", _L_indirect_dma_start(out=Y2[k2], out_offset=None, in_=YS, in_offset=bass.IndirectOffsetOnAxis(ap=slot_i[:, t, 1:2], axis=0), bounds_check=NSL - 1, oob_is_err=False), s_g[k2], reads=[B_YS, B_slot[t]], writes=[B_Y2[k2]])

        def fin_front(t):
            k2, k3 = t % 2, t % 3
            h_ap, B_h, B_r = accr[k3], B_accr[k3], B_rs
            fw.op("dve", _L_scalar_tensor_tensor(out=h_ap, in0=Y1[k2], scalar=w12[:, t, 0:1], in1=h_ap, op0=ALU.mult, op1=ALU.add), reads=[B_Y1[k2], B_w12[t], B_h], writes=[B_h])
            fw.op("dve", _L_scalar_tensor_tensor(out=h_ap, in0=Y2[k2], scalar=w12[:, t, 1:2], in1=h_ap, op0=ALU.mult, op1=ALU.add), reads=[B_Y2[k2], B_w12[t], B_h], writes=[B_h])
            fw.op("dve", _L_bn_stats(out=st2[:, 0:6], in_=h_ap[:, 0:512]), reads=[B_h], writes=[B_r])
            fw.op("dve", _L_bn_stats(out=st2[:, 6:12], in_=h_ap[:, 512:1024]), reads=[B_h], writes=[B_r])
            fw.op("dve", _L_bn_aggr(out=mv2, in_=st2), reads=[B_r], writes=[B_r])
            fw.op("act", _L_activation(out=rstd2, in_=mv2[:, 1:2], func=AF.Sqrt, bias=EPS, scale=1.0), reads=[B_r], writes=[B_r])
            fw.op("dve", _L_reciprocal(out=rstd2, in_=rstd2), reads=[B_r], writes=[B_r])
            fw.op("dve", _L_tensor_scalar(out=h_ap, in0=h_ap, scalar1=mv2[:, 0:1], scalar2=rstd2, op0=ALU.subtract, op1=ALU.mult), reads=[B_h, B_r], writes=[B_h])
            fw.op("dve", _L_tensor_tensor(out=h_ap, in0=h_ap, in1=lng2, op=ALU.mult), reads=[B_h, B_ln2], writes=[B_h])

        def fin_back(t):
            k3 = t % 3
            tk = t * 128
            fw.op("pool", _L_tensor_tensor(out=accr[k3], in0=accr[k3], in1=lnb2, op=ALU.add), reads=[B_accr[k3], B_ln2], writes=[B_accr[k3]])
            fw.dma("sp", _L_dma_start(out=out[tk:tk + 128, :], in_=accr[k3]), s_out[k3], reads=[B_accr[k3]], writes=[])

        fin_issue(0)
        for t in range(NT):
            if t + 1 < NT:
                fin_issue(t + 1)
            fin_front(t)
            if t >= 1:
                fin_back(t - 1)
        fin_back(NT - 1)
        fw.barrier()
        with nc.allow_non_contiguous_dma(reason="tiny strided parameter loads"):
            fw.replay()
    nc._fw_names = fw.names
    return nc


def own_blocks(r, NB):
    NSLOT = NB // 4
    js = []
    for i in range(NSLOT):
        if i < NSLOT // 2:
            js.append(r + 4 * i)
        else:
            js.append(NB - 1 - r - 4 * (NSLOT - 1 - i))
    return js


def make_in_maps(inputs, S):
    NB = S // 256
    x = np.asarray(inputs["x"], dtype=np.float32)
    p = np.asarray(inputs["p"], dtype=np.float32)
    nbatch = x.shape[0]
    shared = {
        "w_in": inputs["w_in"][0], "w_conv": inputs["w_conv"][0], "w_out": inputs["w_out"][0],
        "ln1_g": inputs["ln1_g"][0], "ln1_b": inputs["ln1_b"][0],
        "w_rg": inputs["w_router_g"][0], "b_rg": inputs["b_router_g"][0],
        "w_re": inputs["w_router_e"][0], "b_re": inputs["b_router_e"][0],
        "w_gate": inputs["w_gate"][0], "w_up": inputs["w_up"][0], "w_down": inputs["w_down"][0],
        "w_pg": inputs["w_ple_gate"][0], "w_pp": inputs["w_ple_proj"][0],
        "ln2_g": inputs["ln2_g"][0], "ln2_b": inputs["ln2_b"][0],
    }
    shared = {k: np.ascontiguousarray(np.asarray(v, dtype=np.float32)) for k, v in shared.items()}
    maps = []
    for b in range(nbatch):
        for r in range(4):
            js = own_blocks(r, NB)
            xo = np.zeros((len(js), 258, D), np.float32)
            po = np.zeros((len(js), 256, PLE), np.float32)
            nm1 = np.zeros((len(js), 8, 32), np.float32)
            nm2 = np.zeros((len(js), 8, 32), np.float32)
            for i, j in enumerate(js):
                lo = 256 * j - 2
                if lo >= 0:
                    xo[i] = x[b, lo:lo + 258]
                else:
                    xo[i, 2:] = x[b, 0:256]
                po[i] = p[0, b, 256 * j:256 * j + 256]
                nm1[i, :, j:] = NEGINF
                nm2[i, :, j:] = NEG
            m = dict(shared)
            m["xf"] = np.ascontiguousarray(x[b])
            m["xo"] = xo
            m["po"] = po
            m["nm1"] = nm1.reshape(-1)
            m["nm2"] = nm2.reshape(-1)
            maps.append(m)
    return maps


_NC_CACHE = {}


def kernel(**inputs):
    x = np.asarray(inputs["x"])
    nbatch, S, _ = x.shape
    NB = S // 256
    if S not in _NC_CACHE:
        _NC_CACHE[S] = build_nc(S)
    nc = _NC_CACHE[S]
    maps = make_in_maps(inputs, S)
    res = run_bass_kernel_spmd(nc, maps, core_ids=list(range(len(maps))))
    outp = np.zeros((nbatch, S, D), np.float32)
    k = 0
    for b in range(nbatch):
        for r in range(4):
            o = np.asarray(res.results[k]["out"], dtype=np.float32)
            for i, j in enumerate(own_blocks(r, NB)):
                outp[b, 256 * j:256 * j + 256] = o[256 * i:256 * i + 256]
            k += 1
    return outp
```

```python
import numpy as np
from contextlib import ExitStack

import concourse.bass as bass
import concourse.mybir as mybir
from concourse.bass_utils import run_bass_kernel_spmd

F32 = mybir.dt.float32
BF16 = mybir.dt.bfloat16
U8 = mybir.dt.uint8
I32 = mybir.dt.int32
AF = mybir.ActivationFunctionType
ALU = mybir.AluOpType
AX = mybir.AxisListType

D = 1024
DIN = 3072
NH = 8
HD = 64
NE = 32
DE = 512
PLE = 256
CAP = 256
ALPHA = float(2 ** 0.25)
EPS = 1e-5
NEG = -30000.0
NEGINF = -1.0e30


class Eng:
    def __init__(self, name, sem, same_wait):
        self.name = name
        self.sem = sem
        self.count = 0
        self.waited = {}
        self.ops = []
        self.same_wait = same_wait


class Buf:
    __slots__ = ("name", "w", "r", "excl")

    def __init__(self, name, excl=False):
        self.name = name
        self.w = None
        self.r = []
        self.excl = excl


class FW:
    def __init__(self, nc, stack):
        self.nc = nc
        self.stack = stack
        self.eng = {}
        for n, sw in (("pe", False), ("act", True), ("dve", True), ("pool", True), ("sp", False)):
            s = stack.enter_context(nc.semaphore("sem_" + n))
            self.eng[n] = Eng(n, s, sw)
        self.dsems = []
        self.names = {}
        self.bc_reg = None

    def dsem(self, name):
        s = [self.stack.enter_context(self.nc.semaphore(name)), 0]
        self.dsems.append(s)
        return s

    def _wait(self, e, s, v):
        if isinstance(s, list):
            s, v = s[0], s[1]
        if v <= 0:
            return
        k = id(s)
        if e.waited.get(k, 0) < v:
            e.waited[k] = v
            e.ops.append(lambda en, s=s, v=v: en.wait_ge(s, v))

    def _deps(self, e, reads, writes):
        for b in reads:
            if b.w is not None:
                s, v = b.w
                if not (s is e.sem and not e.same_wait):
                    self._wait(e, s, v)
            if b.excl:
                for (s, v) in b.r:
                    if not (s is e.sem and not e.same_wait):
                        self._wait(e, s, v)
        for b in writes:
            if b.w is not None:
                s, v = b.w
                if not (s is e.sem and not e.same_wait):
                    self._wait(e, s, v)
            for (s, v) in b.r:
                if not (s is e.sem and not e.same_wait):
                    self._wait(e, s, v)

    def _mark(self, tok, reads, writes):
        for b in reads:
            if b.excl:
                b.r = [tok]
            else:
                b.r.append(tok)
                if len(b.r) > 64:
                    b.r = b.r[-64:]
        for b in writes:
            b.w = tok
            b.r = []

    def op(self, engname, fn, reads=(), writes=()):
        e = self.eng[engname]
        self._deps(e, reads, writes)
        e.count += 1
        tok = (e.sem, e.count)
        import sys as _sys
        line = _sys._getframe(1).f_lineno

        def run(en, fn=fn, s=e.sem, line=line):
            ins = fn(en)
            try:
                self.names[ins.ins.name] = line
            except Exception:
                pass
            return ins.then_inc(s, 1)
        e.ops.append(run)
        self._mark(tok, reads, writes)
        return tok

    def dma(self, engname, fn, sem_state, reads=(), writes=()):
        e = self.eng[engname]
        saved = []
        for b in writes:
            if b.w is not None and b.w[0] is sem_state and not b.r:
                saved.append((b, b.w))
                b.w = None
        self._deps(e, reads, writes)
        for b, w in saved:
            b.w = w
        sem_state[1] += 16
        tok = (sem_state, None)
        e.ops.append(lambda en, fn=fn, s=sem_state[0]: fn(en).then_inc(s, 16))
        self._mark(tok, reads, writes)
        return tok

    def barrier(self):
        names = ["pe", "act", "dve", "pool", "sp"]
        snap = [(self.eng[n].sem, self.eng[n].count) for n in names]
        dsnap = [(s[0], s[1]) for s in self.dsems]
        for n in names:
            e = self.eng[n]
            for (s, v) in snap:
                if s is e.sem:
                    continue
                self._wait(e, s, v)
            for (s, v) in dsnap:
                self._wait(e, s, v)

    def replay(self):
        nc = self.nc
        with nc.Block() as block:
            @block.tensor
            def _(en):
                for f in self.eng["pe"].ops:
                    f(en)

            @block.scalar
            def _(en):
                for f in self.eng["act"].ops:
                    f(en)

            @block.vector
            def _(en):
                for f in self.eng["dve"].ops:
                    f(en)

            @block.gpsimd
            def _(en):
                for f in self.eng["pool"].ops:
                    f(en)

            @block.sync
            def _(en):
                for f in self.eng["sp"].ops:
                    f(en)


def build_nc(S=8192, dbg=False, stop_after=None):
    NB = S // 256
    NSLOT = NB // 4
    NOWN = NSLOT * 256
    TOT = S + NOWN
    NT = NOWN // 128
    NCH = S // 512
    NKT = TOT // 128
    NSL = NE * CAP

    nc = bass.Bass("TRN2", target_bir_lowering=False)

    def din(name, shape, dt=F32):
        return nc.dram_tensor(name, list(shape), dt, kind="ExternalInput").ap()

    xfT = din("xfT", [D, S])
    xoT = din("xoT", [NSLOT, D, 258])
    xo = din("xo", [NSLOT, 258, D])
    po = din("po", [NSLOT, 256, PLE])
    nm1_d = din("nm1", [NSLOT * 256])
    nm2_d = din("nm2", [NSLOT * 256])
    w_in = din("w_in", [D, DIN])
    w_conv = din("w_conv", [3, 512])
    w_out = din("w_out", [D, D])
    ln1_g = din("ln1_g", [D])
    ln1_b = din("ln1_b", [D])
    w_rg = din("w_rg", [D, 4])
    b_rg = din("b_rg", [4])
    w_re = din("w_re", [D, NE])
    b_re = din("b_re", [NE])
    w_gate = din("w_gate", [NE, D, DE])
    w_up = din("w_up", [NE, D, DE])
    w_down = din("w_down", [NE, DE, D])
    w_pg = din("w_pg", [D, D])
    w_pp = din("w_pp", [PLE, D])
    ln2_g = din("ln2_g", [D])
    ln2_b = din("ln2_b", [D])
    out = nc.dram_tensor("out", [NOWN, D], F32, kind="ExternalOutput").ap()
    if dbg:
        dbg_x1 = nc.dram_tensor("dbg_x1", [NOWN, D], F32, kind="ExternalOutput").ap()
        dbg_mix = nc.dram_tensor("dbg_mix", [NOWN, D], F32, kind="ExternalOutput").ap()

    KT = nc.dram_tensor("KT_scr", [NH * HD, TOT], BF16).ap()
    VV = nc.dram_tensor("VV_scr", [NH, 128, NKT, 65], BF16).ap()
    XS = nc.dram_tensor("XS_scr", [NSL, D], BF16).ap()
    YS = nc.dram_tensor("YS_scr", [NSL, D], F32).ap()
    ACCD = nc.dram_tensor("ACC_scr", [NOWN, D], F32).ap()

    st = ExitStack()
    with st:
        fw = FW(nc, st)
        ARENA = 98304 + 59392 + 20480 + 8192
        arena = st.enter_context(nc.sbuf_tensor("arena", [128, ARENA], U8))
        R0, R1, R2, RC = 0, 98304, 98304 + 59392, 98304 + 59392 + 20480

        class Alloc:
            def __init__(self, base, size):
                self.base, self.size, self.off = base, size, 0

            def __call__(self, nelem, dtype, pat=None, **kw):
                esz = 4 if dtype in (F32, I32) else 2
                nbytes = nelem * esz
                o = self.base + self.off
                self.off += (nbytes + 63) // 64 * 64
                assert self.off <= self.size, ("arena overflow", self.base, self.off, self.size)
                a = arena[:, o:o + nbytes].bitcast(dtype)
                if pat:
                    a = a.rearrange(pat, **kw)
                return a

        psall = st.enter_context(nc.psum_tensor("psall", [128, 4096], F32))
        psf = [psall[:, i * 512:(i + 1) * 512] for i in range(6)]
        psb = [psall[:, (6 + i) * 512:(7 + i) * 512].bitcast(BF16) for i in range(2)]
        psw = [psall[:, j * 1024:(j + 1) * 1024] for j in range(2)]
        pse = psall[:, 6 * 512:7 * 512]
        PW = [Buf("psw%d" % j, excl=True) for j in range(2)]
        PF = [Buf("psf%d" % i, excl=True) for i in range(6)]
        PB = [Buf("psb%d" % i, excl=True) for i in range(2)]

        def alt(i):
            return "act" if i % 2 == 0 else "dve"


        def MM(o, l, r, st_, sp_):
            return lambda e: e.matmul(o, lhsT=l, rhs=r, start=st_, stop=sp_)

        def TR(o, i_, idn):
            return lambda e: e.transpose(out=o, in_=i_, identity=idn)

        def ACTF(o, i_, f, **kw):
            return lambda e: e.activation(out=o, in_=i_, func=f, **kw)

        def TT(o, a, b, op):
            return lambda e: e.tensor_tensor(out=o, in0=a, in1=b, op=op)

        def TS(o, a, s1, s2, op0, op1=None):
            if op1 is None:
                return lambda e: e.tensor_scalar(out=o, in0=a, scalar1=s1, scalar2=None, op0=op0)
            return lambda e: e.tensor_scalar(out=o, in0=a, scalar1=s1, scalar2=s2, op0=op0, op1=op1)

        def STT(o, a, sc, b, op0, op1):
            return lambda e: e.scalar_tensor_tensor(out=o, in0=a, scalar=sc, in1=b, op0=op0, op1=op1)

        def MS(ap, v):
            return lambda e: e.memset(ap, v)

        def RD(o, i_, op):
            return lambda e: e.tensor_reduce(out=o, in_=i_, axis=AX.X, op=op)

        def MX(o, i_):
            return lambda e: e.max(out=o, in_=i_)

        def RCP(o, i_):
            return lambda e: e.reciprocal(out=o, in_=i_)

        def DM(o, i_):
            return lambda e: e.dma_start(out=o, in_=i_)

        def CPY(o, i_):
            return lambda e: e.tensor_copy(out=o, in_=i_)

        def ASEL(o, i_, pattern, cmp, fill, base, cm):
            return lambda e: e.affine_select(out=o, in_=i_, pattern=pattern, compare_op=cmp, fill=fill, base=base, channel_multiplier=cm)

        def BNS(o, i_):
            return lambda e: e.bn_stats(out=o, in_=i_)

        def BNA(o, i_):
            return lambda e: e.bn_aggr(out=o, in_=i_)

        def SCAT(dst, idx, src, bc):
            return lambda e: e.indirect_dma_start(out=dst, out_offset=bass.IndirectOffsetOnAxis(ap=idx, axis=0), in_=src, in_offset=None, bounds_check=bc, oob_is_err=False)

        def GATH(dst, src, idx, bc):
            return lambda e: e.indirect_dma_start(out=dst, out_offset=None, in_=src, in_offset=bass.IndirectOffsetOnAxis(ap=idx, axis=0), bounds_check=bc, oob_is_err=False)


        def _L_matmul(o, lhsT=None, rhs=None, start=None, stop=None):
            return lambda e: e.matmul(o, lhsT=lhsT, rhs=rhs, start=start, stop=stop)

        def _mk(meth):
            def f(*a, **kw):
                return lambda e: getattr(e, meth)(*a, **kw)
            return f

        _L_transpose = _mk("transpose")
        _L_activation = _mk("activation")
        _L_tensor_tensor = _mk("tensor_tensor")
        _L_tensor_scalar = _mk("tensor_scalar")
        _L_scalar_tensor_tensor = _mk("scalar_tensor_tensor")
        _L_memset = _mk("memset")
        _L_tensor_reduce = _mk("tensor_reduce")
        _L_max = _mk("max")
        _L_reciprocal = _mk("reciprocal")
        _L_dma_start = _mk("dma_start")
        _L_tensor_copy = _mk("tensor_copy")
        _L_affine_select = _mk("affine_select")
        _L_bn_stats = _mk("bn_stats")
        _L_bn_aggr = _mk("bn_aggr")
        def _L_indirect_dma_start(*a, **kw):
            def f(e):
                if fw.bc_reg is None:
                    fw.bc_reg = e.to_reg(kw["bounds_check"])
                kw2 = dict(kw)
                kw2["bounds_check"] = fw.bc_reg
                return e.indirect_dma_start(*a, **kw2)
            return f
        _L_iota = _mk("iota")

        def copy_op(eng, out_ap, in_ap):
            if eng == "act":
                return lambda e: e.activation(out=out_ap, in_=in_ap, func=AF.Copy)
            return lambda e: e.tensor_copy(out=out_ap, in_=in_ap)

        ac = Alloc(RC, 8192)
        ident_f = ac(128, F32)
        ident_b = ac(128, BF16)
        ones_f = ac(128, F32)
        CBm = ac(512, BF16, "p (t q) -> p t q", t=2)
        wc = ac(12, F32, "p (c k) -> p c k", c=4)
        km = ac(4 * 32, F32, "p (c n) -> p c n", c=4)
        kmb = ac(4 * 32, BF16, "p (c n) -> p c n", c=4)
        kmh = ac(8 * 32, BF16, "p (h n) -> p h n", h=8)
        slot_i = ac(NT * 2, I32, "p (t k) -> p t k", k=2)
        w12 = ac(NT * 2, F32, "p (t k) -> p t k", k=2)
        Lst = ac(128, F32)
        eC = ac(32, F32)
        Spre = ac(32, F32)
        bias36 = ac(36, F32)
        ztile = ac(1024, BF16)
        B_ident = Buf("ident")
        B_const = Buf("const")
        B_km = Buf("km")
        B_kmh = Buf("kmh")
        B_slot = [Buf("slot%d" % t) for t in range(NT)]
        B_w12 = [Buf("w12_%d" % t) for t in range(NT)]
        B_Spre = Buf("Spre")

        s_setup = fw.dsem("setup")

        fw.op("pool", _L_memset(ident_f, 0.0), writes=[B_ident])
        fw.op("pool", _L_affine_select(out=ident_f, in_=ident_f, pattern=[[-1, 128]], compare_op=ALU.not_equal, fill=1.0, base=0, channel_multiplier=1), reads=[B_ident], writes=[B_ident])
        fw.op("pool", _L_tensor_copy(out=ident_b, in_=ident_f), reads=[B_ident], writes=[B_const])
        fw.op("pool", _L_memset(ones_f, 1.0), writes=[B_const])
        fw.op("pool", _L_memset(CBm, 0.0), writes=[B_const])
        fw.op("pool", _L_affine_select(out=CBm, in_=CBm, pattern=[[-128, 2], [1, 256]], compare_op=ALU.is_ge, fill=NEG, base=0, channel_multiplier=-1), reads=[B_const], writes=[B_const])
        fw.op("pool", _L_memset(Lst, 1.0), writes=[B_const])
        fw.op("pool", _L_affine_select(out=Lst, in_=Lst, pattern=[[1, 128]], compare_op=ALU.is_ge, fill=0.0, base=-1, channel_multiplier=-1), reads=[B_const], writes=[B_const])
        fw.op("pool", _L_iota(eC, pattern=[[CAP, 32]], base=0, channel_multiplier=0, allow_small_or_imprecise_dtypes=True), writes=[B_const])
        fw.op("pool", _L_memset(Spre, 0.0), writes=[B_Spre])
        for c_ in range(4):
            for k_ in range(3):
                fw.dma("sp", _L_dma_start(out=wc[:, c_, k_:k_ + 1], in_=w_conv[k_, c_ * 128:(c_ + 1) * 128].rearrange("(p o) -> p o", o=1)), s_setup, writes=[B_const])
        fw.dma("sp", _L_dma_start(out=bias36[:, 0:4], in_=b_rg.partition_broadcast(128)), s_setup, writes=[B_const])
        fw.dma("sp", _L_dma_start(out=bias36[:, 4:36], in_=b_re.partition_broadcast(128)), s_setup, writes=[B_const])

        a0 = Alloc(R0, 98304)
        w_in_bf = a0(8 * DIN, BF16, "p (c n) -> p c n", c=8)
        Qaug = Alloc(R0 + 49152, 32768)(8 * NOWN, BF16, "p (h n) -> p h n", h=8)
        yconvT = Alloc(R0 + 81920, 16384)(4 * NOWN, BF16, "p (c n) -> p c n", c=4)
        B_win = [Buf("win%d" % c) for c in range(8)]
        B_Q = [Buf("Q%d" % i) for i in range(NSLOT)]
        B_yc = [Buf("yc%d" % i) for i in range(NSLOT)]

        a1 = Alloc(R1, 59392)
        wst = [a1(DIN, F32) for _ in range(2)]
        B_wst = [Buf("wst%d" % i) for i in range(2)]
        s_wst = [fw.dsem("wst%d" % i) for i in range(2)]
        B_z = Buf("ztile")
        s_z = fw.dsem("zfill")
        B_XS = Buf("XS")

        fw.op("pool", _L_memset(ztile, 0.0), writes=[B_z])
        XSv = XS.rearrange("(t p) d -> t p d", p=128)
        for c in range(8):
            sl = c % 2
            fw.dma("sp", _L_dma_start(out=wst[sl], in_=w_in[c * 128:(c + 1) * 128, :]), s_wst[sl], writes=[B_wst[sl]])
            for k, eng in enumerate(("pool", "act", "dve")):
                o, i_ = w_in_bf[:, c, k * 1024:(k + 1) * 1024], wst[sl][:, k * 1024:(k + 1) * 1024]
                fw.op(eng, copy_op(eng, o, i_), reads=[B_wst[sl]], writes=[B_win[c]])
        fw.barrier()

        a1 = Alloc(R1, 59392)
        xs = [a1(8 * 512, F32, "p (c n) -> p c n", c=8) for _ in range(2)]
        xT = [a1(8 * 512, BF16, "p (c n) -> p c n", c=8) for _ in range(2)]
        KTst = [a1(4 * 512, BF16, "p (c n) -> p c n", c=4) for _ in range(2)]
        a2 = Alloc(R2, 20480)
        Vst = [a2(8 * 4 * 65, BF16, "p (h k e) -> p h k e", h=8, k=4) for _ in range(2)]
        B_xs = [Buf("xs%d" % i) for i in range(2)]
        B_xT = [[Buf("xT%d_%d" % (i, g)) for g in range(3)] for i in range(2)]
        XG = ((("act", 0, 3), ("dve", 3, 6), ("pool", 6, 8)))

        def xg(c):
            return min(c // 3, 2)
        B_KTst = [Buf("KTst%d" % i) for i in range(2)]
        B_Vst = [Buf("Vst%d" % i) for i in range(2)]
        s_xs = [fw.dsem("xs%d" % i) for i in range(2)]
        s_kst = [fw.dsem("kst%d" % i) for i in range(2)]
        s_vst = [fw.dsem("vst%d" % i) for i in range(2)]
        B_KT = Buf("KT")
        B_VV = Buf("VV")
        for i in range(2):
            fw.op("pool", _L_memset(Vst[i], 1.0), writes=[B_Vst[i]])
        fw.op("pool", _L_memset(km, 0.0), writes=[B_km])
        fw.op("pool", _L_memset(Qaug[64:128], 0.0), writes=B_Q)
        ZF_PER = (NSL // 128 + NCH - 1) // NCH

        KTv = KT.rearrange("(c p) n -> p c n", p=128)
        VVv = VV.rearrange("h p k e -> p h k e")
        xfTv = xfT.rearrange("(c p) n -> p c n", p=128)

        def load_chunk(t):
            sl = t % 2
            fw.dma("sp", _L_dma_start(out=xs[sl], in_=xfTv[:, :, t * 512:(t + 1) * 512]), s_xs[sl], writes=[B_xs[sl]])

        ev = [0]
        load_chunk(0)
        for t in range(NCH):
            sl = t % 2
            if t + 1 < NCH:
                load_chunk(t + 1)
            for zt in range(t * ZF_PER, min((t + 1) * ZF_PER, NSL // 128)):
                fw.dma("act", _L_dma_start(out=XSv[zt], in_=ztile), s_z, reads=[B_z], writes=[B_XS])
            for g, (eng, c0, c1) in enumerate(XG):
                fw.op(eng, copy_op(eng, xT[sl][:, c0:c1, :], xs[sl][:, c0:c1, :]), reads=[B_xs[sl]], writes=[B_xT[sl][g]])
            for pr in range(4):
                bk = 2 + pr % 2
                for c in range(8):
                    fw.op("pe", _L_matmul(psf[bk][:, :], lhsT=w_in_bf[:, c, 2048 + pr * 128:2048 + (pr + 1) * 128], rhs=xT[sl][:, c, :], start=(c == 0), stop=(c == 7)), reads=[B_xT[sl][xg(c)], B_win[c]], writes=[PF[bk]])
                fw.op("act", copy_op("act", KTst[sl][:, pr, :], psf[bk][:, :]), reads=[PF[bk]], writes=[B_KTst[sl]])
                fw.op("dve", _L_tensor_reduce(out=km[:, pr, 2 * t:2 * t + 2], in_=psf[bk][:, :].rearrange("p (a b) -> p a b", a=2), axis=AX.X, op=ALU.add), reads=[PF[bk]], writes=[B_km])
            fw.dma("sp", _L_dma_start(out=KTv[:, :, t * 512:(t + 1) * 512], in_=KTst[sl]), s_kst[sl], reads=[B_KTst[sl]], writes=[])
            for q in range(4):
                bk = 4 + q % 2
                for c in range(8):
                    fw.op("pe", _L_matmul(psf[bk][:, :], lhsT=xT[sl][:, c, q * 128:(q + 1) * 128], rhs=w_in_bf[:, c, 2560:3072], start=(c == 0), stop=(c == 7)), reads=[B_xT[sl][xg(c)], B_win[c]], writes=[PF[bk]])
                eng = alt(ev[0]); ev[0] += 1
                fw.op(eng, copy_op(eng, Vst[sl][:, :, q, 0:64], psf[bk][:, :].rearrange("p (h e) -> p h e", h=8)), reads=[PF[bk]], writes=[B_Vst[sl]])
            fw.dma("sp", _L_dma_start(out=VVv[:, :, 4 * t:4 * t + 4, :], in_=Vst[sl]), s_vst[sl], reads=[B_Vst[sl]], writes=[])
        fw.barrier()

        a1 = Alloc(R1, 59392)
        xos = [a1(8 * 258, F32, "p (c n) -> p c n", c=8) for _ in range(2)]
        xTo = [a1(8 * 258, BF16, "p (c n) -> p c n", c=8) for _ in range(2)]
        KTst2 = [a1(4 * 256, BF16, "p (c n) -> p c n", c=4) for _ in range(2)]
        Vst2 = [a1(8 * 2 * 65, BF16, "p (h k e) -> p h k e", h=8, k=2) for _ in range(2)]
        ccs = [a1(258, F32) for _ in range(2)]
        uu = [a1(258, F32) for _ in range(2)]
        tt_ = [a1(256, F32) for _ in range(2)]
        B_xos = [Buf("xos%d" % i) for i in range(2)]
        B_xTo = [[Buf("xTo%d_%d" % (i, g)) for g in range(3)] for i in range(2)]
        B_K2 = [Buf("K2_%d" % i) for i in range(2)]
        B_V2 = [Buf("V2_%d" % i) for i in range(2)]
        B_cc = [Buf("cc%d" % i) for i in range(2)]
        B_uu = [Buf("uu%d" % i) for i in range(2)]
        B_tt = [Buf("tt%d" % i) for i in range(2)]
        s_xos = [fw.dsem("xos%d" % i) for i in range(2)]
        s_k2 = [fw.dsem("k2_%d" % i) for i in range(2)]
        s_v2 = [fw.dsem("v2_%d" % i) for i in range(2)]
        for i in range(2):
            fw.op("pool", _L_memset(Vst2[i], 1.0), writes=[B_V2[i]])

        def load_own(i):
            sl = i % 2
            fw.dma("sp", _L_dma_start(out=xos[sl], in_=xoT[i].rearrange("(c p) n -> p c n", p=128)), s_xos[sl], writes=[B_xos[sl]])

        load_own(0)
        cvi = [0]
        for i in range(NSLOT):
            sl = i % 2
            if i + 1 < NSLOT:
                load_own(i + 1)
            c0 = i * 256
            for g, (eng, c0_, c1_) in enumerate(XG):
                fw.op(eng, copy_op(eng, xTo[sl][:, c0_:c1_, :], xos[sl][:, c0_:c1_, :]), reads=[B_xos[sl]], writes=[B_xTo[sl][g]])
            for hp in range(4):
                bk = 2 + hp % 2
                for hh in range(2):
                    h = 2 * hp + hh
                    for c in range(8):
                        fw.op("pe", _L_matmul(psf[bk][0:64, hh * 256:(hh + 1) * 256], lhsT=w_in_bf[:, c, 1536 + h * 64:1536 + (h + 1) * 64], rhs=xTo[sl][:, c, 2:258], start=(c == 0), stop=(c == 7)), reads=[B_xTo[sl][xg(c)], B_win[c]], writes=[PF[bk]])
                eng = alt(ev[0]); ev[0] += 1
                fw.op(eng, copy_op(eng, Qaug[0:64, 2 * hp:2 * hp + 2, c0:c0 + 256], psf[bk][0:64, :].rearrange("p (a b) -> p a b", a=2)), reads=[PF[bk]], writes=[B_Q[i]])
            for pp in range(2):
                bk = 4 + pp % 2
                for q in range(2):
                    pr = 2 * pp + q
                    for c in range(8):
                        fw.op("pe", _L_matmul(psf[bk][:, q * 256:(q + 1) * 256], lhsT=w_in_bf[:, c, 2048 + pr * 128:2048 + (pr + 1) * 128], rhs=xTo[sl][:, c, 2:258], start=(c == 0), stop=(c == 7)), reads=[B_xTo[sl][xg(c)], B_win[c]], writes=[PF[bk]])
                eng = alt(ev[0]); ev[0] += 1
                fw.op(eng, copy_op(eng, KTst2[sl][:, 2 * pp:2 * pp + 2, :], psf[bk][:, :].rearrange("p (a b) -> p a b", a=2)), reads=[PF[bk]], writes=[B_K2[sl]])
            fw.dma("sp", _L_dma_start(out=KTv[:, :, S + c0:S + c0 + 256], in_=KTst2[sl]), s_k2[sl], reads=[B_K2[sl]], writes=[])
            for q in range(2):
                bk = 2 + q % 2
                for c in range(8):
                    fw.op("pe", _L_matmul(psf[bk][:, :], lhsT=xTo[sl][:, c, 2 + q * 128:2 + (q + 1) * 128], rhs=w_in_bf[:, c, 2560:3072], start=(c == 0), stop=(c == 7)), reads=[B_xTo[sl][xg(c)], B_win[c]], writes=[PF[bk]])
                eng = alt(ev[0]); ev[0] += 1
                fw.op(eng, copy_op(eng, Vst2[sl][:, :, q, 0:64], psf[bk][:, :].rearrange("p (h e) -> p h e", h=8)), reads=[PF[bk]], writes=[B_V2[sl]])
            fw.dma("sp", _L_dma_start(out=VVv[:, :, S // 128 + 2 * i:S // 128 + 2 * i + 2, :], in_=Vst2[sl]), s_v2[sl], reads=[B_V2[sl]], writes=[])
            for cc in range(4):
                k2 = cvi[0] % 2
                cvi[0] += 1
                banks = (0, 1, 4) if cc % 2 == 0 else (5, 2, 3)
                specs = ((banks[0], 0 + cc * 128, 2, 256), (banks[1], 512 + cc * 128, 0, 258), (banks[2], 1024 + cc * 128, 0, 258))
                for (bk, col, o0, n) in specs:
                    for c in range(8):
                        fw.op("pe", _L_matmul(psf[bk][:, 0:n], lhsT=w_in_bf[:, c, col:col + 128], rhs=xTo[sl][:, c, o0:o0 + n], start=(c == 0), stop=(c == 7)), reads=[B_xTo[sl][xg(c)], B_win[c]], writes=[PF[bk]])
                bcb, bcc, bch = banks
                fw.op("act", copy_op("act", ccs[k2], psf[bcc][:, 0:258]), reads=[PF[bcc]], writes=[B_cc[k2]])
                fw.op("dve", _L_tensor_tensor(out=uu[k2], in0=ccs[k2], in1=psf[bch][:, 0:258], op=ALU.mult), reads=[B_cc[k2], PF[bch]], writes=[B_uu[k2]])
                fw.op("dve", _L_tensor_scalar(out=tt_[k2], in0=uu[k2][:, 2:258], scalar1=wc[:, cc, 2:3], scalar2=None, op0=ALU.mult), reads=[B_uu[k2], B_const], writes=[B_tt[k2]])
                fw.op("dve", _L_scalar_tensor_tensor(out=tt_[k2], in0=uu[k2][:, 1:257], scalar=wc[:, cc, 1:2], in1=tt_[k2], op0=ALU.mult, op1=ALU.add), reads=[B_uu[k2], B_const, B_tt[k2]], writes=[B_tt[k2]])
                fw.op("dve", _L_scalar_tensor_tensor(out=tt_[k2], in0=uu[k2][:, 0:256], scalar=wc[:, cc, 0:1], in1=tt_[k2], op0=ALU.mult, op1=ALU.add), reads=[B_uu[k2], B_const, B_tt[k2]], writes=[B_tt[k2]])
                fw.op("dve", _L_tensor_tensor(out=yconvT[:, cc, c0:c0 + 256], in0=tt_[k2], in1=psf[bcb][:, 0:256], op=ALU.mult), reads=[B_tt[k2], PF[bcb]], writes=[B_yc[i]])
        fw.barrier()

        a2 = Alloc(R2, 20480)
        nm1 = a2(NSLOT * 256, F32, "p (i n) -> p i n", i=NSLOT)
        nm2 = a2(NSLOT * 256, F32, "p (i n) -> p i n", i=NSLOT)
        gm = [a2(256, F32, "p (h n) -> p h n", h=8) for _ in range(2)]
        m8 = [a2(64, F32, "p (h n) -> p h n", h=8) for _ in range(2)]
        a1 = Alloc(R1, 59392)
        bpad = [a1(8 * 128, BF16, "p (h n) -> p h n", h=8) for _ in range(2)]
        tA3 = [a1(256, F32, "p (h n) -> p h n", h=8) for _ in range(2)]
        tB3 = [a1(256, F32, "p (h n) -> p h n", h=8) for _ in range(2)]
        mm3 = [a1(24, F32, "p (r h) -> p r h", r=3) for _ in range(2)]
        B_nm = Buf("nm")
        B_gm = [Buf("gm%d" % i) for i in range(2)]
        B_m8 = [Buf("m8%d" % i) for i in range(2)]
        B_bp = [Buf("bp%d" % i) for i in range(2)]
        s_nm = fw.dsem("nm")
        s_kmh = fw.dsem("kmh")
        fw.dma("sp", _L_dma_start(out=nm1.rearrange("p i n -> p (i n)"), in_=nm1_d.partition_broadcast(128)), s_nm, writes=[B_nm])
        fw.dma("sp", _L_dma_start(out=nm2.rearrange("p i n -> p (i n)"), in_=nm2_d.partition_broadcast(128)), s_nm, writes=[B_nm])
        for i in range(2):
            fw.op("pool", _L_memset(bpad[i], 0.0), writes=[B_bp[i]])
        fw.op("dve", _L_tensor_copy(out=kmb, in_=km), reads=[B_km], writes=[B_km])
        kmh_v = kmh.rearrange("p (a b) n -> p a b n", b=2)
        fw.dma("sp", _L_dma_start(out=kmh_v[0:64, :, 0, :], in_=kmb[0:64, :, :]), s_kmh, reads=[B_km], writes=[B_kmh])
        fw.dma("sp", _L_dma_start(out=kmh_v[0:64, :, 1, :], in_=kmb[64:128, :, :]), s_kmh, reads=[B_km], writes=[B_kmh])
        for i in range(NSLOT):
            for t in range(2):
                k2 = (2 * i + t) % 2
                q0 = i * 256 + t * 128
                gb = k2
                for h in range(8):
                    fw.op("pe", _L_matmul(psf[gb][:, h * 32:(h + 1) * 32], lhsT=Qaug[0:64, h, q0:q0 + 128], rhs=kmh[0:64, h, :], start=True, stop=True), reads=[B_Q[i], B_kmh], writes=[PF[gb]])
                fw.op("dve", _L_tensor_tensor(out=gm[k2].rearrange("p h n -> p (h n)"), in0=psf[gb][:, 0:256], in1=nm1[:, i, :], op=ALU.add), reads=[PF[gb], B_nm], writes=[B_gm[k2]])
                G, A_, B_ = gm[k2], tA3[k2], tB3[k2]
                rd = [B_gm[k2], B_m8[k2]]
                wr_ = [B_m8[k2]]

                def bc(r):
                    return mm3[k2][:, r, :].unsqueeze(2).to_broadcast([128, 8, 32])
                fw.op("dve", _L_tensor_reduce(out=mm3[k2][:, 0, :], in_=G, axis=AX.X, op=ALU.max), reads=rd, writes=wr_)
                fw.op("dve", _L_tensor_tensor(out=A_, in0=G, in1=bc(0), op=ALU.is_ge), reads=rd, writes=wr_)
                fw.op("dve", _L_scalar_tensor_tensor(out=B_, in0=A_, scalar=-1.0e9, in1=G, op0=ALU.mult, op1=ALU.add), reads=rd, writes=wr_)
                fw.op("dve", _L_tensor_reduce(out=mm3[k2][:, 1, :], in_=B_, axis=AX.X, op=ALU.max), reads=rd, writes=wr_)
                fw.op("dve", _L_tensor_tensor(out=A_, in0=B_, in1=bc(1), op=ALU.is_ge), reads=rd, writes=wr_)
                fw.op("dve", _L_scalar_tensor_tensor(out=B_, in0=A_, scalar=-1.0e9, in1=B_, op0=ALU.mult, op1=ALU.add), reads=rd, writes=wr_)
                fw.op("dve", _L_tensor_reduce(out=mm3[k2][:, 2, :], in_=B_, axis=AX.X, op=ALU.max), reads=rd, writes=wr_)
                fw.op("dve", _L_tensor_tensor(out=A_, in0=G, in1=bc(2), op=ALU.is_lt), reads=rd, writes=wr_)
                fw.op("dve", _L_scalar_tensor_tensor(out=bpad[k2][:, :, 64:96], in0=A_, scalar=NEG, in1=nm2[:, i, :].rearrange("p (h n) -> p h n", h=8), op0=ALU.mult, op1=ALU.add), reads=rd + [B_nm], writes=[B_bp[k2]])
                pb = k2
                for h in range(8):
                    fw.op("pe", _L_transpose(out=psb[pb][:, h * 128:(h + 1) * 128], in_=bpad[k2][:, h, :], identity=ident_b), reads=[B_bp[k2], B_const], writes=[PB[pb]])
                fw.op("act", copy_op("act", Qaug[64:96, :, q0:q0 + 128], psb[pb][64:96, :].rearrange("p (h n) -> p h n", h=8)), reads=[PB[pb]], writes=[B_Q[i]])
        fw.barrier()

        if stop_after == "A":
            pass

        aB0 = Alloc(R0, 49152)
        KTaug = [aB0(TOT, BF16) for _ in range(2)]
        aB1 = Alloc(R1, 59392)
        yattnT = aB1(8 * NOWN, BF16, "p (h n) -> p h n", h=8)
        aB1.off = 32768
        Vh = [aB1(NKT * 65, BF16, "p (k e) -> p k e", e=65) for _ in range(2)]
        Pt = [aB1(1024, BF16) for _ in range(2)]
        aB2 = Alloc(R2, 20480)
        rcp = [aB2(256, F32) for _ in range(2)]
        bcs = [aB2(256, F32) for _ in range(2)]
        B_KTa = [Buf("KTa%d" % i) for i in range(2)]
        B_Vh = [Buf("Vh%d" % i) for i in range(2)]
        B_Pt = [Buf("Pt%d" % i) for i in range(2)]
        B_rcp = [Buf("rcp%d" % i) for i in range(2)]
        B_bcs = [Buf("bcs%d" % i) for i in range(2)]
        B_ya = [[Buf("ya%d_%d" % (h, i)) for i in range(NSLOT)] for h in range(8)]
        s_kta = [fw.dsem("kta%d" % i) for i in range(2)]
        s_vh = [fw.dsem("vh%d" % i) for i in range(2)]
        for s_ in range(2):
            fw.op("pool", _L_memset(KTaug[s_][64:128, :], 0.0), writes=[B_KTa[s_]])
            fw.op("pool", _L_memset(KTaug[s_][64:96, 0:S], 1.0), writes=[B_KTa[s_]])
            fw.op("pool", _L_affine_select(out=KTaug[s_][64:96, 0:S], in_=KTaug[s_][64:96, 0:S], pattern=[[1, S]], compare_op=ALU.is_ge, fill=0.0, base=0, channel_multiplier=-256), reads=[B_KTa[s_]], writes=[B_KTa[s_]])
            fw.op("pool", _L_affine_select(out=KTaug[s_][64:96, 0:S], in_=KTaug[s_][64:96, 0:S], pattern=[[-1, S]], compare_op=ALU.is_ge, fill=0.0, base=255, channel_multiplier=256), reads=[B_KTa[s_]], writes=[B_KTa[s_]])
        KTh = KT.rearrange("(h d) n -> h d n", d=64)

        def load_head(h):
            s_ = h % 2
            fw.dma("sp", _L_dma_start(out=KTaug[s_][0:64, :], in_=KTh[h]), s_kta[s_], reads=[B_KT], writes=[B_KTa[s_]])
            fw.dma("sp", _L_dma_start(out=Vh[s_], in_=VV[h]), s_vh[s_], reads=[B_VV], writes=[B_Vh[s_]])

        load_head(0)
        epi_pending = []
        sb_i = [0]
        ob_i = [0]
        pt_i = [0]
        for h in range(8):
            s_ = h % 2
            if h + 1 < 8:
                load_head(h + 1)
            for i in range(NSLOT):
                q0 = i * 256
                units = [256 * n for n in range(4 * i + 3)] + [S + q0]
                nu = len(units)
                ob = 4 + ob_i[0] % 2
                ob_i[0] += 1
                ndu = nu // 2

                def qk2(du):
                    j = sb_i[0] % 2
                    sb_i[0] += 1
                    for k in range(2):
                        u = 2 * du + k
                        kc = units[u]
                        own = (u == nu - 1)
                        for t in range(2):
                            o_ap = psw[j][:, k * 512 + t * 256:k * 512 + (t + 1) * 256]
                            fw.op("pe", _L_matmul(o_ap, lhsT=KTaug[s_][:, kc + t * 128:kc + (t + 1) * 128], rhs=Qaug[:, h, q0:q0 + 256], start=True, stop=(not own)), reads=[B_KTa[s_], B_Q[i]], writes=[PW[j]])
                            if own:
                                fw.op("pe", _L_matmul(o_ap, lhsT=ident_b, rhs=CBm[:, t, :], start=False, stop=True), reads=[B_const], writes=[PW[j]])
                    return j

                def expo2(j):
                    pi = pt_i[0] % 2
                    pt_i[0] += 1
                    fw.op("act", _L_activation(out=Pt[pi], in_=psw[j], func=AF.Exp, scale=0.125), reads=[PW[j]], writes=[B_Pt[pi]])
                    return pi

                def pv2(du, pi):
                    for k in range(2):
                        u = 2 * du + k
                        kc = units[u]
                        for t in range(2):
                            kt = kc // 128 + t
                            fw.op("pe", _L_matmul(psf[ob][0:65, 0:256], lhsT=Vh[s_][:, kt, :], rhs=Pt[pi][:, k * 512 + t * 256:k * 512 + (t + 1) * 256], start=(u == 0 and t == 0), stop=(u == nu - 1 and t == 1)), reads=[B_Vh[s_], B_Pt[pi]], writes=[PF[ob]])

                js = [qk2(0)]
                for du in range(ndu):
                    if du + 1 < ndu:
                        js.append(qk2(du + 1))
                    pi = expo2(js[du])
                    pv2(du, pi)
                    if du == 0 and epi_pending:
                        epi_pending.pop()()
                k2 = ob_i[0] % 2
                fw.op("dve", _L_reciprocal(out=rcp[k2][64:65, :], in_=psf[ob][64:65, 0:256]), reads=[PF[ob]], writes=[B_rcp[k2]])

                def epilogue(k2=k2, ob=ob, h=h, i=i, q0=q0):
                    fw.op("pe", _L_matmul(pse[0:64, 0:256], lhsT=ones_f[64:65, 0:64], rhs=rcp[k2][64:65, :], start=True, stop=True), reads=[B_rcp[k2], B_const], writes=[PB[0]])
                    fw.op("dve", copy_op("dve", bcs[k2][0:64, :], pse[0:64, 0:256]), reads=[PB[0]], writes=[B_bcs[k2]])
                    fw.op("dve", _L_tensor_tensor(out=yattnT[0:64, h, q0:q0 + 256], in0=psf[ob][0:64, 0:256], in1=bcs[k2][0:64, :], op=ALU.mult), reads=[PF[ob], B_bcs[k2]], writes=[B_ya[h][i]])
                epi_pending.append(epilogue)
        while epi_pending:
            epi_pending.pop()()
        fw.barrier()

        aC = Alloc(R0, 81920)
        wo_c = aC(4 * D, BF16, "p (c n) -> p c n", c=4)
        wo_a = aC(8 * D, BF16, "p (c n) -> p c n", c=8)
        wpg = aC(8 * D, BF16, "p (c n) -> p c n", c=8)
        wpp = aC(2 * D, BF16, "p (c n) -> p c n", c=2)
        wr = aC(8 * 36, F32, "p (c n) -> p c n", c=8)
        lng1 = aC(D, F32)
        lnb1 = aC(D, F32)
        cst = [aC(D, F32) for _ in range(4)]
        B_wC = Buf("wC")
        B_cst = [Buf("cst%d" % i) for i in range(4)]
        s_cst = [fw.dsem("cst%d" % i) for i in range(4)]
        s_wC = fw.dsem("wCs")
        fw.dma("sp", _L_dma_start(out=lng1, in_=ln1_g.partition_broadcast(128)), s_wC, writes=[B_wC])
        fw.dma("sp", _L_dma_start(out=lnb1, in_=ln1_b.partition_broadcast(128)), s_wC, writes=[B_wC])
        with nc.allow_non_contiguous_dma(reason="small router weights"):
            fw.dma("sp", _L_dma_start(out=wr[:, :, 0:4], in_=w_rg.rearrange("(c p) n -> p c n", p=128)), s_wC, writes=[B_wC])
            fw.dma("sp", _L_dma_start(out=wr[:, :, 4:36], in_=w_re.rearrange("(c p) n -> p c n", p=128)), s_wC, writes=[B_wC])
        pieces = []
        for c in range(4):
            pieces.append((w_out[c * 128:(c + 1) * 128, :], wo_c[:, c, :], 128))
        for hh in range(8):
            pieces.append((w_out[512 + hh * 64:512 + (hh + 1) * 64, :], wo_a[0:64, hh, :], 64))
        for c in range(8):
            pieces.append((w_pg[c * 128:(c + 1) * 128, :], wpg[:, c, :], 128))
        for c in range(2):
            pieces.append((w_pp[c * 128:(c + 1) * 128, :], wpp[:, c, :], 128))
        for k, (src, dst, npart) in enumerate(pieces):
            sl = k % 4
            fw.dma("sp", _L_dma_start(out=cst[sl][0:npart, :], in_=src), s_cst[sl], writes=[B_cst[sl]])
            eng = ("pool", "act", "dve")[k % 3]
            fw.op(eng, copy_op(eng, dst, cst[sl][0:npart, :]), reads=[B_cst[sl]], writes=[B_wC])

        aC1 = Alloc(R1 + 32768, 59392 - 32768)
        aC2 = Alloc(R2, 20480)
        xr = [aC1(D, F32) for _ in range(2)]
        h1 = [aC1(D, F32) for _ in range(2)]
        x1b = [aC1(D, BF16) for _ in range(2)]
        x1Tb = aC1(8 * 128, BF16, "p (c n) -> p c n", c=8)
        pTb = aC1(2 * 128, BF16, "p (c n) -> p c n", c=2)
        prow = [aC1(PLE, F32) for _ in range(2)]
        x1Tf = aC2(8 * 128, F32, "p (c n) -> p c n", c=8)
        sgt = aC2(D, F32)
        acc = [aC2(D, F32) for _ in range(2)]
        rs = aC2(512, F32)
        B_xr = [Buf("xr%d" % i) for i in range(2)]
        B_h1 = [Buf("h1_%d" % i) for i in range(2)]
        B_x1b = [Buf("x1b%d" % i) for i in range(2)]
        B_x1Tb = Buf("x1Tb")
        B_x1Tf = Buf("x1Tf")
        B_pTb = Buf("pTb")
        B_prow = [Buf("prow%d" % i) for i in range(2)]
        B_sgt = Buf("sgt")
        B_acc = [Buf("acc%d" % i) for i in range(2)]
        B_rs = Buf("rs")
        s_xr = [fw.dsem("xr%d" % i) for i in range(2)]
        s_pr = [fw.dsem("pr%d" % i) for i in range(2)]
        s_acc = [fw.dsem("accst%d" % i) for i in range(2)]
        s_sc = [fw.dsem("scat%d" % i) for i in range(2)]
        s_dbg = fw.dsem("dbg")
        B_ACCD = [Buf("ACCD%d" % t) for t in range(NT)]
        B_dbg = Buf("dbg")
        L36 = rs[:, 0:36]
        gmax = rs[:, 36:37]
        gsum = rs[:, 37:38]
        gp = rs[:, 38:39]
        oh = rs[:, 40:44]
        pen = rs[:, 44:48]
        junk4 = rs[:, 48:52]
        lem = rs[:, 64:96]
        r8 = rs[:, 96:104]
        sel1 = rs[:, 128:160]
        sel2 = rs[:, 160:192]
        selb = rs[:, 192:224]
        tmpa = rs[:, 224:256]
        tmpb = rs[:, 256:288]
        dd = rs[:, 288:289]
        rr = rs[:, 289:290]
        den = rs[:, 290:291]
        slf = rs[:, 292:294]
        stats = rs[:, 300:312]
        mv = rs[:, 312:314]
        rstd = rs[:, 314:315]

        dmy = rs[:, 320:324]
        B_dmy = Buf("dmy")
        fw.op("pool", _L_memset(rs[:, 316:324], 0.0), writes=[B_dmy])

        def prefetch_table(func):
            fw.op("act", _L_activation(out=dmy[:, 2:4], in_=dmy[:, 0:2], func=func), reads=[], writes=[B_dmy])

        def layer_norm(eng_list, h_ap, B_h, g_ap, b_ap, B_gb):
            fw.op("dve", _L_bn_stats(out=stats[:, 0:6], in_=h_ap[:, 0:512]), reads=[B_h], writes=[B_rs])
            fw.op("dve", _L_bn_stats(out=stats[:, 6:12], in_=h_ap[:, 512:1024]), reads=[B_h], writes=[B_rs])
            fw.op("dve", _L_bn_aggr(out=mv, in_=stats), reads=[B_rs], writes=[B_rs])
            fw.op("act", _L_activation(out=rstd, in_=mv[:, 1:2], func=AF.Sqrt, bias=EPS, scale=1.0), reads=[B_rs], writes=[B_rs])
            prefetch_table(AF.Exp)
            fw.op("dve", _L_reciprocal(out=rstd, in_=rstd), reads=[B_rs], writes=[B_rs])
            fw.op("dve", _L_tensor_scalar(out=h_ap, in0=h_ap, scalar1=mv[:, 0:1], scalar2=rstd, op0=ALU.subtract, op1=ALU.mult), reads=[B_h, B_rs], writes=[B_h])
            fw.op("dve", _L_tensor_tensor(out=h_ap, in0=h_ap, in1=g_ap, op=ALU.mult), reads=[B_h, B_gb], writes=[B_h])
            fw.op("dve", _L_tensor_tensor(out=h_ap, in0=h_ap, in1=b_ap, op=ALU.add), reads=[B_h, B_gb], writes=[B_h])

        def load_tok(t):
            k2 = t % 2
            i, hf = t // 2, t % 2
            fw.dma("sp", _L_dma_start(out=xr[k2], in_=xo[i, 2 + hf * 128:2 + (hf + 1) * 128, :]), s_xr[k2], writes=[B_xr[k2]])
            fw.dma("sp", _L_dma_start(out=prow[k2], in_=po[i, hf * 128:(hf + 1) * 128, :]), s_pr[k2], writes=[B_prow[k2]])

        def out_proj(t):
            i = t // 2
            tk = t * 128
            for half in range(2):
                bk = 2 + half
                n_mm = 12
                m = 0
                for cc in range(4):
                    fw.op("pe", _L_matmul(psf[bk][:, :], lhsT=yconvT[:, cc, tk:tk + 128], rhs=wo_c[:, cc, half * 512:(half + 1) * 512], start=(m == 0), stop=False), reads=[B_yc[i], B_wC], writes=[PF[bk]])
                    m += 1
                for hh in range(8):
                    fw.op("pe", _L_matmul(psf[bk][:, :], lhsT=yattnT[0:64, hh, tk:tk + 128], rhs=wo_a[0:64, hh, half * 512:(half + 1) * 512], start=False, stop=(m == n_mm - 1)), reads=[B_ya[hh][i], B_wC], writes=[PF[bk]])
                    m += 1

        load_tok(0)
        out_proj(0)
        prefetch_table(AF.Sqrt)
        for t in range(NT):
            k2 = t % 2
            i, hf = t // 2, t % 2
            tk = t * 128
            if t + 1 < NT:
                load_tok(t + 1)
            for half in range(2):
                bk = 2 + half
                fw.op("dve", _L_scalar_tensor_tensor(out=h1[k2][:, half * 512:(half + 1) * 512], in0=xr[k2][:, half * 512:(half + 1) * 512], scalar=ALPHA, in1=psf[bk][:, :], op0=ALU.mult, op1=ALU.add), reads=[B_xr[k2], PF[bk]], writes=[B_h1[k2]])
            if dbg:
                fw.dma("sp", _L_dma_start(out=dbg_mix[tk:tk + 128, :], in_=h1[k2]), s_dbg, reads=[B_h1[k2]], writes=[B_dbg])
            layer_norm(None, h1[k2], B_h1[k2], lng1, lnb1, B_wC)
            if dbg:
                fw.dma("sp", _L_dma_start(out=dbg_x1[tk:tk + 128, :], in_=h1[k2]), s_dbg, reads=[B_h1[k2]], writes=[B_dbg])
            for g in range(2):
                bk = 2 + g
                for q in range(4):
                    c = 4 * g + q
                    fw.op("pe", _L_transpose(out=psf[bk][:, q * 128:(q + 1) * 128], in_=h1[k2][:, c * 128:(c + 1) * 128], identity=ident_f), reads=[B_h1[k2], B_ident], writes=[PF[bk]])
                fw.op("act", copy_op("act", x1Tf[:, 4 * g:4 * g + 4, :], psf[bk][:, :].rearrange("p (a b) -> p a b", a=4)), reads=[PF[bk]], writes=[B_x1Tf])
                fw.op("dve", copy_op("dve", x1Tb[:, 4 * g:4 * g + 4, :], psf[bk][:, :].rearrange("p (a b) -> p a b", a=4)), reads=[PF[bk]], writes=[B_x1Tb])
            fw.op("act", copy_op("act", x1b[k2], h1[k2]), reads=[B_h1[k2]], writes=[B_x1b[k2]])
            for c in range(8):
                fw.op("pe", _L_matmul(psf[4][:, 0:36], lhsT=x1Tf[:, c, :], rhs=wr[:, c, :], start=(c == 0), stop=(c == 7)), reads=[B_x1Tf, B_wC], writes=[PF[4]])
            for half in range(2):
                bk = half
                for c in range(8):
                    fw.op("pe", _L_matmul(psf[bk][:, :], lhsT=x1Tb[:, c, :], rhs=wpg[:, c, half * 512:(half + 1) * 512], start=(c == 0), stop=(c == 7)), reads=[B_x1Tb, B_wC], writes=[PF[bk]])
            fw.op("dve", _L_tensor_tensor(out=L36, in0=psf[4][:, 0:36], in1=bias36, op=ALU.add), reads=[PF[4], B_const], writes=[B_rs])
            fw.op("dve", _L_tensor_reduce(out=gmax, in_=rs[:, 0:4], axis=AX.X, op=ALU.max), reads=[B_rs], writes=[B_rs])
            fw.op("dve", _L_tensor_scalar(out=dd, in0=gmax, scalar1=-1.0, scalar2=None, op0=ALU.mult), reads=[B_rs], writes=[B_rs])
            fw.op("act", _L_activation(out=junk4, in_=rs[:, 0:4], func=AF.Exp, bias=dd, scale=1.0, accum_out=gsum), reads=[B_rs], writes=[B_rs])
            fw.op("dve", _L_reciprocal(out=gp, in_=gsum), reads=[B_rs], writes=[B_rs])
            fw.op("dve", _L_tensor_scalar(out=oh, in0=rs[:, 0:4], scalar1=gmax, scalar2=None, op0=ALU.is_equal), reads=[B_rs], writes=[B_rs])
            fw.op("dve", _L_tensor_scalar(out=pen, in0=oh, scalar1=1.0, scalar2=1.0e30, op0=ALU.subtract, op1=ALU.mult), reads=[B_rs], writes=[B_rs])
            for g in range(4):
                fw.op("dve", _L_tensor_scalar(out=lem[:, 8 * g:8 * g + 8], in0=rs[:, 4 + 8 * g:12 + 8 * g], scalar1=pen[:, g:g + 1], scalar2=None, op0=ALU.add), reads=[B_rs], writes=[B_rs])
            fw.op("dve", _L_max(out=r8, in_=lem), reads=[B_rs], writes=[B_rs])
            fw.op("dve", _L_tensor_scalar(out=sel1, in0=lem, scalar1=r8[:, 0:1], scalar2=None, op0=ALU.is_equal), reads=[B_rs], writes=[B_rs])
            fw.op("dve", _L_tensor_scalar(out=sel2, in0=lem, scalar1=r8[:, 1:2], scalar2=None, op0=ALU.is_equal), reads=[B_rs], writes=[B_rs])
            fw.op("dve", _L_tensor_tensor(out=selb, in0=sel1, in1=sel2, op=ALU.add), reads=[B_rs], writes=[B_rs])
            fw.op("dve", _L_tensor_tensor(out=dd, in0=r8[:, 1:2], in1=r8[:, 0:1], op=ALU.subtract), reads=[B_rs], writes=[B_rs])
            fw.op("act", _L_activation(out=rr, in_=dd, func=AF.Exp), reads=[B_rs], writes=[B_rs])
            prefetch_table(AF.Sigmoid)
            fw.op("dve", _L_tensor_scalar(out=den, in0=rr, scalar1=1.0, scalar2=None, op0=ALU.add), reads=[B_rs], writes=[B_rs])
            fw.op("dve", _L_reciprocal(out=den, in_=den), reads=[B_rs], writes=[B_rs])
            fw.op("dve", _L_tensor_tensor(out=w12[:, t, 0:1], in0=gp, in1=den, op=ALU.mult), reads=[B_rs], writes=[B_w12[t]])
            fw.op("dve", _L_tensor_tensor(out=w12[:, t, 1:2], in0=w12[:, t, 0:1], in1=rr, op=ALU.mult), reads=[B_rs, B_w12[t]], writes=[B_w12[t]])
            fw.op("pe", _L_matmul(psf[5][:, 0:32], lhsT=Lst, rhs=selb, start=True, stop=False), reads=[B_rs, B_const], writes=[PF[5]])
            fw.op("pe", _L_matmul(psf[5][:, 0:32], lhsT=ones_f, rhs=Spre, start=False, stop=True), reads=[B_Spre, B_const], writes=[PF[5]])
            if t + 1 < NT:
                out_proj(t + 1)
            fw.op("dve", _L_tensor_tensor(out=tmpa, in0=psf[5][:, 0:32], in1=eC, op=ALU.add), reads=[PF[5], B_const], writes=[B_rs])
            fw.op("dve", _L_tensor_scalar(out=tmpb, in0=psf[5][:, 0:32], scalar1=float(CAP), scalar2=1.0e6, op0=ALU.is_ge, op1=ALU.mult), reads=[PF[5]], writes=[B_rs])
            fw.op("dve", _L_tensor_tensor(out=tmpa, in0=tmpa, in1=tmpb, op=ALU.add), reads=[B_rs], writes=[B_rs])
            fw.op("dve", _L_tensor_tensor(out=tmpb, in0=tmpa, in1=sel1, op=ALU.mult), reads=[B_rs], writes=[B_rs])
            fw.op("dve", _L_tensor_reduce(out=slf[:, 0:1], in_=tmpb, axis=AX.X, op=ALU.add), reads=[B_rs], writes=[B_rs])
            fw.op("dve", _L_tensor_tensor(out=tmpb, in0=tmpa, in1=sel2, op=ALU.mult), reads=[B_rs], writes=[B_rs])
            fw.op("dve", _L_tensor_reduce(out=slf[:, 1:2], in_=tmpb, axis=AX.X, op=ALU.add), reads=[B_rs], writes=[B_rs])
            fw.op("dve", _L_tensor_copy(out=slot_i[:, t, :], in_=slf), reads=[B_rs], writes=[B_slot[t]])
            fw.op("dve", _L_tensor_tensor(out=Spre, in0=Spre, in1=selb, op=ALU.add), reads=[B_rs, B_Spre], writes=[B_Spre])
            for k in range(2):
                fw.dma("pool", _L_indirect_dma_start(out=XS, out_offset=bass.IndirectOffsetOnAxis(ap=slot_i[:, t, k:k + 1], axis=0), in_=x1b[k2], in_offset=None, bounds_check=NSL - 1, oob_is_err=False), s_sc[k2], reads=[B_x1b[k2], B_slot[t], B_XS], writes=[B_XS])
            for half in range(2):
                fw.op("act", _L_activation(out=sgt[:, half * 512:(half + 1) * 512], in_=psf[half][:, :], func=AF.Sigmoid), reads=[PF[half]], writes=[B_sgt])
            prefetch_table(AF.Sqrt)
            for c in range(2):
                fw.op("pe", _L_transpose(out=psf[4][:, c * 128:(c + 1) * 128], in_=prow[k2][:, c * 128:(c + 1) * 128], identity=ident_f), reads=[B_prow[k2], B_ident], writes=[PF[4]])
            fw.op("act", copy_op("act", pTb, psf[4][:, 0:256].rearrange("p (a b) -> p a b", a=2)), reads=[PF[4]], writes=[B_pTb])
            for half in range(2):
                bk = half
                for c in range(2):
                    fw.op("pe", _L_matmul(psf[bk][:, :], lhsT=pTb[:, c, :], rhs=wpp[:, c, half * 512:(half + 1) * 512], start=(c == 0), stop=(c == 1)), reads=[B_pTb, B_wC], writes=[PF[bk]])
                fw.op("dve", _L_tensor_tensor(out=acc[k2][:, half * 512:(half + 1) * 512], in0=sgt[:, half * 512:(half + 1) * 512], in1=psf[bk][:, :], op=ALU.mult), reads=[B_sgt, PF[bk]], writes=[B_acc[k2]])
            fw.op("dve", _L_scalar_tensor_tensor(out=acc[k2], in0=h1[k2], scalar=ALPHA, in1=acc[k2], op0=ALU.mult, op1=ALU.add), reads=[B_h1[k2], B_acc[k2]], writes=[B_acc[k2]])
            fw.dma("sp", _L_dma_start(out=ACCD[tk:tk + 128, :], in_=acc[k2]), s_acc[k2], reads=[B_acc[k2]], writes=[B_ACCD[t]])
        fw.barrier()

        aD = Alloc(R0, 98304)
        wstg = [aD(8 * DE, F32) for _ in range(3)]
        wbf = [aD(8 * DE, BF16) for _ in range(6)]
        aD1 = Alloc(R1, 59392)
        Xe = [aD1(D, BF16) for _ in range(2)]
        XeT = [aD1(8 * 256, BF16, "p (c n) -> p c n", c=8) for _ in range(2)]
        hT = [aD1(4 * 256, BF16, "p (c n) -> p c n", c=4) for _ in range(2)]
        sgu = [aD1(256, F32) for _ in range(2)]
        Yst = [aD1(D, F32) for _ in range(2)]
        Y1 = [aD1(D, F32) for _ in range(2)]
        Y2 = [aD1(D, F32) for _ in range(2)]
        accr = [aD1(D, F32) for _ in range(3)]
        aD2 = Alloc(R2, 20480)
        lng2 = aD2(D, F32)
        lnb2 = aD2(D, F32)
        rs2 = aD2(64, F32)
        B_wstg = [Buf("wstg%d" % i) for i in range(3)]
        B_wbf = [Buf("wbf%d" % i) for i in range(6)]
        B_Xe = [Buf("Xe%d" % i) for i in range(2)]
        B_XeT = [Buf("XeT%d" % i) for i in range(2)]
        B_hT = [Buf("hT%d" % i) for i in range(2)]
        B_sgu = [Buf("sgu%d" % i) for i in range(2)]
        B_Yst = [Buf("Yst%d" % i) for i in range(2)]
        B_Y1 = [Buf("Y1_%d" % i) for i in range(2)]
        B_Y2 = [Buf("Y2_%d" % i) for i in range(2)]
        B_accr = [Buf("accr%d" % i) for i in range(3)]
        B_ln2 = Buf("ln2")
        B_YS = Buf("YS")
        s_wstg = [fw.dsem("wstg%d" % i) for i in range(3)]
        s_xe = [fw.dsem("xe%d" % i) for i in range(2)]
        s_yst = [fw.dsem("yst%d" % i) for i in range(2)]
        s_g = [fw.dsem("gath%d" % i) for i in range(2)]
        s_accr = [fw.dsem("accr%d" % i) for i in range(3)]
        s_out = [fw.dsem("out%d" % i) for i in range(3)]
        s_ln2 = fw.dsem("ln2")
        B_out = Buf("out")
        fw.dma("sp", _L_dma_start(out=lng2, in_=ln2_g.partition_broadcast(128)), s_ln2, writes=[B_ln2])
        fw.dma("sp", _L_dma_start(out=lnb2, in_=ln2_b.partition_broadcast(128)), s_ln2, writes=[B_ln2])

        mats = []
        for ex in range(NE):
            mats.append(("g", ex))
            mats.append(("u", ex))
            mats.append(("d", ex))

        CAST_ENG = ("pool", "act", "dve", "dve", "pool", "act", "dve", "act", "pool", "dve", "act", "dve")

        pending = {"act": [], "dve": []}

        def load_mat(mi):
            kind, ex = mats[mi]
            sl = mi % 3
            bs = mi % 6
            if kind in ("g", "u"):
                src = (w_gate if kind == "g" else w_up)[ex].rearrange("(p c) f -> p c f", c=8)
                dstv = wstg[sl].rearrange("p (c f) -> p c f", c=8)
            else:
                src = w_down[ex].rearrange("(c p) n -> p c n", p=128)
                dstv = wstg[sl].rearrange("p (c n) -> p c n", c=4)
            fw.dma("sp", _L_dma_start(out=dstv, in_=src), s_wstg[sl], writes=[B_wstg[sl]])
            for k in range(4):
                eng = CAST_ENG[(4 * mi + k) % 12]
                o, i_ = wbf[bs][:, k * 1024:(k + 1) * 1024], wstg[sl][:, k * 1024:(k + 1) * 1024]

                def emit(eng=eng, o=o, i_=i_, sl=sl, bs=bs):
                    fw.op(eng, copy_op(eng, o, i_), reads=[B_wstg[sl]], writes=[B_wbf[bs]])
                if eng == "pool":
                    emit()
                else:
                    pending[eng].append(emit)

        def drain(eng, n):
            for _ in range(n):
                if pending[eng]:
                    pending[eng].pop(0)()

        XSe = XS.rearrange("(e s p) d -> e s p d", s=2, p=128)
        YSe = YS.rearrange("(e s p) d -> e s p d", s=2, p=128)

        def load_xe(ex):
            for s2 in range(2):
                fw.dma("act", _L_dma_start(out=Xe[s2], in_=XSe[ex, s2]), s_xe[s2], reads=[B_XS], writes=[B_Xe[s2]])

        for mi in range(3):
            load_mat(mi)
        drain("act", 99)
        drain("dve", 99)
        load_xe(0)
        yi = [0]
        for ex in range(NE):
            k2 = ex % 2
            bb = (3 * ex) % 6
            wg = wbf[bb].rearrange("p (c f) -> p c f", c=8)
            wu = wbf[bb + 1].rearrange("p (c f) -> p c f", c=8)
            wd = wbf[bb + 2].rearrange("p (c n) -> p c n", c=4)
            if ex + 1 < NE:
                for q in range(3):
                    load_mat(3 * (ex + 1) + q)
            for s2 in range(2):
                pb = s2
                for c in range(8):
                    fw.op("pe", _L_transpose(out=psb[pb][:, c * 128:(c + 1) * 128], in_=Xe[s2].rearrange("t (p c) -> t c p", c=8)[:, c, :], identity=ident_b), reads=[B_Xe[s2], B_const], writes=[PB[pb]])
                eng = alt(s2)
                fw.op(eng, copy_op(eng, XeT[k2][:, :, s2 * 128:(s2 + 1) * 128], psb[pb][:, :].rearrange("p (c n) -> p c n", c=8)), reads=[PB[pb]], writes=[B_XeT[k2]])
            if ex + 1 < NE:
                load_xe(ex + 1)
            for fo in range(4):
                bg, bu = (0, 1) if fo % 2 == 0 else (2, 3)
                for (bk, wm, bi) in ((bg, wg, bb), (bu, wu, bb + 1)):
                    for c in range(8):
                        fw.op("pe", _L_matmul(psf[bk][:, 0:256], lhsT=wm[:, c, fo * 128:(fo + 1) * 128], rhs=XeT[k2][:, c, :], start=(c == 0), stop=(c == 7)), reads=[B_XeT[k2], B_wbf[bi]], writes=[PF[bk]])
                sq = fo % 2
                fw.op("act", _L_activation(out=sgu[sq], in_=psf[bg][:, 0:256], func=AF.Silu), reads=[PF[bg]], writes=[B_sgu[sq]])
                fw.op("dve", _L_tensor_tensor(out=hT[k2][:, fo, :], in0=sgu[sq], in1=psf[bu][:, 0:256], op=ALU.mult), reads=[B_sgu[sq], PF[bu]], writes=[B_hT[k2]])
                drain("act", 1)
                drain("dve", 1)
            for s2 in range(2):
                ys = yi[0] % 2
                yi[0] += 1
                for half in range(2):
                    bk = 4 + half
                    for fo in range(4):
                        fw.op("pe", _L_matmul(psf[bk][:, :], lhsT=hT[k2][:, fo, s2 * 128:(s2 + 1) * 128], rhs=wd[:, fo, half * 512:(half + 1) * 512], start=(fo == 0), stop=(fo == 3)), reads=[B_hT[k2], B_wbf[bb + 2]], writes=[PF[bk]])
                    eng = alt(half)
                    fw.op(eng, copy_op(eng, Yst[ys][:, half * 512:(half + 1) * 512], psf[bk][:, :]), reads=[PF[bk]], writes=[B_Yst[ys]])
                fw.dma("act", _L_dma_start(out=YSe[ex, s2], in_=Yst[ys]), s_yst[ys], reads=[B_Yst[ys]], writes=[])
            drain("act", 99)
            drain("dve", 99)

        fw.barrier()

        st2 = rs2[:, 0:12]
        mv2 = rs2[:, 12:14]
        rstd2 = rs2[:, 14:15]

        def fin_issue(t):
            k2, k3 = t % 2, t % 3
            tk = t * 128
            fw.op("pool", _L_memset(Y1[k2], 0.0), writes=[B_Y1[k2]])
            fw.op("pool", _L_memset(Y2[k2], 0.0), writes=[B_Y2[k2]])
            fw.dma("sp", _L_dma_start(out=accr[k3], in_=ACCD[tk:tk + 128, :]), s_accr[k3], reads=[B_ACCD[t]], writes=[B_accr[k3]])
            fw.dma("pool", _L_indirect_dma_start(out=Y1[k2], out_offset=None, in_=YS, in_offset=bass.IndirectOffsetOnAxis(ap=slot_i[:, t, 0:1], axis=0), bounds_check=NSL - 1, oob_is_err=False), s_g[k2], reads=[B_YS, B_slot[t]], writes=[B_Y1[k2]])
            fw.dma("pool", _L_indirect_dma_start(out=Y2[k2], out_offset=None, in_=YS, in_offset=bass.IndirectOffsetOnAxis(ap=slot_i[:, t, 1:2], axis=0), bounds_check=NSL - 1, oob_is_err=False), s_g[k2], reads=[B_YS, B_slot[t]], writes=[B_Y2[k2]])

        def fin_front(t):
            k2, k3 = t % 2, t % 3
            h_ap, B_h, B_r = accr[k3], B_accr[k3], B_rs
            fw.op("dve", _L_scalar_tensor_tensor(out=h_ap, in0=Y1[k2], scalar=w12[:, t, 0:1], in1=h_ap, op0=ALU.mult, op1=ALU.add), reads=[B_Y1[k2], B_w12[t], B_h], writes=[B_h])
            fw.op("dve", _L_scalar_tensor_tensor(out=h_ap, in0=Y2[k2], scalar=w12[:, t, 1:2], in1=h_ap, op0=ALU.mult, op1=ALU.add), reads=[B_Y2[k2], B_w12[t], B_h], writes=[B_h])
            fw.op("dve", _L_bn_stats(out=st2[:, 0:6], in_=h_ap[:, 0:512]), reads=[B_h], writes=[B_r])
            fw.op("dve", _L_bn_stats(out=st2[:, 6:12], in_=h_ap[:, 512:1024]), reads=[B_h], writes=[B_r])
            fw.op("dve", _L_bn_aggr(out=mv2, in_=st2), reads=[B_r], writes=[B_r])
            fw.op("act", _L_activation(out=rstd2, in_=mv2[:, 1:2], func=AF.Sqrt, bias=EPS, scale=1.0), reads=[B_r], writes=[B_r])
            fw.op("dve", _L_reciprocal(out=rstd2, in_=rstd2), reads=[B_r], writes=[B_r])
            fw.op("dve", _L_tensor_scalar(out=h_ap, in0=h_ap, scalar1=mv2[:, 0:1], scalar2=rstd2, op0=ALU.subtract, op1=ALU.mult), reads=[B_h, B_r], writes=[B_h])
            fw.op("dve", _L_tensor_tensor(out=h_ap, in0=h_ap, in1=lng2, op=ALU.mult), reads=[B_h, B_ln2], writes=[B_h])

        def fin_back(t):
            k3 = t % 3
            tk = t * 128
            fw.op("dve", _L_tensor_tensor(out=accr[k3], in0=accr[k3], in1=lnb2, op=ALU.add), reads=[B_accr[k3], B_ln2], writes=[B_accr[k3]])
            fw.dma("sp", _L_dma_start(out=out[tk:tk + 128, :], in_=accr[k3]), s_out[k3], reads=[B_accr[k3]], writes=[])

        fin_issue(0)
        for t in range(NT):
            if t + 1 < NT:
                fin_issue(t + 1)
            fin_front(t)
            if t >= 1:
                fin_back(t - 1)
        fin_back(NT - 1)
        fw.barrier()
        with nc.allow_non_contiguous_dma(reason="tiny strided parameter loads"):
            fw.replay()
    nc._fw_names = fw.names
    return nc


def own_blocks(r, NB):
    NSLOT = NB // 4
    js = []
    for i in range(NSLOT):
        if i < NSLOT // 2:
            js.append(r + 4 * i)
        else:
            js.append(NB - 1 - r - 4 * (NSLOT - 1 - i))
    return js


def make_in_maps(inputs, S):
    NB = S // 256
    x = np.asarray(inputs["x"], dtype=np.float32)
    p = np.asarray(inputs["p"], dtype=np.float32)
    nbatch = x.shape[0]
    shared = {
        "w_in": inputs["w_in"][0], "w_conv": inputs["w_conv"][0], "w_out": inputs["w_out"][0],
        "ln1_g": inputs["ln1_g"][0], "ln1_b": inputs["ln1_b"][0],
        "w_rg": inputs["w_router_g"][0], "b_rg": inputs["b_router_g"][0],
        "w_re": inputs["w_router_e"][0], "b_re": inputs["b_router_e"][0],
        "w_gate": inputs["w_gate"][0], "w_up": inputs["w_up"][0], "w_down": inputs["w_down"][0],
        "w_pg": inputs["w_ple_gate"][0], "w_pp": inputs["w_ple_proj"][0],
        "ln2_g": inputs["ln2_g"][0], "ln2_b": inputs["ln2_b"][0],
    }
    shared = {k: np.ascontiguousarray(np.asarray(v, dtype=np.float32)) for k, v in shared.items()}
    maps = []
    for b in range(nbatch):
        for r in range(4):
            js = own_blocks(r, NB)
            xo = np.zeros((len(js), 258, D), np.float32)
            po = np.zeros((len(js), 256, PLE), np.float32)
            nm1 = np.zeros((len(js), 8, 32), np.float32)
            nm2 = np.zeros((len(js), 8, 32), np.float32)
            for i, j in enumerate(js):
                lo = 256 * j - 2
                if lo >= 0:
                    xo[i] = x[b, lo:lo + 258]
                else:
                    xo[i, 2:] = x[b, 0:256]
                po[i] = p[0, b, 256 * j:256 * j + 256]
                nm1[i, :, j:] = NEGINF
                nm2[i, :, j:] = NEG
            m = dict(shared)
            m["xfT"] = np.ascontiguousarray(x[b].T)
            m["xoT"] = np.ascontiguousarray(xo.transpose(0, 2, 1))
            m["xo"] = xo
            m["po"] = po
            m["nm1"] = nm1.reshape(-1)
            m["nm2"] = nm2.reshape(-1)
            maps.append(m)
    return maps


_NC_CACHE = {}


def kernel(**inputs):
    x = np.asarray(inputs["x"])
    nbatch, S, _ = x.shape
    NB = S // 256
    if S not in _NC_CACHE:
        _NC_CACHE[S] = build_nc(S)
    nc = _NC_CACHE[S]
    maps = make_in_maps(inputs, S)
    res = run_bass_kernel_spmd(nc, maps, core_ids=list(range(len(maps))))
    outp = np.zeros((nbatch, S, D), np.float32)
    k = 0
    for b in range(nbatch):
        for r in range(4):
            o = np.asarray(res.results[k]["out"], dtype=np.float32)
            for i, j in enumerate(own_blocks(r, NB)):
                outp[b, 256 * j:256 * j + 256] = o[256 * i:256 * i + 256]
            k += 1
    return outp
```

```python
import numpy as np
from contextlib import ExitStack

import concourse.bass as bass
import concourse.mybir as mybir
from concourse.bass_utils import run_bass_kernel_spmd

F32 = mybir.dt.float32
BF16 = mybir.dt.bfloat16
U8 = mybir.dt.uint8
I32 = mybir.dt.int32
AF = mybir.ActivationFunctionType
ALU = mybir.AluOpType
AX = mybir.AxisListType

D = 1024
DIN = 3072
NH = 8
HD = 64
NE = 32
DE = 512
PLE = 256
CAP = 256
ALPHA = float(2 ** 0.25)
EPS = 1e-5
NEG = -30000.0
NEGINF = -1.0e30


class Eng:
    def __init__(self, name, sem, same_wait):
        self.name = name
        self.sem = sem
        self.count = 0
        self.waited = {}
        self.ops = []
        self.same_wait = same_wait


class Buf:
    __slots__ = ("name", "w", "r", "excl")

    def __init__(self, name, excl=False):
        self.name = name
        self.w = None
        self.r = []
        self.excl = excl


class FW:
    def __init__(self, nc, stack):
        self.nc = nc
        self.stack = stack
        self.eng = {}
        for n, sw in (("pe", False), ("act", True), ("dve", True), ("pool", True), ("sp", False)):
            s = stack.enter_context(nc.semaphore("sem_" + n))
            self.eng[n] = Eng(n, s, sw)
        self.dsems = []
        self.names = {}
        self.bc_reg = None

    def dsem(self, name):
        s = [self.stack.enter_context(self.nc.semaphore(name)), 0]
        self.dsems.append(s)
        return s

    def _wait(self, e, s, v):
        if isinstance(s, list):
            s, v = s[0], s[1]
        if v <= 0:
            return
        k = id(s)
        if e.waited.get(k, 0) < v:
            e.waited[k] = v
            e.ops.append(lambda en, s=s, v=v: en.wait_ge(s, v))

    def _deps(self, e, reads, writes):
        for b in reads:
            if b.w is not None:
                s, v = b.w
                if not (s is e.sem and not e.same_wait):
                    self._wait(e, s, v)
            if b.excl:
                for (s, v) in b.r:
                    if not (s is e.sem and not e.same_wait):
                        self._wait(e, s, v)
        for b in writes:
            if b.w is not None:
                s, v = b.w
                if not (s is e.sem and not e.same_wait):
                    self._wait(e, s, v)
            for (s, v) in b.r:
                if not (s is e.sem and not e.same_wait):
                    self._wait(e, s, v)

    def _mark(self, tok, reads, writes):
        for b in reads:
            if b.excl:
                b.r = [tok]
            else:
                b.r.append(tok)
                if len(b.r) > 64:
                    b.r = b.r[-64:]
        for b in writes:
            b.w = tok
            b.r = []

    def op(self, engname, fn, reads=(), writes=()):
        e = self.eng[engname]
        self._deps(e, reads, writes)
        e.count += 1
        tok = (e.sem, e.count)
        import sys as _sys
        line = _sys._getframe(1).f_lineno

        def run(en, fn=fn, s=e.sem, line=line):
            ins = fn(en)
            try:
                self.names[ins.ins.name] = line
            except Exception:
                pass
            return ins.then_inc(s, 1)
        e.ops.append(run)
        self._mark(tok, reads, writes)
        return tok

    def dma(self, engname, fn, sem_state, reads=(), writes=()):
        e = self.eng[engname]
        saved = []
        for b in writes:
            if b.w is not None and b.w[0] is sem_state and not b.r:
                saved.append((b, b.w))
                b.w = None
        self._deps(e, reads, writes)
        for b, w in saved:
            b.w = w
        sem_state[1] += 16
        tok = (sem_state, None)
        e.ops.append(lambda en, fn=fn, s=sem_state[0]: fn(en).then_inc(s, 16))
        self._mark(tok, reads, writes)
        return tok

    def barrier(self):
        names = ["pe", "act", "dve", "pool", "sp"]
        snap = [(self.eng[n].sem, self.eng[n].count) for n in names]
        dsnap = [(s[0], s[1]) for s in self.dsems]
        for n in names:
            e = self.eng[n]
            for (s, v) in snap:
                if s is e.sem:
                    continue
                self._wait(e, s, v)
            for (s, v) in dsnap:
                self._wait(e, s, v)

    def replay(self):
        nc = self.nc
        with nc.Block() as block:
            @block.tensor
            def _(en):
                for f in self.eng["pe"].ops:
                    f(en)

            @block.scalar
            def _(en):
                for f in self.eng["act"].ops:
                    f(en)

            @block.vector
            def _(en):
                for f in self.eng["dve"].ops:
                    f(en)

            @block.gpsimd
            def _(en):
                for f in self.eng["pool"].ops:
                    f(en)

            @block.sync
            def _(en):
                for f in self.eng["sp"].ops:
                    f(en)


def build_nc(S=8192, dbg=False, stop_after=None):
    NB = S // 256
    NSLOT = NB // 4
    NOWN = NSLOT * 256
    TOT = S + NOWN
    NT = NOWN // 128
    NCH = S // 512
    NKT = TOT // 128
    NSL = NE * CAP

    nc = bass.Bass("TRN2", target_bir_lowering=False)

    def din(name, shape, dt=F32):
        return nc.dram_tensor(name, list(shape), dt, kind="ExternalInput").ap()

    xfT = din("xfT", [D, S])
    xoT = din("xoT", [NSLOT, D, 258])
    xo = din("xo", [NSLOT, 258, D])
    po = din("po", [NSLOT, 256, PLE])
    nm1_d = din("nm1", [NSLOT * 256])
    nm2_d = din("nm2", [NSLOT * 256])
    w_in = din("w_in", [D, DIN])
    w_conv = din("w_conv", [3, 512])
    w_out = din("w_out", [D, D])
    ln1_g = din("ln1_g", [D])
    ln1_b = din("ln1_b", [D])
    w_rg = din("w_rg", [D, 4])
    b_rg = din("b_rg", [4])
    w_re = din("w_re", [D, NE])
    b_re = din("b_re", [NE])
    w_gate = din("w_gate", [NE, D, DE])
    w_up = din("w_up", [NE, D, DE])
    w_down = din("w_down", [NE, DE, D])
    w_pg = din("w_pg", [D, D])
    w_pp = din("w_pp", [PLE, D])
    ln2_g = din("ln2_g", [D])
    ln2_b = din("ln2_b", [D])
    out = nc.dram_tensor("out", [NOWN, D], F32, kind="ExternalOutput").ap()
    if dbg:
        dbg_x1 = nc.dram_tensor("dbg_x1", [NOWN, D], F32, kind="ExternalOutput").ap()
        dbg_mix = nc.dram_tensor("dbg_mix", [NOWN, D], F32, kind="ExternalOutput").ap()

    KT = nc.dram_tensor("KT_scr", [NH * HD, TOT], BF16).ap()
    VV = nc.dram_tensor("VV_scr", [NH, 128, NKT, 65], BF16).ap()
    XS = nc.dram_tensor("XS_scr", [NSL, D], BF16).ap()
    YS = nc.dram_tensor("YS_scr", [NSL, D], F32).ap()
    ACCD = nc.dram_tensor("ACC_scr", [NOWN, D], F32).ap()

    st = ExitStack()
    with st:
        fw = FW(nc, st)
        ARENA = 98304 + 59392 + 20480 + 8192
        arena = st.enter_context(nc.sbuf_tensor("arena", [128, ARENA], U8))
        R0, R1, R2, RC = 0, 98304, 98304 + 59392, 98304 + 59392 + 20480

        class Alloc:
            def __init__(self, base, size):
                self.base, self.size, self.off = base, size, 0

            def __call__(self, nelem, dtype, pat=None, **kw):
                esz = 4 if dtype in (F32, I32) else 2
                nbytes = nelem * esz
                o = self.base + self.off
                self.off += (nbytes + 63) // 64 * 64
                assert self.off <= self.size, ("arena overflow", self.base, self.off, self.size)
                a = arena[:, o:o + nbytes].bitcast(dtype)
                if pat:
                    a = a.rearrange(pat, **kw)
                return a

        psall = st.enter_context(nc.psum_tensor("psall", [128, 4096], F32))
        psf = [psall[:, i * 512:(i + 1) * 512] for i in range(6)]
        psb = [psall[:, (6 + i) * 512:(7 + i) * 512].bitcast(BF16) for i in range(2)]
        psw = [psall[:, j * 1024:(j + 1) * 1024] for j in range(2)]
        pse = psall[:, 6 * 512:7 * 512]
        PW = [Buf("psw%d" % j, excl=True) for j in range(2)]
        PF = [Buf("psf%d" % i, excl=True) for i in range(6)]
        PB = [Buf("psb%d" % i, excl=True) for i in range(2)]

        def alt(i):
            return "act" if i % 2 == 0 else "dve"


        def MM(o, l, r, st_, sp_):
            return lambda e: e.matmul(o, lhsT=l, rhs=r, start=st_, stop=sp_)

        def TR(o, i_, idn):
            return lambda e: e.transpose(out=o, in_=i_, identity=idn)

        def ACTF(o, i_, f, **kw):
            return lambda e: e.activation(out=o, in_=i_, func=f, **kw)

        def TT(o, a, b, op):
            return lambda e: e.tensor_tensor(out=o, in0=a, in1=b, op=op)

        def TS(o, a, s1, s2, op0, op1=None):
            if op1 is None:
                return lambda e: e.tensor_scalar(out=o, in0=a, scalar1=s1, scalar2=None, op0=op0)
            return lambda e: e.tensor_scalar(out=o, in0=a, scalar1=s1, scalar2=s2, op0=op0, op1=op1)

        def STT(o, a, sc, b, op0, op1):
            return lambda e: e.scalar_tensor_tensor(out=o, in0=a, scalar=sc, in1=b, op0=op0, op1=op1)

        def MS(ap, v):
            return lambda e: e.memset(ap, v)

        def RD(o, i_, op):
            return lambda e: e.tensor_reduce(out=o, in_=i_, axis=AX.X, op=op)

        def MX(o, i_):
            return lambda e: e.max(out=o, in_=i_)

        def RCP(o, i_):
            return lambda e: e.reciprocal(out=o, in_=i_)

        def DM(o, i_):
            return lambda e: e.dma_start(out=o, in_=i_)

        def CPY(o, i_):
            return lambda e: e.tensor_copy(out=o, in_=i_)

        def ASEL(o, i_, pattern, cmp, fill, base, cm):
            return lambda e: e.affine_select(out=o, in_=i_, pattern=pattern, compare_op=cmp, fill=fill, base=base, channel_multiplier=cm)

        def BNS(o, i_):
            return lambda e: e.bn_stats(out=o, in_=i_)

        def BNA(o, i_):
            return lambda e: e.bn_aggr(out=o, in_=i_)

        def SCAT(dst, idx, src, bc):
            return lambda e: e.indirect_dma_start(out=dst, out_offset=bass.IndirectOffsetOnAxis(ap=idx, axis=0), in_=src, in_offset=None, bounds_check=bc, oob_is_err=False)

        def GATH(dst, src, idx, bc):
            return lambda e: e.indirect_dma_start(out=dst, out_offset=None, in_=src, in_offset=bass.IndirectOffsetOnAxis(ap=idx, axis=0), bounds_check=bc, oob_is_err=False)


        def _L_matmul(o, lhsT=None, rhs=None, start=None, stop=None):
            return lambda e: e.matmul(o, lhsT=lhsT, rhs=rhs, start=start, stop=stop)

        def _mk(meth):
            def f(*a, **kw):
                return lambda e: getattr(e, meth)(*a, **kw)
            return f

        _L_transpose = _mk("transpose")
        _L_activation = _mk("activation")
        _L_tensor_tensor = _mk("tensor_tensor")
        _L_tensor_scalar = _mk("tensor_scalar")
        _L_scalar_tensor_tensor = _mk("scalar_tensor_tensor")
        _L_memset = _mk("memset")
        _L_tensor_reduce = _mk("tensor_reduce")
        _L_max = _mk("max")
        _L_reciprocal = _mk("reciprocal")
        _L_dma_start = _mk("dma_start")
        _L_tensor_copy = _mk("tensor_copy")
        _L_affine_select = _mk("affine_select")
        _L_bn_stats = _mk("bn_stats")
        _L_bn_aggr = _mk("bn_aggr")
        def _L_indirect_dma_start(*a, **kw):
            def f(e):
                if fw.bc_reg is None:
                    fw.bc_reg = e.to_reg(kw["bounds_check"])
                kw2 = dict(kw)
                kw2["bounds_check"] = fw.bc_reg
                return e.indirect_dma_start(*a, **kw2)
            return f
        _L_iota = _mk("iota")

        def copy_op(eng, out_ap, in_ap):
            if eng == "act":
                return lambda e: e.activation(out=out_ap, in_=in_ap, func=AF.Copy)
            return lambda e: e.tensor_copy(out=out_ap, in_=in_ap)

        ac = Alloc(RC, 8192)
        ident_f = ac(128, F32)
        ident_b = ac(128, BF16)
        ones_f = ac(128, F32)
        CBm = ac(512, BF16, "p (t q) -> p t q", t=2)
        wc = ac(12, F32, "p (c k) -> p c k", c=4)
        km = ac(4 * 32, F32, "p (c n) -> p c n", c=4)
        kmb = ac(4 * 32, BF16, "p (c n) -> p c n", c=4)
        kmh = ac(8 * 32, BF16, "p (h n) -> p h n", h=8)
        slot_i = ac(NT * 2, I32, "p (t k) -> p t k", k=2)
        w12 = ac(NT * 2, F32, "p (t k) -> p t k", k=2)
        Lst = ac(128, F32)
        eC = ac(32, F32)
        Spre = ac(32, F32)
        bias36 = ac(36, F32)
        ztile = ac(1024, BF16)
        B_ident = Buf("ident")
        B_const = Buf("const")
        B_km = Buf("km")
        B_kmh = Buf("kmh")
        B_slot = [Buf("slot%d" % t) for t in range(NT)]
        B_w12 = [Buf("w12_%d" % t) for t in range(NT)]
        B_Spre = Buf("Spre")

        s_setup = fw.dsem("setup")

        fw.op("pool", _L_memset(ident_f, 0.0), writes=[B_ident])
        fw.op("pool", _L_affine_select(out=ident_f, in_=ident_f, pattern=[[-1, 128]], compare_op=ALU.not_equal, fill=1.0, base=0, channel_multiplier=1), reads=[B_ident], writes=[B_ident])
        fw.op("pool", _L_tensor_copy(out=ident_b, in_=ident_f), reads=[B_ident], writes=[B_const])
        fw.op("pool", _L_memset(ones_f, 1.0), writes=[B_const])
        fw.op("pool", _L_memset(CBm, 0.0), writes=[B_const])
        fw.op("pool", _L_affine_select(out=CBm, in_=CBm, pattern=[[-128, 2], [1, 256]], compare_op=ALU.is_ge, fill=NEG, base=0, channel_multiplier=-1), reads=[B_const], writes=[B_const])
        fw.op("pool", _L_memset(Lst, 1.0), writes=[B_const])
        fw.op("pool", _L_affine_select(out=Lst, in_=Lst, pattern=[[1, 128]], compare_op=ALU.is_ge, fill=0.0, base=-1, channel_multiplier=-1), reads=[B_const], writes=[B_const])
        fw.op("pool", _L_iota(eC, pattern=[[CAP, 32]], base=0, channel_multiplier=0, allow_small_or_imprecise_dtypes=True), writes=[B_const])
        fw.op("pool", _L_memset(Spre, 0.0), writes=[B_Spre])
        for c_ in range(4):
            for k_ in range(3):
                fw.dma("sp", _L_dma_start(out=wc[:, c_, k_:k_ + 1], in_=w_conv[k_, c_ * 128:(c_ + 1) * 128].rearrange("(p o) -> p o", o=1)), s_setup, writes=[B_const])
        fw.dma("sp", _L_dma_start(out=bias36[:, 0:4], in_=b_rg.partition_broadcast(128)), s_setup, writes=[B_const])
        fw.dma("sp", _L_dma_start(out=bias36[:, 4:36], in_=b_re.partition_broadcast(128)), s_setup, writes=[B_const])

        a0 = Alloc(R0, 98304)
        w_in_bf = a0(8 * DIN, BF16, "p (c n) -> p c n", c=8)
        Qaug = Alloc(R0 + 49152, 32768)(8 * NOWN, BF16, "p (h n) -> p h n", h=8)
        yconvT = Alloc(R0 + 81920, 16384)(4 * NOWN, BF16, "p (c n) -> p c n", c=4)
        B_win = [Buf("win%d" % c) for c in range(8)]
        B_Q = [Buf("Q%d" % i) for i in range(NSLOT)]
        B_yc = [Buf("yc%d" % i) for i in range(NSLOT)]

        a1 = Alloc(R1, 59392)
        wst = [a1(DIN, F32) for _ in range(2)]
        B_wst = [Buf("wst%d" % i) for i in range(2)]
        s_wst = [fw.dsem("wst%d" % i) for i in range(2)]
        B_z = Buf("ztile")
        s_z = fw.dsem("zfill")
        B_XS = Buf("XS")

        fw.op("pool", _L_memset(ztile, 0.0), writes=[B_z])
        XSv = XS.rearrange("(t p) d -> t p d", p=128)
        for c in range(8):
            sl = c % 2
            fw.dma("sp", _L_dma_start(out=wst[sl], in_=w_in[c * 128:(c + 1) * 128, :]), s_wst[sl], writes=[B_wst[sl]])
            for k, eng in enumerate(("pool", "act", "dve")):
                o, i_ = w_in_bf[:, c, k * 1024:(k + 1) * 1024], wst[sl][:, k * 1024:(k + 1) * 1024]
                fw.op(eng, copy_op(eng, o, i_), reads=[B_wst[sl]], writes=[B_win[c]])
        fw.barrier()

        a1 = Alloc(R1, 59392)
        xs = [a1(8 * 512, F32, "p (c n) -> p c n", c=8) for _ in range(2)]
        xT = [a1(8 * 512, BF16, "p (c n) -> p c n", c=8) for _ in range(2)]
        KTst = [a1(4 * 512, BF16, "p (c n) -> p c n", c=4) for _ in range(2)]
        a2 = Alloc(R2, 20480)
        Vst = [a2(8 * 4 * 65, BF16, "p (h k e) -> p h k e", h=8, k=4) for _ in range(2)]
        B_xs = [Buf("xs%d" % i) for i in range(2)]
        B_xT = [[Buf("xT%d_%d" % (i, g)) for g in range(3)] for i in range(2)]
        XG = ((("act", 0, 3), ("dve", 3, 6), ("pool", 6, 8)))

        def xg(c):
            return min(c // 3, 2)
        B_KTst = [Buf("KTst%d" % i) for i in range(2)]
        B_Vst = [Buf("Vst%d" % i) for i in range(2)]
        s_xs = [fw.dsem("xs%d" % i) for i in range(2)]
        s_kst = [fw.dsem("kst%d" % i) for i in range(2)]
        s_vst = [fw.dsem("vst%d" % i) for i in range(2)]
        B_KT = Buf("KT")
        B_VV = Buf("VV")
        for i in range(2):
            fw.op("pool", _L_memset(Vst[i], 1.0), writes=[B_Vst[i]])
        fw.op("pool", _L_memset(km, 0.0), writes=[B_km])
        fw.op("pool", _L_memset(Qaug[64:128], 0.0), writes=B_Q)
        ZF_PER = (NSL // 128 + NCH - 1) // NCH

        KTv = KT.rearrange("(c p) n -> p c n", p=128)
        VVv = VV.rearrange("h p k e -> p h k e")
        xfTv = xfT.rearrange("(c p) n -> p c n", p=128)

        def load_chunk(t):
            sl = t % 2
            fw.dma("sp", _L_dma_start(out=xs[sl], in_=xfTv[:, :, t * 512:(t + 1) * 512]), s_xs[sl], writes=[B_xs[sl]])

        ev = [0]
        load_chunk(0)
        for t in range(NCH):
            sl = t % 2
            if t + 1 < NCH:
                load_chunk(t + 1)
            for zt in range(t * ZF_PER, min((t + 1) * ZF_PER, NSL // 128)):
                fw.dma("act", _L_dma_start(out=XSv[zt], in_=ztile), s_z, reads=[B_z], writes=[B_XS])
            for g, (eng, c0, c1) in enumerate(XG):
                fw.op(eng, copy_op(eng, xT[sl][:, c0:c1, :], xs[sl][:, c0:c1, :]), reads=[B_xs[sl]], writes=[B_xT[sl][g]])
            for pr in range(4):
                bk = 2 + pr % 2
                for c in range(8):
                    fw.op("pe", _L_matmul(psf[bk][:, :], lhsT=w_in_bf[:, c, 2048 + pr * 128:2048 + (pr + 1) * 128], rhs=xT[sl][:, c, :], start=(c == 0), stop=(c == 7)), reads=[B_xT[sl][xg(c)], B_win[c]], writes=[PF[bk]])
                fw.op("act", copy_op("act", KTst[sl][:, pr, :], psf[bk][:, :]), reads=[PF[bk]], writes=[B_KTst[sl]])
                fw.op("dve", _L_tensor_reduce(out=km[:, pr, 2 * t:2 * t + 2], in_=psf[bk][:, :].rearrange("p (a b) -> p a b", a=2), axis=AX.X, op=ALU.add), reads=[PF[bk]], writes=[B_km])
            fw.dma("sp", _L_dma_start(out=KTv[:, :, t * 512:(t + 1) * 512], in_=KTst[sl]), s_kst[sl], reads=[B_KTst[sl]], writes=[])
            for q in range(4):
                bk = 4 + q % 2
                for c in range(8):
                    fw.op("pe", _L_matmul(psf[bk][:, :], lhsT=xT[sl][:, c, q * 128:(q + 1) * 128], rhs=w_in_bf[:, c, 2560:3072], start=(c == 0), stop=(c == 7)), reads=[B_xT[sl][xg(c)], B_win[c]], writes=[PF[bk]])
                eng = alt(ev[0]); ev[0] += 1
                fw.op(eng, copy_op(eng, Vst[sl][:, :, q, 0:64], psf[bk][:, :].rearrange("p (h e) -> p h e", h=8)), reads=[PF[bk]], writes=[B_Vst[sl]])
            fw.dma("sp", _L_dma_start(out=VVv[:, :, 4 * t:4 * t + 4, :], in_=Vst[sl]), s_vst[sl], reads=[B_Vst[sl]], writes=[])
        fw.barrier()

        a1 = Alloc(R1, 59392)
        xos = [a1(8 * 258, F32, "p (c n) -> p c n", c=8) for _ in range(2)]
        xTo = [a1(8 * 258, BF16, "p (c n) -> p c n", c=8) for _ in range(2)]
        KTst2 = [a1(4 * 256, BF16, "p (c n) -> p c n", c=4) for _ in range(2)]
        Vst2 = [a1(8 * 2 * 65, BF16, "p (h k e) -> p h k e", h=8, k=2) for _ in range(2)]
        ccs = [a1(258, F32) for _ in range(2)]
        uu = [a1(258, F32) for _ in range(2)]
        tt_ = [a1(256, F32) for _ in range(2)]
        B_xos = [Buf("xos%d" % i) for i in range(2)]
        B_xTo = [[Buf("xTo%d_%d" % (i, g)) for g in range(3)] for i in range(2)]
        B_K2 = [Buf("K2_%d" % i) for i in range(2)]
        B_V2 = [Buf("V2_%d" % i) for i in range(2)]
        B_cc = [Buf("cc%d" % i) for i in range(2)]
        B_uu = [Buf("uu%d" % i) for i in range(2)]
        B_tt = [Buf("tt%d" % i) for i in range(2)]
        s_xos = [fw.dsem("xos%d" % i) for i in range(2)]
        s_k2 = [fw.dsem("k2_%d" % i) for i in range(2)]
        s_v2 = [fw.dsem("v2_%d" % i) for i in range(2)]
        for i in range(2):
            fw.op("pool", _L_memset(Vst2[i], 1.0), writes=[B_V2[i]])

        def load_own(i):
            sl = i % 2
            fw.dma("sp", _L_dma_start(out=xos[sl], in_=xoT[i].rearrange("(c p) n -> p c n", p=128)), s_xos[sl], writes=[B_xos[sl]])

        load_own(0)
        cvi = [0]
        for i in range(NSLOT):
            sl = i % 2
            if i + 1 < NSLOT:
                load_own(i + 1)
            c0 = i * 256
            for g, (eng, c0_, c1_) in enumerate(XG):
                fw.op(eng, copy_op(eng, xTo[sl][:, c0_:c1_, :], xos[sl][:, c0_:c1_, :]), reads=[B_xos[sl]], writes=[B_xTo[sl][g]])
            for hp in range(4):
                bk = 2 + hp % 2
                for hh in range(2):
                    h = 2 * hp + hh
                    for c in range(8):
                        fw.op("pe", _L_matmul(psf[bk][0:64, hh * 256:(hh + 1) * 256], lhsT=w_in_bf[:, c, 1536 + h * 64:1536 + (h + 1) * 64], rhs=xTo[sl][:, c, 2:258], start=(c == 0), stop=(c == 7)), reads=[B_xTo[sl][xg(c)], B_win[c]], writes=[PF[bk]])
                eng = alt(ev[0]); ev[0] += 1
                fw.op(eng, copy_op(eng, Qaug[0:64, 2 * hp:2 * hp + 2, c0:c0 + 256], psf[bk][0:64, :].rearrange("p (a b) -> p a b", a=2)), reads=[PF[bk]], writes=[B_Q[i]])
            for pp in range(2):
                bk = 4 + pp % 2
                for q in range(2):
                    pr = 2 * pp + q
                    for c in range(8):
                        fw.op("pe", _L_matmul(psf[bk][:, q * 256:(q + 1) * 256], lhsT=w_in_bf[:, c, 2048 + pr * 128:2048 + (pr + 1) * 128], rhs=xTo[sl][:, c, 2:258], start=(c == 0), stop=(c == 7)), reads=[B_xTo[sl][xg(c)], B_win[c]], writes=[PF[bk]])
                eng = alt(ev[0]); ev[0] += 1
                fw.op(eng, copy_op(eng, KTst2[sl][:, 2 * pp:2 * pp + 2, :], psf[bk][:, :].rearrange("p (a b) -> p a b", a=2)), reads=[PF[bk]], writes=[B_K2[sl]])
            fw.dma("sp", _L_dma_start(out=KTv[:, :, S + c0:S + c0 + 256], in_=KTst2[sl]), s_k2[sl], reads=[B_K2[sl]], writes=[])
            for q in range(2):
                bk = 2 + q % 2
                for c in range(8):
                    fw.op("pe", _L_matmul(psf[bk][:, :], lhsT=xTo[sl][:, c, 2 + q * 128:2 + (q + 1) * 128], rhs=w_in_bf[:, c, 2560:3072], start=(c == 0), stop=(c == 7)), reads=[B_xTo[sl][xg(c)], B_win[c]], writes=[PF[bk]])
                eng = alt(ev[0]); ev[0] += 1
                fw.op(eng, copy_op(eng, Vst2[sl][:, :, q, 0:64], psf[bk][:, :].rearrange("p (h e) -> p h e", h=8)), reads=[PF[bk]], writes=[B_V2[sl]])
            fw.dma("sp", _L_dma_start(out=VVv[:, :, S // 128 + 2 * i:S // 128 + 2 * i + 2, :], in_=Vst2[sl]), s_v2[sl], reads=[B_V2[sl]], writes=[])
            for cc in range(4):
                k2 = cvi[0] % 2
                cvi[0] += 1
                banks = (0, 1, 4) if cc % 2 == 0 else (5, 2, 3)
                specs = ((banks[0], 0 + cc * 128, 2, 256), (banks[1], 512 + cc * 128, 0, 258), (banks[2], 1024 + cc * 128, 0, 258))
                for (bk, col, o0, n) in specs:
                    for c in range(8):
                        fw.op("pe", _L_matmul(psf[bk][:, 0:n], lhsT=w_in_bf[:, c, col:col + 128], rhs=xTo[sl][:, c, o0:o0 + n], start=(c == 0), stop=(c == 7)), reads=[B_xTo[sl][xg(c)], B_win[c]], writes=[PF[bk]])
                bcb, bcc, bch = banks
                fw.op("act", copy_op("act", ccs[k2], psf[bcc][:, 0:258]), reads=[PF[bcc]], writes=[B_cc[k2]])
                fw.op("dve", _L_tensor_tensor(out=uu[k2], in0=ccs[k2], in1=psf[bch][:, 0:258], op=ALU.mult), reads=[B_cc[k2], PF[bch]], writes=[B_uu[k2]])
                fw.op("dve", _L_tensor_scalar(out=tt_[k2], in0=uu[k2][:, 2:258], scalar1=wc[:, cc, 2:3], scalar2=None, op0=ALU.mult), reads=[B_uu[k2], B_const], writes=[B_tt[k2]])
                fw.op("dve", _L_scalar_tensor_tensor(out=tt_[k2], in0=uu[k2][:, 1:257], scalar=wc[:, cc, 1:2], in1=tt_[k2], op0=ALU.mult, op1=ALU.add), reads=[B_uu[k2], B_const, B_tt[k2]], writes=[B_tt[k2]])
                fw.op("dve", _L_scalar_tensor_tensor(out=tt_[k2], in0=uu[k2][:, 0:256], scalar=wc[:, cc, 0:1], in1=tt_[k2], op0=ALU.mult, op1=ALU.add), reads=[B_uu[k2], B_const, B_tt[k2]], writes=[B_tt[k2]])
                fw.op("dve", _L_tensor_tensor(out=yconvT[:, cc, c0:c0 + 256], in0=tt_[k2], in1=psf[bcb][:, 0:256], op=ALU.mult), reads=[B_tt[k2], PF[bcb]], writes=[B_yc[i]])
        fw.barrier()

        a2 = Alloc(R2, 20480)
        nm1 = a2(NSLOT * 256, F32, "p (i n) -> p i n", i=NSLOT)
        nm2 = a2(NSLOT * 256, F32, "p (i n) -> p i n", i=NSLOT)
        gm = [a2(256, F32, "p (h n) -> p h n", h=8) for _ in range(2)]
        m8 = [a2(64, F32, "p (h n) -> p h n", h=8) for _ in range(2)]
        a1 = Alloc(R1, 59392)
        bpad = [a1(8 * 128, BF16, "p (h n) -> p h n", h=8) for _ in range(2)]
        tA3 = [a1(256, F32, "p (h n) -> p h n", h=8) for _ in range(2)]
        tB3 = [a1(256, F32, "p (h n) -> p h n", h=8) for _ in range(2)]
        mm3 = [a1(24, F32, "p (r h) -> p r h", r=3) for _ in range(2)]
        B_nm = Buf("nm")
        B_gm = [Buf("gm%d" % i) for i in range(2)]
        B_m8 = [Buf("m8%d" % i) for i in range(2)]
        B_bp = [Buf("bp%d" % i) for i in range(2)]
        s_nm = fw.dsem("nm")
        s_kmh = fw.dsem("kmh")
        fw.dma("sp", _L_dma_start(out=nm1.rearrange("p i n -> p (i n)"), in_=nm1_d.partition_broadcast(128)), s_nm, writes=[B_nm])
        fw.dma("sp", _L_dma_start(out=nm2.rearrange("p i n -> p (i n)"), in_=nm2_d.partition_broadcast(128)), s_nm, writes=[B_nm])
        for i in range(2):
            fw.op("pool", _L_memset(bpad[i], 0.0), writes=[B_bp[i]])
        aB0 = Alloc(R0, 49152)
        KTaug = [aB0(TOT, BF16) for _ in range(2)]
        B_KTa = [Buf("KTa%d" % i) for i in range(2)]
        for s_ in range(2):
            fw.op("pool", _L_memset(KTaug[s_][64:128, :], 0.0), writes=[B_KTa[s_]])
            fw.op("pool", _L_memset(KTaug[s_][64:96, 0:S], 1.0), writes=[B_KTa[s_]])
            fw.op("pool", _L_affine_select(out=KTaug[s_][64:96, 0:S], in_=KTaug[s_][64:96, 0:S], pattern=[[1, S]], compare_op=ALU.is_ge, fill=0.0, base=0, channel_multiplier=-256), reads=[B_KTa[s_]], writes=[B_KTa[s_]])
            fw.op("pool", _L_affine_select(out=KTaug[s_][64:96, 0:S], in_=KTaug[s_][64:96, 0:S], pattern=[[-1, S]], compare_op=ALU.is_ge, fill=0.0, base=255, channel_multiplier=256), reads=[B_KTa[s_]], writes=[B_KTa[s_]])
        fw.op("dve", _L_tensor_copy(out=kmb, in_=km), reads=[B_km], writes=[B_km])
        kmh_v = kmh.rearrange("p (a b) n -> p a b n", b=2)
        fw.dma("sp", _L_dma_start(out=kmh_v[0:64, :, 0, :], in_=kmb[0:64, :, :]), s_kmh, reads=[B_km], writes=[B_kmh])
        fw.dma("sp", _L_dma_start(out=kmh_v[0:64, :, 1, :], in_=kmb[64:128, :, :]), s_kmh, reads=[B_km], writes=[B_kmh])
        for i in range(NSLOT):
            for t in range(2):
                k2 = (2 * i + t) % 2
                q0 = i * 256 + t * 128
                gb = k2
                for h in range(8):
                    fw.op("pe", _L_matmul(psf[gb][:, h * 32:(h + 1) * 32], lhsT=Qaug[0:64, h, q0:q0 + 128], rhs=kmh[0:64, h, :], start=True, stop=True), reads=[B_Q[i], B_kmh], writes=[PF[gb]])
                fw.op("dve", _L_tensor_tensor(out=gm[k2].rearrange("p h n -> p (h n)"), in0=psf[gb][:, 0:256], in1=nm1[:, i, :], op=ALU.add), reads=[PF[gb], B_nm], writes=[B_gm[k2]])
                G, A_, B_ = gm[k2], tA3[k2], tB3[k2]
                rd = [B_gm[k2], B_m8[k2]]
                wr_ = [B_m8[k2]]

                def bc(r):
                    return mm3[k2][:, r, :].unsqueeze(2).to_broadcast([128, 8, 32])
                fw.op("dve", _L_tensor_reduce(out=mm3[k2][:, 0, :], in_=G, axis=AX.X, op=ALU.max), reads=rd, writes=wr_)
                fw.op("dve", _L_tensor_tensor(out=A_, in0=G, in1=bc(0), op=ALU.is_ge), reads=rd, writes=wr_)
                fw.op("dve", _L_scalar_tensor_tensor(out=B_, in0=A_, scalar=-1.0e9, in1=G, op0=ALU.mult, op1=ALU.add), reads=rd, writes=wr_)
                fw.op("dve", _L_tensor_reduce(out=mm3[k2][:, 1, :], in_=B_, axis=AX.X, op=ALU.max), reads=rd, writes=wr_)
                fw.op("dve", _L_tensor_tensor(out=A_, in0=B_, in1=bc(1), op=ALU.is_ge), reads=rd, writes=wr_)
                fw.op("dve", _L_scalar_tensor_tensor(out=B_, in0=A_, scalar=-1.0e9, in1=B_, op0=ALU.mult, op1=ALU.add), reads=rd, writes=wr_)
                fw.op("dve", _L_tensor_reduce(out=mm3[k2][:, 2, :], in_=B_, axis=AX.X, op=ALU.max), reads=rd, writes=wr_)
                fw.op("dve", _L_tensor_tensor(out=A_, in0=G, in1=bc(2), op=ALU.is_lt), reads=rd, writes=wr_)
                fw.op("dve", _L_scalar_tensor_tensor(out=bpad[k2][:, :, 64:96], in0=A_, scalar=NEG, in1=nm2[:, i, :].rearrange("p (h n) -> p h n", h=8), op0=ALU.mult, op1=ALU.add), reads=rd + [B_nm], writes=[B_bp[k2]])
                pb = k2
                for h in range(8):
                    fw.op("pe", _L_transpose(out=psb[pb][:, h * 128:(h + 1) * 128], in_=bpad[k2][:, h, :], identity=ident_b), reads=[B_bp[k2], B_const], writes=[PB[pb]])
                fw.op("act", copy_op("act", Qaug[64:96, :, q0:q0 + 128], psb[pb][64:96, :].rearrange("p (h n) -> p h n", h=8)), reads=[PB[pb]], writes=[B_Q[i]])
        fw.barrier()

        if stop_after == "A":
            pass

        aB1 = Alloc(R1, 59392)
        yattnT = aB1(8 * NOWN, BF16, "p (h n) -> p h n", h=8)
        aB1.off = 32768
        Vh = [aB1(NKT * 65, BF16, "p (k e) -> p k e", e=65) for _ in range(2)]
        Pt = [aB1(1024, BF16) for _ in range(2)]
        aB2 = Alloc(R2, 20480)
        rcp = [aB2(256, F32) for _ in range(2)]
        bcs = [aB2(256, F32) for _ in range(2)]
        B_Vh = [Buf("Vh%d" % i) for i in range(2)]
        B_Pt = [Buf("Pt%d" % i) for i in range(2)]
        B_rcp = [Buf("rcp%d" % i) for i in range(2)]
        B_bcs = [Buf("bcs%d" % i) for i in range(2)]
        B_ya = [[Buf("ya%d_%d" % (h, i)) for i in range(NSLOT)] for h in range(8)]
        s_kta = [fw.dsem("kta%d" % i) for i in range(2)]
        s_vh = [fw.dsem("vh%d" % i) for i in range(2)]
        KTh = KT.rearrange("(h d) n -> h d n", d=64)

        def load_head(h):
            s_ = h % 2
            fw.dma("sp", _L_dma_start(out=KTaug[s_][0:64, :], in_=KTh[h]), s_kta[s_], reads=[B_KT], writes=[B_KTa[s_]])
            fw.dma("sp", _L_dma_start(out=Vh[s_], in_=VV[h]), s_vh[s_], reads=[B_VV], writes=[B_Vh[s_]])

        load_head(0)
        epi_pending = []
        sb_i = [0]
        ob_i = [0]
        pt_i = [0]
        for h in range(8):
            s_ = h % 2
            if h + 1 < 8:
                load_head(h + 1)
            for i in range(NSLOT):
                q0 = i * 256
                units = [256 * n for n in range(4 * i + 3)] + [S + q0]
                nu = len(units)
                ob = 4 + ob_i[0] % 2
                ob_i[0] += 1
                ndu = nu // 2

                def qk2(du):
                    j = sb_i[0] % 2
                    sb_i[0] += 1
                    for k in range(2):
                        u = 2 * du + k
                        kc = units[u]
                        own = (u == nu - 1)
                        for t in range(2):
                            o_ap = psw[j][:, k * 512 + t * 256:k * 512 + (t + 1) * 256]
                            fw.op("pe", _L_matmul(o_ap, lhsT=KTaug[s_][:, kc + t * 128:kc + (t + 1) * 128], rhs=Qaug[:, h, q0:q0 + 256], start=True, stop=(not own)), reads=[B_KTa[s_], B_Q[i]], writes=[PW[j]])
                            if own:
                                fw.op("pe", _L_matmul(o_ap, lhsT=ident_b, rhs=CBm[:, t, :], start=False, stop=True), reads=[B_const], writes=[PW[j]])
                    return j

                def expo2(j):
                    pi = pt_i[0] % 2
                    pt_i[0] += 1
                    fw.op("act", _L_activation(out=Pt[pi], in_=psw[j], func=AF.Exp, scale=0.125), reads=[PW[j]], writes=[B_Pt[pi]])
                    return pi

                def pv2(du, pi):
                    for k in range(2):
                        u = 2 * du + k
                        kc = units[u]
                        for t in range(2):
                            kt = kc // 128 + t
                            fw.op("pe", _L_matmul(psf[ob][0:65, 0:256], lhsT=Vh[s_][:, kt, :], rhs=Pt[pi][:, k * 512 + t * 256:k * 512 + (t + 1) * 256], start=(u == 0 and t == 0), stop=(u == nu - 1 and t == 1)), reads=[B_Vh[s_], B_Pt[pi]], writes=[PF[ob]])

                js = [qk2(0)]
                for du in range(ndu):
                    if du + 1 < ndu:
                        js.append(qk2(du + 1))
                    pi = expo2(js[du])
                    pv2(du, pi)
                    if du == 0 and epi_pending:
                        epi_pending.pop()()
                k2 = ob_i[0] % 2
                fw.op("dve", _L_reciprocal(out=rcp[k2][64:65, :], in_=psf[ob][64:65, 0:256]), reads=[PF[ob]], writes=[B_rcp[k2]])

                def epilogue(k2=k2, ob=ob, h=h, i=i, q0=q0):
                    fw.op("pe", _L_matmul(pse[0:64, 0:256], lhsT=ones_f[64:65, 0:64], rhs=rcp[k2][64:65, :], start=True, stop=True), reads=[B_rcp[k2], B_const], writes=[PB[0]])
                    fw.op("dve", copy_op("dve", bcs[k2][0:64, :], pse[0:64, 0:256]), reads=[PB[0]], writes=[B_bcs[k2]])
                    fw.op("dve", _L_tensor_tensor(out=yattnT[0:64, h, q0:q0 + 256], in0=psf[ob][0:64, 0:256], in1=bcs[k2][0:64, :], op=ALU.mult), reads=[PF[ob], B_bcs[k2]], writes=[B_ya[h][i]])
                epi_pending.append(epilogue)
        while epi_pending:
            epi_pending.pop()()
        fw.barrier()

        aC = Alloc(R0, 81920)
        wo_c = aC(4 * D, BF16, "p (c n) -> p c n", c=4)
        wo_a = aC(8 * D, BF16, "p (c n) -> p c n", c=8)
        wpg = aC(8 * D, BF16, "p (c n) -> p c n", c=8)
        wpp = aC(2 * D, BF16, "p (c n) -> p c n", c=2)
        wr = aC(8 * 36, F32, "p (c n) -> p c n", c=8)
        lng1 = aC(D, F32)
        lnb1 = aC(D, F32)
        cst = [aC(D, F32) for _ in range(4)]
        B_wC = Buf("wC")
        B_cst = [Buf("cst%d" % i) for i in range(4)]
        s_cst = [fw.dsem("cst%d" % i) for i in range(4)]
        s_wC = fw.dsem("wCs")
        fw.dma("sp", _L_dma_start(out=lng1, in_=ln1_g.partition_broadcast(128)), s_wC, writes=[B_wC])
        fw.dma("sp", _L_dma_start(out=lnb1, in_=ln1_b.partition_broadcast(128)), s_wC, writes=[B_wC])
        with nc.allow_non_contiguous_dma(reason="small router weights"):
            fw.dma("sp", _L_dma_start(out=wr[:, :, 0:4], in_=w_rg.rearrange("(c p) n -> p c n", p=128)), s_wC, writes=[B_wC])
            fw.dma("sp", _L_dma_start(out=wr[:, :, 4:36], in_=w_re.rearrange("(c p) n -> p c n", p=128)), s_wC, writes=[B_wC])
        pieces = []
        for c in range(4):
            pieces.append((w_out[c * 128:(c + 1) * 128, :], wo_c[:, c, :], 128))
        for hh in range(8):
            pieces.append((w_out[512 + hh * 64:512 + (hh + 1) * 64, :], wo_a[0:64, hh, :], 64))
        for c in range(8):
            pieces.append((w_pg[c * 128:(c + 1) * 128, :], wpg[:, c, :], 128))
        for c in range(2):
            pieces.append((w_pp[c * 128:(c + 1) * 128, :], wpp[:, c, :], 128))
        for k, (src, dst, npart) in enumerate(pieces):
            sl = k % 4
            fw.dma("sp", _L_dma_start(out=cst[sl][0:npart, :], in_=src), s_cst[sl], writes=[B_cst[sl]])
            eng = ("pool", "act", "dve")[k % 3]
            fw.op(eng, copy_op(eng, dst, cst[sl][0:npart, :]), reads=[B_cst[sl]], writes=[B_wC])

        aC1 = Alloc(R1 + 32768, 59392 - 32768)
        aC2 = Alloc(R2, 20480)
        xr = [aC1(D, F32) for _ in range(2)]
        h1 = [aC1(D, F32) for _ in range(2)]
        x1b = [aC1(D, BF16) for _ in range(2)]
        x1Tb = aC1(8 * 128, BF16, "p (c n) -> p c n", c=8)
        pTb = aC1(2 * 128, BF16, "p (c n) -> p c n", c=2)
        prow = [aC1(PLE, F32) for _ in range(2)]
        x1Tf = aC2(8 * 128, F32, "p (c n) -> p c n", c=8)
        sgt = aC2(D, F32)
        acc = [aC2(D, F32) for _ in range(2)]
        rs = aC2(512, F32)
        B_xr = [Buf("xr%d" % i) for i in range(2)]
        B_h1 = [Buf("h1_%d" % i) for i in range(2)]
        B_x1b = [Buf("x1b%d" % i) for i in range(2)]
        B_x1Tb = Buf("x1Tb")
        B_x1Tf = Buf("x1Tf")
        B_pTb = Buf("pTb")
        B_prow = [Buf("prow%d" % i) for i in range(2)]
        B_sgt = Buf("sgt")
        B_acc = [Buf("acc%d" % i) for i in range(2)]
        B_rs = Buf("rs")
        s_xr = [fw.dsem("xr%d" % i) for i in range(2)]
        s_pr = [fw.dsem("pr%d" % i) for i in range(2)]
        s_acc = [fw.dsem("accst%d" % i) for i in range(2)]
        s_sc = [fw.dsem("scat%d" % i) for i in range(2)]
        s_dbg = fw.dsem("dbg")
        B_ACCD = [Buf("ACCD%d" % t) for t in range(NT)]
        B_dbg = Buf("dbg")
        L36 = rs[:, 0:36]
        gmax = rs[:, 36:37]
        gsum = rs[:, 37:38]
        gp = rs[:, 38:39]
        oh = rs[:, 40:44]
        pen = rs[:, 44:48]
        junk4 = rs[:, 48:52]
        lem = rs[:, 64:96]
        r8 = rs[:, 96:104]
        sel1 = rs[:, 128:160]
        sel2 = rs[:, 160:192]
        selb = rs[:, 192:224]
        tmpa = rs[:, 224:256]
        tmpb = rs[:, 256:288]
        dd = rs[:, 288:289]
        rr = rs[:, 289:290]
        den = rs[:, 290:291]
        slf = rs[:, 292:294]
        stats = rs[:, 300:312]
        mv = rs[:, 312:314]
        rstd = rs[:, 314:315]

        dmy = rs[:, 320:324]
        B_dmy = Buf("dmy")
        fw.op("pool", _L_memset(rs[:, 316:324], 0.0), writes=[B_dmy])

        def prefetch_table(func):
            fw.op("act", _L_activation(out=dmy[:, 2:4], in_=dmy[:, 0:2], func=func), reads=[], writes=[B_dmy])

        def layer_norm(eng_list, h_ap, B_h, g_ap, b_ap, B_gb):
            fw.op("dve", _L_bn_stats(out=stats[:, 0:6], in_=h_ap[:, 0:512]), reads=[B_h], writes=[B_rs])
            fw.op("dve", _L_bn_stats(out=stats[:, 6:12], in_=h_ap[:, 512:1024]), reads=[B_h], writes=[B_rs])
            fw.op("dve", _L_bn_aggr(out=mv, in_=stats), reads=[B_rs], writes=[B_rs])
            fw.op("act", _L_activation(out=rstd, in_=mv[:, 1:2], func=AF.Sqrt, bias=EPS, scale=1.0), reads=[B_rs], writes=[B_rs])
            prefetch_table(AF.Exp)
            fw.op("dve", _L_reciprocal(out=rstd, in_=rstd), reads=[B_rs], writes=[B_rs])
            fw.op("dve", _L_tensor_scalar(out=h_ap, in0=h_ap, scalar1=mv[:, 0:1], scalar2=rstd, op0=ALU.subtract, op1=ALU.mult), reads=[B_h, B_rs], writes=[B_h])
            fw.op("dve", _L_tensor_tensor(out=h_ap, in0=h_ap, in1=g_ap, op=ALU.mult), reads=[B_h, B_gb], writes=[B_h])
            fw.op("dve", _L_tensor_tensor(out=h_ap, in0=h_ap, in1=b_ap, op=ALU.add), reads=[B_h, B_gb], writes=[B_h])

        def load_tok(t):
            k2 = t % 2
            i, hf = t // 2, t % 2
            fw.dma("sp", _L_dma_start(out=xr[k2], in_=xo[i, 2 + hf * 128:2 + (hf + 1) * 128, :]), s_xr[k2], writes=[B_xr[k2]])
            fw.dma("sp", _L_dma_start(out=prow[k2], in_=po[i, hf * 128:(hf + 1) * 128, :]), s_pr[k2], writes=[B_prow[k2]])

        def out_proj(t):
            i = t // 2
            tk = t * 128
            for half in range(2):
                bk = 2 + half
                n_mm = 12
                m = 0
                for cc in range(4):
                    fw.op("pe", _L_matmul(psf[bk][:, :], lhsT=yconvT[:, cc, tk:tk + 128], rhs=wo_c[:, cc, half * 512:(half + 1) * 512], start=(m == 0), stop=False), reads=[B_yc[i], B_wC], writes=[PF[bk]])
                    m += 1
                for hh in range(8):
                    fw.op("pe", _L_matmul(psf[bk][:, :], lhsT=yattnT[0:64, hh, tk:tk + 128], rhs=wo_a[0:64, hh, half * 512:(half + 1) * 512], start=False, stop=(m == n_mm - 1)), reads=[B_ya[hh][i], B_wC], writes=[PF[bk]])
                    m += 1

        load_tok(0)
        out_proj(0)
        prefetch_table(AF.Sqrt)
        for t in range(NT):
            k2 = t % 2
            i, hf = t // 2, t % 2
            tk = t * 128
            if t + 1 < NT:
                load_tok(t + 1)
            for half in range(2):
                bk = 2 + half
                fw.op("dve", _L_scalar_tensor_tensor(out=h1[k2][:, half * 512:(half + 1) * 512], in0=xr[k2][:, half * 512:(half + 1) * 512], scalar=ALPHA, in1=psf[bk][:, :], op0=ALU.mult, op1=ALU.add), reads=[B_xr[k2], PF[bk]], writes=[B_h1[k2]])
            if dbg:
                fw.dma("sp", _L_dma_start(out=dbg_mix[tk:tk + 128, :], in_=h1[k2]), s_dbg, reads=[B_h1[k2]], writes=[B_dbg])
            layer_norm(None, h1[k2], B_h1[k2], lng1, lnb1, B_wC)
            if dbg:
                fw.dma("sp", _L_dma_start(out=dbg_x1[tk:tk + 128, :], in_=h1[k2]), s_dbg, reads=[B_h1[k2]], writes=[B_dbg])
            for g in range(2):
                bk = 2 + g
                for q in range(4):
                    c = 4 * g + q
                    fw.op("pe", _L_transpose(out=psf[bk][:, q * 128:(q + 1) * 128], in_=h1[k2][:, c * 128:(c + 1) * 128], identity=ident_f), reads=[B_h1[k2], B_ident], writes=[PF[bk]])
                fw.op("act", copy_op("act", x1Tf[:, 4 * g:4 * g + 4, :], psf[bk][:, :].rearrange("p (a b) -> p a b", a=4)), reads=[PF[bk]], writes=[B_x1Tf])
                fw.op("dve", copy_op("dve", x1Tb[:, 4 * g:4 * g + 4, :], psf[bk][:, :].rearrange("p (a b) -> p a b", a=4)), reads=[PF[bk]], writes=[B_x1Tb])
            fw.op("act", copy_op("act", x1b[k2], h1[k2]), reads=[B_h1[k2]], writes=[B_x1b[k2]])
            for c in range(8):
                fw.op("pe", _L_matmul(psf[4][:, 0:36], lhsT=x1Tf[:, c, :], rhs=wr[:, c, :], start=(c == 0), stop=(c == 7)), reads=[B_x1Tf, B_wC], writes=[PF[4]])
            for half in range(2):
                bk = half
                for c in range(8):
                    fw.op("pe", _L_matmul(psf[bk][:, :], lhsT=x1Tb[:, c, :], rhs=wpg[:, c, half * 512:(half + 1) * 512], start=(c == 0), stop=(c == 7)), reads=[B_x1Tb, B_wC], writes=[PF[bk]])
            fw.op("dve", _L_tensor_tensor(out=L36, in0=psf[4][:, 0:36], in1=bias36, op=ALU.add), reads=[PF[4], B_const], writes=[B_rs])
            fw.op("dve", _L_tensor_reduce(out=gmax, in_=rs[:, 0:4], axis=AX.X, op=ALU.max), reads=[B_rs], writes=[B_rs])
            fw.op("dve", _L_tensor_scalar(out=dd, in0=gmax, scalar1=-1.0, scalar2=None, op0=ALU.mult), reads=[B_rs], writes=[B_rs])
            fw.op("act", _L_activation(out=junk4, in_=rs[:, 0:4], func=AF.Exp, bias=dd, scale=1.0, accum_out=gsum), reads=[B_rs], writes=[B_rs])
            fw.op("dve", _L_reciprocal(out=gp, in_=gsum), reads=[B_rs], writes=[B_rs])
            fw.op("dve", _L_tensor_scalar(out=oh, in0=rs[:, 0:4], scalar1=gmax, scalar2=None, op0=ALU.is_equal), reads=[B_rs], writes=[B_rs])
            fw.op("dve", _L_tensor_scalar(out=pen, in0=oh, scalar1=1.0, scalar2=1.0e30, op0=ALU.subtract, op1=ALU.mult), reads=[B_rs], writes=[B_rs])
            for g in range(4):
                fw.op("dve", _L_tensor_scalar(out=lem[:, 8 * g:8 * g + 8], in0=rs[:, 4 + 8 * g:12 + 8 * g], scalar1=pen[:, g:g + 1], scalar2=None, op0=ALU.add), reads=[B_rs], writes=[B_rs])
            fw.op("dve", _L_max(out=r8, in_=lem), reads=[B_rs], writes=[B_rs])
            fw.op("dve", _L_tensor_scalar(out=sel1, in0=lem, scalar1=r8[:, 0:1], scalar2=None, op0=ALU.is_equal), reads=[B_rs], writes=[B_rs])
            fw.op("dve", _L_tensor_scalar(out=sel2, in0=lem, scalar1=r8[:, 1:2], scalar2=None, op0=ALU.is_equal), reads=[B_rs], writes=[B_rs])
            fw.op("dve", _L_tensor_tensor(out=selb, in0=sel1, in1=sel2, op=ALU.add), reads=[B_rs], writes=[B_rs])
            fw.op("dve", _L_tensor_tensor(out=dd, in0=r8[:, 1:2], in1=r8[:, 0:1], op=ALU.subtract), reads=[B_rs], writes=[B_rs])
            fw.op("act", _L_activation(out=rr, in_=dd, func=AF.Exp), reads=[B_rs], writes=[B_rs])
            prefetch_table(AF.Sigmoid)
            fw.op("dve", _L_tensor_scalar(out=den, in0=rr, scalar1=1.0, scalar2=None, op0=ALU.add), reads=[B_rs], writes=[B_rs])
            fw.op("dve", _L_reciprocal(out=den, in_=den), reads=[B_rs], writes=[B_rs])
            fw.op("dve", _L_tensor_tensor(out=w12[:, t, 0:1], in0=gp, in1=den, op=ALU.mult), reads=[B_rs], writes=[B_w12[t]])
            fw.op("dve", _L_tensor_tensor(out=w12[:, t, 1:2], in0=w12[:, t, 0:1], in1=rr, op=ALU.mult), reads=[B_rs, B_w12[t]], writes=[B_w12[t]])
            fw.op("pe", _L_matmul(psf[5][:, 0:32], lhsT=Lst, rhs=selb, start=True, stop=False), reads=[B_rs, B_const], writes=[PF[5]])
            fw.op("pe", _L_matmul(psf[5][:, 0:32], lhsT=ones_f, rhs=Spre, start=False, stop=True), reads=[B_Spre, B_const], writes=[PF[5]])
            if t + 1 < NT:
                out_proj(t + 1)
            fw.op("dve", _L_tensor_tensor(out=tmpa, in0=psf[5][:, 0:32], in1=eC, op=ALU.add), reads=[PF[5], B_const], writes=[B_rs])
            fw.op("dve", _L_tensor_scalar(out=tmpb, in0=psf[5][:, 0:32], scalar1=float(CAP), scalar2=1.0e6, op0=ALU.is_ge, op1=ALU.mult), reads=[PF[5]], writes=[B_rs])
            fw.op("dve", _L_tensor_tensor(out=tmpa, in0=tmpa, in1=tmpb, op=ALU.add), reads=[B_rs], writes=[B_rs])
            fw.op("dve", _L_tensor_tensor(out=tmpb, in0=tmpa, in1=sel1, op=ALU.mult), reads=[B_rs], writes=[B_rs])
            fw.op("dve", _L_tensor_reduce(out=slf[:, 0:1], in_=tmpb, axis=AX.X, op=ALU.add), reads=[B_rs], writes=[B_rs])
            fw.op("dve", _L_tensor_tensor(out=tmpb, in0=tmpa, in1=sel2, op=ALU.mult), reads=[B_rs], writes=[B_rs])
            fw.op("dve", _L_tensor_reduce(out=slf[:, 1:2], in_=tmpb, axis=AX.X, op=ALU.add), reads=[B_rs], writes=[B_rs])
            fw.op("dve", _L_tensor_copy(out=slot_i[:, t, :], in_=slf), reads=[B_rs], writes=[B_slot[t]])
            fw.op("dve", _L_tensor_tensor(out=Spre, in0=Spre, in1=selb, op=ALU.add), reads=[B_rs, B_Spre], writes=[B_Spre])
            for k in range(2):
                fw.dma("pool", _L_indirect_dma_start(out=XS, out_offset=bass.IndirectOffsetOnAxis(ap=slot_i[:, t, k:k + 1], axis=0), in_=x1b[k2], in_offset=None, bounds_check=NSL - 1, oob_is_err=False), s_sc[k2], reads=[B_x1b[k2], B_slot[t], B_XS], writes=[B_XS])
            for half in range(2):
                fw.op("act", _L_activation(out=sgt[:, half * 512:(half + 1) * 512], in_=psf[half][:, :], func=AF.Sigmoid), reads=[PF[half]], writes=[B_sgt])
            prefetch_table(AF.Sqrt)
            for c in range(2):
                fw.op("pe", _L_transpose(out=psf[4][:, c * 128:(c + 1) * 128], in_=prow[k2][:, c * 128:(c + 1) * 128], identity=ident_f), reads=[B_prow[k2], B_ident], writes=[PF[4]])
            fw.op("act", copy_op("act", pTb, psf[4][:, 0:256].rearrange("p (a b) -> p a b", a=2)), reads=[PF[4]], writes=[B_pTb])
            for half in range(2):
                bk = half
                for c in range(2):
                    fw.op("pe", _L_matmul(psf[bk][:, :], lhsT=pTb[:, c, :], rhs=wpp[:, c, half * 512:(half + 1) * 512], start=(c == 0), stop=(c == 1)), reads=[B_pTb, B_wC], writes=[PF[bk]])
                fw.op("dve", _L_tensor_tensor(out=acc[k2][:, half * 512:(half + 1) * 512], in0=sgt[:, half * 512:(half + 1) * 512], in1=psf[bk][:, :], op=ALU.mult), reads=[B_sgt, PF[bk]], writes=[B_acc[k2]])
            fw.op("dve", _L_scalar_tensor_tensor(out=acc[k2], in0=h1[k2], scalar=ALPHA, in1=acc[k2], op0=ALU.mult, op1=ALU.add), reads=[B_h1[k2], B_acc[k2]], writes=[B_acc[k2]])
            fw.dma("sp", _L_dma_start(out=ACCD[tk:tk + 128, :], in_=acc[k2]), s_acc[k2], reads=[B_acc[k2]], writes=[B_ACCD[t]])
        fw.barrier()

        aD = Alloc(R0, 98304)
        wstg = [aD(8 * DE, F32) for _ in range(3)]
        wbf = [aD(8 * DE, BF16) for _ in range(6)]
        aD1 = Alloc(R1, 59392)
        Xe = [aD1(D, BF16) for _ in range(2)]
        XeT = [aD1(8 * 256, BF16, "p (c n) -> p c n", c=8) for _ in range(2)]
        hT = [aD1(4 * 256, BF16, "p (c n) -> p c n", c=4) for _ in range(2)]
        sgu = [aD1(256, F32) for _ in range(2)]
        Yst = [aD1(D, F32) for _ in range(2)]
        Y1 = [aD1(D, F32) for _ in range(2)]
        Y2 = [aD1(D, F32) for _ in range(2)]
        accr = [aD1(D, F32) for _ in range(3)]
        aD2 = Alloc(R2, 20480)
        lng2 = aD2(D, F32)
        lnb2 = aD2(D, F32)
        rs2 = aD2(64, F32)
        B_wstg = [Buf("wstg%d" % i) for i in range(3)]
        B_wbf = [Buf("wbf%d" % i) for i in range(6)]
        B_Xe = [Buf("Xe%d" % i) for i in range(2)]
        B_XeT = [Buf("XeT%d" % i) for i in range(2)]
        B_hT = [Buf("hT%d" % i) for i in range(2)]
        B_sgu = [Buf("sgu%d" % i) for i in range(2)]
        B_Yst = [Buf("Yst%d" % i) for i in range(2)]
        B_Y1 = [Buf("Y1_%d" % i) for i in range(2)]
        B_Y2 = [Buf("Y2_%d" % i) for i in range(2)]
        B_accr = [Buf("accr%d" % i) for i in range(3)]
        B_ln2 = Buf("ln2")
        B_YS = Buf("YS")
        s_wstg = [fw.dsem("wstg%d" % i) for i in range(3)]
        s_xe = [fw.dsem("xe%d" % i) for i in range(2)]
        s_yst = [fw.dsem("yst%d" % i) for i in range(2)]
        s_g = [fw.dsem("gath%d" % i) for i in range(2)]
        s_accr = [fw.dsem("accr%d" % i) for i in range(3)]
        s_out = [fw.dsem("out%d" % i) for i in range(3)]
        s_ln2 = fw.dsem("ln2")
        B_out = Buf("out")
        fw.dma("sp", _L_dma_start(out=lng2, in_=ln2_g.partition_broadcast(128)), s_ln2, writes=[B_ln2])
        fw.dma("sp", _L_dma_start(out=lnb2, in_=ln2_b.partition_broadcast(128)), s_ln2, writes=[B_ln2])

        mats = []
        for ex in range(NE):
            mats.append(("g", ex))
            mats.append(("u", ex))
            mats.append(("d", ex))

        CAST_ENG = ("pool", "act", "dve", "dve", "pool", "act", "dve", "act", "pool", "dve", "act", "dve")

        pending = {"act": [], "dve": []}

        def load_mat(mi):
            kind, ex = mats[mi]
            sl = mi % 3
            bs = mi % 6
            if kind in ("g", "u"):
                src = (w_gate if kind == "g" else w_up)[ex].rearrange("(p c) f -> p c f", c=8)
                dstv = wstg[sl].rearrange("p (c f) -> p c f", c=8)
            else:
                src = w_down[ex].rearrange("(c p) n -> p c n", p=128)
                dstv = wstg[sl].rearrange("p (c n) -> p c n", c=4)
            fw.dma("sp", _L_dma_start(out=dstv, in_=src), s_wstg[sl], writes=[B_wstg[sl]])
            for k in range(4):
                eng = CAST_ENG[(4 * mi + k) % 12]
                o, i_ = wbf[bs][:, k * 1024:(k + 1) * 1024], wstg[sl][:, k * 1024:(k + 1) * 1024]

                def emit(eng=eng, o=o, i_=i_, sl=sl, bs=bs):
                    fw.op(eng, copy_op(eng, o, i_), reads=[B_wstg[sl]], writes=[B_wbf[bs]])
                if eng == "pool":
                    emit()
                else:
                    pending[eng].append(emit)

        def drain(eng, n):
            for _ in range(n):
                if pending[eng]:
                    pending[eng].pop(0)()

        XSe = XS.rearrange("(e s p) d -> e s p d", s=2, p=128)
        YSe = YS.rearrange("(e s p) d -> e s p d", s=2, p=128)

        def load_xe(ex):
            for s2 in range(2):
                fw.dma("act", _L_dma_start(out=Xe[s2], in_=XSe[ex, s2]), s_xe[s2], reads=[B_XS], writes=[B_Xe[s2]])

        for mi in range(3):
            load_mat(mi)
        drain("act", 99)
        drain("dve", 99)
        load_xe(0)
        yi = [0]
        for ex in range(NE):
            k2 = ex % 2
            bb = (3 * ex) % 6
            wg = wbf[bb].rearrange("p (c f) -> p c f", c=8)
            wu = wbf[bb + 1].rearrange("p (c f) -> p c f", c=8)
            wd = wbf[bb + 2].rearrange("p (c n) -> p c n", c=4)
            if ex + 1 < NE:
                for q in range(3):
                    load_mat(3 * (ex + 1) + q)
            for s2 in range(2):
                pb = s2
                for c in range(8):
                    fw.op("pe", _L_transpose(out=psb[pb][:, c * 128:(c + 1) * 128], in_=Xe[s2].rearrange("t (p c) -> t c p", c=8)[:, c, :], identity=ident_b), reads=[B_Xe[s2], B_const], writes=[PB[pb]])
                eng = alt(s2)
                fw.op(eng, copy_op(eng, XeT[k2][:, :, s2 * 128:(s2 + 1) * 128], psb[pb][:, :].rearrange("p (c n) -> p c n", c=8)), reads=[PB[pb]], writes=[B_XeT[k2]])
            if ex + 1 < NE:
                load_xe(ex + 1)
            for fo in range(4):
                bg, bu = (0, 1) if fo % 2 == 0 else (2, 3)
                for (bk, wm, bi) in ((bg, wg, bb), (bu, wu, bb + 1)):
                    for c in range(8):
                        fw.op("pe", _L_matmul(psf[bk][:, 0:256], lhsT=wm[:, c, fo * 128:(fo + 1) * 128], rhs=XeT[k2][:, c, :], start=(c == 0), stop=(c == 7)), reads=[B_XeT[k2], B_wbf[bi]], writes=[PF[bk]])
                sq = fo % 2
                fw.op("act", _L_activation(out=sgu[sq], in_=psf[bg][:, 0:256], func=AF.Silu), reads=[PF[bg]], writes=[B_sgu[sq]])
                fw.op("dve", _L_tensor_tensor(out=hT[k2][:, fo, :], in0=sgu[sq], in1=psf[bu][:, 0:256], op=ALU.mult), reads=[B_sgu[sq], PF[bu]], writes=[B_hT[k2]])
                drain("act", 1)
                drain("dve", 1)
            for s2 in range(2):
                ys = yi[0] % 2
                yi[0] += 1
                for half in range(2):
                    bk = 4 + half
                    for fo in range(4):
                        fw.op("pe", _L_matmul(psf[bk][:, :], lhsT=hT[k2][:, fo, s2 * 128:(s2 + 1) * 128], rhs=wd[:, fo, half * 512:(half + 1) * 512], start=(fo == 0), stop=(fo == 3)), reads=[B_hT[k2], B_wbf[bb + 2]], writes=[PF[bk]])
                    eng = alt(half)
                    fw.op(eng, copy_op(eng, Yst[ys][:, half * 512:(half + 1) * 512], psf[bk][:, :]), reads=[PF[bk]], writes=[B_Yst[ys]])
                fw.dma("act", _L_dma_start(out=YSe[ex, s2], in_=Yst[ys]), s_yst[ys], reads=[B_Yst[ys]], writes=[])
            drain("act", 99)
            drain("dve", 99)

        fw.barrier()

        st2 = rs2[:, 0:12]
        mv2 = rs2[:, 12:14]
        rstd2 = rs2[:, 14:15]

        def fin_issue(t):
            k2, k3 = t % 2, t % 3
            tk = t * 128
            fw.op("pool", _L_memset(Y1[k2], 0.0), writes=[B_Y1[k2]])
            fw.op("pool", _L_memset(Y2[k2], 0.0), writes=[B_Y2[k2]])
            fw.dma("sp", _L_dma_start(out=accr[k3], in_=ACCD[tk:tk + 128, :]), s_accr[k3], reads=[B_ACCD[t]], writes=[B_accr[k3]])
            fw.dma("pool", _L_indirect_dma_start(out=Y1[k2], out_offset=None, in_=YS, in_offset=bass.IndirectOffsetOnAxis(ap=slot_i[:, t, 0:1], axis=0), bounds_check=NSL - 1, oob_is_err=False), s_g[k2], reads=[B_YS, B_slot[t]], writes=[B_Y1[k2]])
            fw.dma("pool", _L_indirect_dma_start(out=Y2[k2], out_offset=None, in_=YS, in_offset=bass.IndirectOffsetOnAxis(ap=slot_i[:, t, 1:2], axis=0), bounds_check=NSL - 1, oob_is_err=False), s_g[k2], reads=[B_YS, B_slot[t]], writes=[B_Y2[k2]])

        def fin_front(t):
            k2, k3 = t % 2, t % 3
            h_ap, B_h, B_r = accr[k3], B_accr[k3], B_rs
            fw.op("dve", _L_scalar_tensor_tensor(out=h_ap, in0=Y1[k2], scalar=w12[:, t, 0:1], in1=h_ap, op0=ALU.mult, op1=ALU.add), reads=[B_Y1[k2], B_w12[t], B_h], writes=[B_h])
            fw.op("dve", _L_scalar_tensor_tensor(out=h_ap, in0=Y2[k2], scalar=w12[:, t, 1:2], in1=h_ap, op0=ALU.mult, op1=ALU.add), reads=[B_Y2[k2], B_w12[t], B_h], writes=[B_h])
            fw.op("dve", _L_bn_stats(out=st2[:, 0:6], in_=h_ap[:, 0:512]), reads=[B_h], writes=[B_r])
            fw.op("dve", _L_bn_stats(out=st2[:, 6:12], in_=h_ap[:, 512:1024]), reads=[B_h], writes=[B_r])
            fw.op("dve", _L_bn_aggr(out=mv2, in_=st2), reads=[B_r], writes=[B_r])
            fw.op("act", _L_activation(out=rstd2, in_=mv2[:, 1:2], func=AF.Sqrt, bias=EPS, scale=1.0), reads=[B_r], writes=[B_r])
            fw.op("dve", _L_reciprocal(out=rstd2, in_=rstd2), reads=[B_r], writes=[B_r])
            fw.op("dve", _L_tensor_scalar(out=h_ap, in0=h_ap, scalar1=mv2[:, 0:1], scalar2=rstd2, op0=ALU.subtract, op1=ALU.mult), reads=[B_h, B_r], writes=[B_h])
            fw.op("dve", _L_tensor_tensor(out=h_ap, in0=h_ap, in1=lng2, op=ALU.mult), reads=[B_h, B_ln2], writes=[B_h])

        def fin_back(t):
            k3 = t % 3
            tk = t * 128
            fw.op("dve", _L_tensor_tensor(out=accr[k3], in0=accr[k3], in1=lnb2, op=ALU.add), reads=[B_accr[k3], B_ln2], writes=[B_accr[k3]])
            fw.dma("sp", _L_dma_start(out=out[tk:tk + 128, :], in_=accr[k3]), s_out[k3], reads=[B_accr[k3]], writes=[])

        fin_issue(0)
        for t in range(NT):
            if t + 1 < NT:
                fin_issue(t + 1)
            fin_front(t)
            if t >= 1:
                fin_back(t - 1)
        fin_back(NT - 1)
        fw.barrier()
        with nc.allow_non_contiguous_dma(reason="tiny strided parameter loads"):
            fw.replay()
    nc._fw_names = fw.names
    return nc


def own_blocks(r, NB):
    NSLOT = NB // 4
    js = []
    for i in range(NSLOT):
        if i < NSLOT // 2:
            js.append(r + 4 * i)
        else:
            js.append(NB - 1 - r - 4 * (NSLOT - 1 - i))
    return js


def make_in_maps(inputs, S):
    NB = S // 256
    x = np.asarray(inputs["x"], dtype=np.float32)
    p = np.asarray(inputs["p"], dtype=np.float32)
    nbatch = x.shape[0]
    shared = {
        "w_in": inputs["w_in"][0], "w_conv": inputs["w_conv"][0], "w_out": inputs["w_out"][0],
        "ln1_g": inputs["ln1_g"][0], "ln1_b": inputs["ln1_b"][0],
        "w_rg": inputs["w_router_g"][0], "b_rg": inputs["b_router_g"][0],
        "w_re": inputs["w_router_e"][0], "b_re": inputs["b_router_e"][0],
        "w_gate": inputs["w_gate"][0], "w_up": inputs["w_up"][0], "w_down": inputs["w_down"][0],
        "w_pg": inputs["w_ple_gate"][0], "w_pp": inputs["w_ple_proj"][0],
        "ln2_g": inputs["ln2_g"][0], "ln2_b": inputs["ln2_b"][0],
    }
    shared = {k: np.ascontiguousarray(np.asarray(v, dtype=np.float32)) for k, v in shared.items()}
    maps = []
    for b in range(nbatch):
        for r in range(4):
            js = own_blocks(r, NB)
            xo = np.zeros((len(js), 258, D), np.float32)
            po = np.zeros((len(js), 256, PLE), np.float32)
            nm1 = np.zeros((len(js), 8, 32), np.float32)
            nm2 = np.zeros((len(js), 8, 32), np.float32)
            for i, j in enumerate(js):
                lo = 256 * j - 2
                if lo >= 0:
                    xo[i] = x[b, lo:lo + 258]
                else:
                    xo[i, 2:] = x[b, 0:256]
                po[i] = p[0, b, 256 * j:256 * j + 256]
                nm1[i, :, j:] = NEGINF
                nm2[i, :, j:] = NEG
            m = dict(shared)
            m["xfT"] = np.ascontiguousarray(x[b].T)
            m["xoT"] = np.ascontiguousarray(xo.transpose(0, 2, 1))
            m["xo"] = xo
            m["po"] = po
            m["nm1"] = nm1.reshape(-1)
            m["nm2"] = nm2.reshape(-1)
            maps.append(m)
    return maps


_NC_CACHE = {}


def kernel(**inputs):
    x = np.asarray(inputs["x"])
    nbatch, S, _ = x.shape
    NB = S // 256
    if S not in _NC_CACHE:
        _NC_CACHE[S] = build_nc(S)
    nc = _NC_CACHE[S]
    maps = make_in_maps(inputs, S)
    res = run_bass_kernel_spmd(nc, maps, core_ids=list(range(len(maps))))
    outp = np.zeros((nbatch, S, D), np.float32)
    k = 0
    for b in range(nbatch):
        for r in range(4):
            o = np.asarray(res.results[k]["out"], dtype=np.float32)
            for i, j in enumerate(own_blocks(r, NB)):
                outp[b, 256 * j:256 * j + 256] = o[256 * i:256 * i + 256]
            k += 1
    return outp
```

```python
import numpy as np
from contextlib import ExitStack

import concourse.bass as bass
import concourse.mybir as mybir
from concourse.bass_utils import run_bass_kernel_spmd

F32 = mybir.dt.float32
BF16 = mybir.dt.bfloat16
U8 = mybir.dt.uint8
I32 = mybir.dt.int32
AF = mybir.ActivationFunctionType
ALU = mybir.AluOpType
AX = mybir.AxisListType

D = 1024
DIN = 3072
NH = 8
HD = 64
NE = 32
DE = 512
PLE = 256
CAP = 256
ALPHA = float(2 ** 0.25)
EPS = 1e-5
NEG = -30000.0
NEGINF = -1.0e30


class Eng:
    def __init__(self, name, sem, same_wait):
        self.name = name
        self.sem = sem
        self.count = 0
        self.waited = {}
        self.ops = []
        self.same_wait = same_wait


class Buf:
    __slots__ = ("name", "w", "r", "excl")

    def __init__(self, name, excl=False):
        self.name = name
        self.w = None
        self.r = []
        self.excl = excl


class FW:
    def __init__(self, nc, stack):
        self.nc = nc
        self.stack = stack
        self.eng = {}
        for n, sw in (("pe", False), ("act", True), ("dve", True), ("pool", True), ("sp", False)):
            s = stack.enter_context(nc.semaphore("sem_" + n))
            self.eng[n] = Eng(n, s, sw)
        self.dsems = []
        self.names = {}
        self.bc_reg = None

    def dsem(self, name):
        s = [self.stack.enter_context(self.nc.semaphore(name)), 0]
        self.dsems.append(s)
        return s

    def _wait(self, e, s, v):
        if isinstance(s, list):
            s, v = s[0], s[1]
        if v <= 0:
            return
        k = id(s)
        if e.waited.get(k, 0) < v:
            e.waited[k] = v
            e.ops.append(lambda en, s=s, v=v: en.wait_ge(s, v))

    def _deps(self, e, reads, writes):
        for b in reads:
            if b.w is not None:
                s, v = b.w
                if not (s is e.sem and not e.same_wait):
                    self._wait(e, s, v)
            if b.excl:
                for (s, v) in b.r:
                    if not (s is e.sem and not e.same_wait):
                        self._wait(e, s, v)
        for b in writes:
            if b.w is not None:
                s, v = b.w
                if not (s is e.sem and not e.same_wait):
                    self._wait(e, s, v)
            for (s, v) in b.r:
                if not (s is e.sem and not e.same_wait):
                    self._wait(e, s, v)

    def _mark(self, tok, reads, writes):
        for b in reads:
            if b.excl:
                b.r = [tok]
            else:
                b.r.append(tok)
                if len(b.r) > 64:
                    b.r = b.r[-64:]
        for b in writes:
            b.w = tok
            b.r = []

    def op(self, engname, fn, reads=(), writes=()):
        e = self.eng[engname]
        self._deps(e, reads, writes)
        e.count += 1
        tok = (e.sem, e.count)
        import sys as _sys
        line = _sys._getframe(1).f_lineno

        def run(en, fn=fn, s=e.sem, line=line):
            ins = fn(en)
            try:
                self.names[ins.ins.name] = line
            except Exception:
                pass
            return ins.then_inc(s, 1)
        e.ops.append(run)
        self._mark(tok, reads, writes)
        return tok

    def dma(self, engname, fn, sem_state, reads=(), writes=()):
        e = self.eng[engname]
        saved = []
        for b in writes:
            if b.w is not None and b.w[0] is sem_state and not b.r:
                saved.append((b, b.w))
                b.w = None
        self._deps(e, reads, writes)
        for b, w in saved:
            b.w = w
        sem_state[1] += 16
        tok = (sem_state, None)
        e.ops.append(lambda en, fn=fn, s=sem_state[0]: fn(en).then_inc(s, 16))
        self._mark(tok, reads, writes)
        return tok

    def barrier(self):
        names = ["pe", "act", "dve", "pool", "sp"]
        snap = [(self.eng[n].sem, self.eng[n].count) for n in names]
        dsnap = [(s[0], s[1]) for s in self.dsems]
        for n in names:
            e = self.eng[n]
            for (s, v) in snap:
                if s is e.sem:
                    continue
                self._wait(e, s, v)
            for (s, v) in dsnap:
                self._wait(e, s, v)

    def replay(self):
        nc = self.nc
        with nc.Block() as block:
            @block.tensor
            def _(en):
                for f in self.eng["pe"].ops:
                    f(en)

            @block.scalar
            def _(en):
                for f in self.eng["act"].ops:
                    f(en)

            @block.vector
            def _(en):
                for f in self.eng["dve"].ops:
                    f(en)

            @block.gpsimd
            def _(en):
                for f in self.eng["pool"].ops:
                    f(en)

            @block.sync
            def _(en):
                for f in self.eng["sp"].ops:
                    f(en)


def build_nc(S=8192, dbg=False, stop_after=None):
    NB = S // 256
    NSLOT = NB // 4
    NOWN = NSLOT * 256
    TOT = S + NOWN
    NT = NOWN // 128
    NCH = S // 512
    NKT = TOT // 128
    NSL = NE * CAP

    nc = bass.Bass("TRN2", target_bir_lowering=False)

    def din(name, shape, dt=F32):
        return nc.dram_tensor(name, list(shape), dt, kind="ExternalInput").ap()

    xfT = din("xfT", [D, S])
    xoT = din("xoT", [NSLOT, D, 258])
    xo = din("xo", [NSLOT, 258, D])
    po = din("po", [NSLOT, 256, PLE])
    nm1_d = din("nm1", [NSLOT * 256])
    nm2_d = din("nm2", [NSLOT * 256])
    w_in = din("w_in", [D, DIN])
    w_conv = din("w_conv", [3, 512])
    w_out = din("w_out", [D, D])
    ln1_g = din("ln1_g", [D])
    ln1_b = din("ln1_b", [D])
    w_rg = din("w_rg", [D, 4])
    b_rg = din("b_rg", [4])
    w_re = din("w_re", [D, NE])
    b_re = din("b_re", [NE])
    w_gate = din("w_gate", [NE, D, DE])
    w_up = din("w_up", [NE, D, DE])
    w_down = din("w_down", [NE, DE, D])
    w_pg = din("w_pg", [D, D])
    w_pp = din("w_pp", [PLE, D])
    ln2_g = din("ln2_g", [D])
    ln2_b = din("ln2_b", [D])
    out = nc.dram_tensor("out", [NOWN, D], F32, kind="ExternalOutput").ap()
    if dbg:
        dbg_x1 = nc.dram_tensor("dbg_x1", [NOWN, D], F32, kind="ExternalOutput").ap()
        dbg_mix = nc.dram_tensor("dbg_mix", [NOWN, D], F32, kind="ExternalOutput").ap()

    KT = nc.dram_tensor("KT_scr", [NH * HD, TOT], BF16).ap()
    VV = nc.dram_tensor("VV_scr", [NH, 128, NKT, 65], BF16).ap()
    XS = nc.dram_tensor("XS_scr", [NSL, D], BF16).ap()
    YS = nc.dram_tensor("YS_scr", [NSL, D], F32).ap()
    ACCD = nc.dram_tensor("ACC_scr", [NOWN, D], F32).ap()

    st = ExitStack()
    with st:
        fw = FW(nc, st)
        ARENA = 98304 + 59392 + 20480 + 8192
        arena = st.enter_context(nc.sbuf_tensor("arena", [128, ARENA], U8))
        R0, R1, R2, RC = 0, 98304, 98304 + 59392, 98304 + 59392 + 20480

        class Alloc:
            def __init__(self, base, size):
                self.base, self.size, self.off = base, size, 0

            def __call__(self, nelem, dtype, pat=None, **kw):
                esz = 4 if dtype in (F32, I32) else 2
                nbytes = nelem * esz
                o = self.base + self.off
                self.off += (nbytes + 63) // 64 * 64
                assert self.off <= self.size, ("arena overflow", self.base, self.off, self.size)
                a = arena[:, o:o + nbytes].bitcast(dtype)
                if pat:
                    a = a.rearrange(pat, **kw)
                return a

        psall = st.enter_context(nc.psum_tensor("psall", [128, 4096], F32))
        psf = [psall[:, i * 512:(i + 1) * 512] for i in range(6)]
        psb = [psall[:, (6 + i) * 512:(7 + i) * 512].bitcast(BF16) for i in range(2)]
        psw = [psall[:, j * 1024:(j + 1) * 1024] for j in range(2)]
        pse = psall[:, 6 * 512:7 * 512]
        PW = [Buf("psw%d" % j, excl=True) for j in range(2)]
        PF = [Buf("psf%d" % i, excl=True) for i in range(6)]
        PB = [Buf("psb%d" % i, excl=True) for i in range(2)]

        def alt(i):
            return "act" if i % 2 == 0 else "dve"


        def MM(o, l, r, st_, sp_):
            return lambda e: e.matmul(o, lhsT=l, rhs=r, start=st_, stop=sp_)

        def TR(o, i_, idn):
            return lambda e: e.transpose(out=o, in_=i_, identity=idn)

        def ACTF(o, i_, f, **kw):
            return lambda e: e.activation(out=o, in_=i_, func=f, **kw)

        def TT(o, a, b, op):
            return lambda e: e.tensor_tensor(out=o, in0=a, in1=b, op=op)

        def TS(o, a, s1, s2, op0, op1=None):
            if op1 is None:
                return lambda e: e.tensor_scalar(out=o, in0=a, scalar1=s1, scalar2=None, op0=op0)
            return lambda e: e.tensor_scalar(out=o, in0=a, scalar1=s1, scalar2=s2, op0=op0, op1=op1)

        def STT(o, a, sc, b, op0, op1):
            return lambda e: e.scalar_tensor_tensor(out=o, in0=a, scalar=sc, in1=b, op0=op0, op1=op1)

        def MS(ap, v):
            return lambda e: e.memset(ap, v)

        def RD(o, i_, op):
            return lambda e: e.tensor_reduce(out=o, in_=i_, axis=AX.X, op=op)

        def MX(o, i_):
            return lambda e: e.max(out=o, in_=i_)

        def RCP(o, i_):
            return lambda e: e.reciprocal(out=o, in_=i_)

        def DM(o, i_):
            return lambda e: e.dma_start(out=o, in_=i_)

        def CPY(o, i_):
            return lambda e: e.tensor_copy(out=o, in_=i_)

        def ASEL(o, i_, pattern, cmp, fill, base, cm):
            return lambda e: e.affine_select(out=o, in_=i_, pattern=pattern, compare_op=cmp, fill=fill, base=base, channel_multiplier=cm)

        def BNS(o, i_):
            return lambda e: e.bn_stats(out=o, in_=i_)

        def BNA(o, i_):
            return lambda e: e.bn_aggr(out=o, in_=i_)

        def SCAT(dst, idx, src, bc):
            return lambda e: e.indirect_dma_start(out=dst, out_offset=bass.IndirectOffsetOnAxis(ap=idx, axis=0), in_=src, in_offset=None, bounds_check=bc, oob_is_err=False)

        def GATH(dst, src, idx, bc):
            return lambda e: e.indirect_dma_start(out=dst, out_offset=None, in_=src, in_offset=bass.IndirectOffsetOnAxis(ap=idx, axis=0), bounds_check=bc, oob_is_err=False)


        def _L_matmul(o, lhsT=None, rhs=None, start=None, stop=None):
            return lambda e: e.matmul(o, lhsT=lhsT, rhs=rhs, start=start, stop=stop)

        def _mk(meth):
            def f(*a, **kw):
                return lambda e: getattr(e, meth)(*a, **kw)
            return f

        _L_transpose = _mk("transpose")
        _L_activation = _mk("activation")
        _L_tensor_tensor = _mk("tensor_tensor")
        _L_tensor_scalar = _mk("tensor_scalar")
        _L_scalar_tensor_tensor = _mk("scalar_tensor_tensor")
        _L_memset = _mk("memset")
        _L_tensor_reduce = _mk("tensor_reduce")
        _L_max = _mk("max")
        _L_reciprocal = _mk("reciprocal")
        _L_dma_start = _mk("dma_start")
        _L_tensor_copy = _mk("tensor_copy")
        _L_affine_select = _mk("affine_select")
        _L_bn_stats = _mk("bn_stats")
        _L_bn_aggr = _mk("bn_aggr")
        def _L_indirect_dma_start(*a, **kw):
            def f(e):
                if fw.bc_reg is None:
                    fw.bc_reg = e.to_reg(kw["bounds_check"])
                kw2 = dict(kw)
                kw2["bounds_check"] = fw.bc_reg
                return e.indirect_dma_start(*a, **kw2)
            return f
        _L_iota = _mk("iota")

        def copy_op(eng, out_ap, in_ap):
            if eng == "act":
                return lambda e: e.activation(out=out_ap, in_=in_ap, func=AF.Copy)
            return lambda e: e.tensor_copy(out=out_ap, in_=in_ap)

        ac = Alloc(RC, 8192)
        ident_f = ac(128, F32)
        ident_b = ac(128, BF16)
        ones_f = ac(128, F32)
        CBm = ac(512, BF16, "p (t q) -> p t q", t=2)
        wc = ac(12, F32, "p (c k) -> p c k", c=4)
        km = ac(4 * 32, F32, "p (c n) -> p c n", c=4)
        kmb = ac(4 * 32, BF16, "p (c n) -> p c n", c=4)
        kmh = ac(8 * 32, BF16, "p (h n) -> p h n", h=8)
        slot_i = ac(NT * 2, I32, "p (t k) -> p t k", k=2)
        w12 = ac(NT * 2, F32, "p (t k) -> p t k", k=2)
        Lst = ac(128, F32)
        eC = ac(32, F32)
        Spre = ac(32, F32)
        bias36 = ac(36, F32)
        ztile = ac(1024, BF16)
        B_ident = Buf("ident")
        B_const = Buf("const")
        B_km = Buf("km")
        B_kmh = Buf("kmh")
        B_slot = [Buf("slot%d" % t) for t in range(NT)]
        B_w12 = [Buf("w12_%d" % t) for t in range(NT)]
        B_Spre = Buf("Spre")

        s_setup = fw.dsem("setup")

        fw.op("pool", _L_memset(ident_f, 0.0), writes=[B_ident])
        fw.op("pool", _L_affine_select(out=ident_f, in_=ident_f, pattern=[[-1, 128]], compare_op=ALU.not_equal, fill=1.0, base=0, channel_multiplier=1), reads=[B_ident], writes=[B_ident])
        fw.op("pool", _L_tensor_copy(out=ident_b, in_=ident_f), reads=[B_ident], writes=[B_const])
        fw.op("pool", _L_memset(ones_f, 1.0), writes=[B_const])
        fw.op("pool", _L_memset(CBm, 0.0), writes=[B_const])
        fw.op("pool", _L_affine_select(out=CBm, in_=CBm, pattern=[[-128, 2], [1, 256]], compare_op=ALU.is_ge, fill=NEG, base=0, channel_multiplier=-1), reads=[B_const], writes=[B_const])
        fw.op("pool", _L_memset(Lst, 1.0), writes=[B_const])
        fw.op("pool", _L_affine_select(out=Lst, in_=Lst, pattern=[[1, 128]], compare_op=ALU.is_ge, fill=0.0, base=-1, channel_multiplier=-1), reads=[B_const], writes=[B_const])
        fw.op("pool", _L_iota(eC, pattern=[[CAP, 32]], base=0, channel_multiplier=0, allow_small_or_imprecise_dtypes=True), writes=[B_const])
        fw.op("pool", _L_memset(Spre, 0.0), writes=[B_Spre])
        for c_ in range(4):
            for k_ in range(3):
                fw.dma("sp", _L_dma_start(out=wc[:, c_, k_:k_ + 1], in_=w_conv[k_, c_ * 128:(c_ + 1) * 128].rearrange("(p o) -> p o", o=1)), s_setup, writes=[B_const])
        fw.dma("sp", _L_dma_start(out=bias36[:, 0:4], in_=b_rg.partition_broadcast(128)), s_setup, writes=[B_const])
        fw.dma("sp", _L_dma_start(out=bias36[:, 4:36], in_=b_re.partition_broadcast(128)), s_setup, writes=[B_const])

        a0 = Alloc(R0, 98304)
        w_in_bf = a0(8 * DIN, BF16, "p (c n) -> p c n", c=8)
        Qaug = Alloc(R0 + 49152, 32768)(8 * NOWN, BF16, "p (h n) -> p h n", h=8)
        yconvT = Alloc(R0 + 81920, 16384)(4 * NOWN, BF16, "p (c n) -> p c n", c=4)
        B_win = [Buf("win%d" % c) for c in range(8)]
        B_Q = [Buf("Q%d" % i) for i in range(NSLOT)]
        B_yc = [Buf("yc%d" % i) for i in range(NSLOT)]

        a1 = Alloc(R1, 59392)
        wst = [a1(DIN, F32) for _ in range(2)]
        B_wst = [Buf("wst%d" % i) for i in range(2)]
        s_wst = [fw.dsem("wst%d" % i) for i in range(2)]
        B_z = Buf("ztile")
        s_z = fw.dsem("zfill")
        B_XS = Buf("XS")

        fw.op("pool", _L_memset(ztile, 0.0), writes=[B_z])
        XSv = XS.rearrange("(t p) d -> t p d", p=128)
        for c in range(8):
            sl = c % 2
            fw.dma("sp", _L_dma_start(out=wst[sl], in_=w_in[c * 128:(c + 1) * 128, :]), s_wst[sl], writes=[B_wst[sl]])
            for k, eng in enumerate(("pool", "act", "dve")):
                o, i_ = w_in_bf[:, c, k * 1024:(k + 1) * 1024], wst[sl][:, k * 1024:(k + 1) * 1024]
                fw.op(eng, copy_op(eng, o, i_), reads=[B_wst[sl]], writes=[B_win[c]])
        fw.barrier()

        a1 = Alloc(R1, 59392)
        xs = [a1(8 * 512, F32, "p (c n) -> p c n", c=8) for _ in range(2)]
        xT = [a1(8 * 512, BF16, "p (c n) -> p c n", c=8) for _ in range(2)]
        KTst = [a1(4 * 512, BF16, "p (c n) -> p c n", c=4) for _ in range(2)]
        a2 = Alloc(R2, 20480)
        Vst = [a2(8 * 4 * 65, BF16, "p (h k e) -> p h k e", h=8, k=4) for _ in range(2)]
        B_xs = [Buf("xs%d" % i) for i in range(2)]
        B_xT = [[Buf("xT%d_%d" % (i, g)) for g in range(3)] for i in range(2)]
        XG = ((("act", 0, 3), ("dve", 3, 6), ("pool", 6, 8)))

        def xg(c):
            return min(c // 3, 2)
        B_KTst = [Buf("KTst%d" % i) for i in range(2)]
        B_Vst = [Buf("Vst%d" % i) for i in range(2)]
        s_xs = [fw.dsem("xs%d" % i) for i in range(2)]
        s_kst = [fw.dsem("kst%d" % i) for i in range(2)]
        s_vst = [fw.dsem("vst%d" % i) for i in range(2)]
        B_KT = Buf("KT")
        B_VV = Buf("VV")
        for i in range(2):
            fw.op("pool", _L_memset(Vst[i], 1.0), writes=[B_Vst[i]])
        fw.op("pool", _L_memset(km, 0.0), writes=[B_km])
        fw.op("pool", _L_memset(Qaug[64:128], 0.0), writes=B_Q)
        ZF_PER = (NSL // 128 + NCH - 1) // NCH

        KTv = KT.rearrange("(c p) n -> p c n", p=128)
        VVv = VV.rearrange("h p k e -> p h k e")
        xfTv = xfT.rearrange("(c p) n -> p c n", p=128)

        def load_chunk(t):
            sl = t % 2
            fw.dma("sp", _L_dma_start(out=xs[sl], in_=xfTv[:, :, t * 512:(t + 1) * 512]), s_xs[sl], writes=[B_xs[sl]])

        ev = [0]
        load_chunk(0)
        for t in range(NCH):
            sl = t % 2
            if t + 1 < NCH:
                load_chunk(t + 1)
            for g, (eng, c0, c1) in enumerate(XG):
                fw.op(eng, copy_op(eng, xT[sl][:, c0:c1, :], xs[sl][:, c0:c1, :]), reads=[B_xs[sl]], writes=[B_xT[sl][g]])
            for pr in range(4):
                bk = 2 + pr % 2
                for c in range(8):
                    fw.op("pe", _L_matmul(psf[bk][:, :], lhsT=w_in_bf[:, c, 2048 + pr * 128:2048 + (pr + 1) * 128], rhs=xT[sl][:, c, :], start=(c == 0), stop=(c == 7)), reads=[B_xT[sl][xg(c)], B_win[c]], writes=[PF[bk]])
                fw.op("act", copy_op("act", KTst[sl][:, pr, :], psf[bk][:, :]), reads=[PF[bk]], writes=[B_KTst[sl]])
                fw.op("dve", _L_tensor_reduce(out=km[:, pr, 2 * t:2 * t + 2], in_=psf[bk][:, :].rearrange("p (a b) -> p a b", a=2), axis=AX.X, op=ALU.add), reads=[PF[bk]], writes=[B_km])
            fw.dma("sp", _L_dma_start(out=KTv[:, :, t * 512:(t + 1) * 512], in_=KTst[sl]), s_kst[sl], reads=[B_KTst[sl]], writes=[])
            for q in range(4):
                bk = 4 + q % 2
                for c in range(8):
                    fw.op("pe", _L_matmul(psf[bk][:, :], lhsT=xT[sl][:, c, q * 128:(q + 1) * 128], rhs=w_in_bf[:, c, 2560:3072], start=(c == 0), stop=(c == 7)), reads=[B_xT[sl][xg(c)], B_win[c]], writes=[PF[bk]])
                eng = alt(ev[0]); ev[0] += 1
                fw.op(eng, copy_op(eng, Vst[sl][:, :, q, 0:64], psf[bk][:, :].rearrange("p (h e) -> p h e", h=8)), reads=[PF[bk]], writes=[B_Vst[sl]])
            fw.dma("sp", _L_dma_start(out=VVv[:, :, 4 * t:4 * t + 4, :], in_=Vst[sl]), s_vst[sl], reads=[B_Vst[sl]], writes=[])
        fw.barrier()

        a1 = Alloc(R1, 59392)
        xos = [a1(8 * 258, F32, "p (c n) -> p c n", c=8) for _ in range(2)]
        xTo = [a1(8 * 258, BF16, "p (c n) -> p c n", c=8) for _ in range(2)]
        KTst2 = [a1(4 * 256, BF16, "p (c n) -> p c n", c=4) for _ in range(2)]
        Vst2 = [a1(8 * 2 * 65, BF16, "p (h k e) -> p h k e", h=8, k=2) for _ in range(2)]
        ccs = [a1(258, F32) for _ in range(2)]
        uu = [a1(258, F32) for _ in range(2)]
        tt_ = [a1(256, F32) for _ in range(2)]
        B_xos = [Buf("xos%d" % i) for i in range(2)]
        B_xTo = [[Buf("xTo%d_%d" % (i, g)) for g in range(3)] for i in range(2)]
        B_K2 = [Buf("K2_%d" % i) for i in range(2)]
        B_V2 = [Buf("V2_%d" % i) for i in range(2)]
        B_cc = [Buf("cc%d" % i) for i in range(2)]
        B_uu = [Buf("uu%d" % i) for i in range(2)]
        B_tt = [Buf("tt%d" % i) for i in range(2)]
        s_xos = [fw.dsem("xos%d" % i) for i in range(2)]
        s_k2 = [fw.dsem("k2_%d" % i) for i in range(2)]
        s_v2 = [fw.dsem("v2_%d" % i) for i in range(2)]
        for i in range(2):
            fw.op("pool", _L_memset(Vst2[i], 1.0), writes=[B_V2[i]])

        def load_own(i):
            sl = i % 2
            fw.dma("sp", _L_dma_start(out=xos[sl], in_=xoT[i].rearrange("(c p) n -> p c n", p=128)), s_xos[sl], writes=[B_xos[sl]])

        load_own(0)
        cvi = [0]
        for i in range(NSLOT):
            sl = i % 2
            if i + 1 < NSLOT:
                load_own(i + 1)
            c0 = i * 256
            for g, (eng, c0_, c1_) in enumerate(XG):
                fw.op(eng, copy_op(eng, xTo[sl][:, c0_:c1_, :], xos[sl][:, c0_:c1_, :]), reads=[B_xos[sl]], writes=[B_xTo[sl][g]])
            for hp in range(4):
                bk = 2 + hp % 2
                for hh in range(2):
                    h = 2 * hp + hh
                    for c in range(8):
                        fw.op("pe", _L_matmul(psf[bk][0:64, hh * 256:(hh + 1) * 256], lhsT=w_in_bf[:, c, 1536 + h * 64:1536 + (h + 1) * 64], rhs=xTo[sl][:, c, 2:258], start=(c == 0), stop=(c == 7)), reads=[B_xTo[sl][xg(c)], B_win[c]], writes=[PF[bk]])
                eng = alt(ev[0]); ev[0] += 1
                fw.op(eng, copy_op(eng, Qaug[0:64, 2 * hp:2 * hp + 2, c0:c0 + 256], psf[bk][0:64, :].rearrange("p (a b) -> p a b", a=2)), reads=[PF[bk]], writes=[B_Q[i]])
            for pp in range(2):
                bk = 4 + pp % 2
                for q in range(2):
                    pr = 2 * pp + q
                    for c in range(8):
                        fw.op("pe", _L_matmul(psf[bk][:, q * 256:(q + 1) * 256], lhsT=w_in_bf[:, c, 2048 + pr * 128:2048 + (pr + 1) * 128], rhs=xTo[sl][:, c, 2:258], start=(c == 0), stop=(c == 7)), reads=[B_xTo[sl][xg(c)], B_win[c]], writes=[PF[bk]])
                eng = alt(ev[0]); ev[0] += 1
                fw.op(eng, copy_op(eng, KTst2[sl][:, 2 * pp:2 * pp + 2, :], psf[bk][:, :].rearrange("p (a b) -> p a b", a=2)), reads=[PF[bk]], writes=[B_K2[sl]])
            fw.dma("sp", _L_dma_start(out=KTv[:, :, S + c0:S + c0 + 256], in_=KTst2[sl]), s_k2[sl], reads=[B_K2[sl]], writes=[])
            for q in range(2):
                bk = 2 + q % 2
                for c in range(8):
                    fw.op("pe", _L_matmul(psf[bk][:, :], lhsT=xTo[sl][:, c, 2 + q * 128:2 + (q + 1) * 128], rhs=w_in_bf[:, c, 2560:3072], start=(c == 0), stop=(c == 7)), reads=[B_xTo[sl][xg(c)], B_win[c]], writes=[PF[bk]])
                eng = alt(ev[0]); ev[0] += 1
                fw.op(eng, copy_op(eng, Vst2[sl][:, :, q, 0:64], psf[bk][:, :].rearrange("p (h e) -> p h e", h=8)), reads=[PF[bk]], writes=[B_V2[sl]])
            fw.dma("sp", _L_dma_start(out=VVv[:, :, S // 128 + 2 * i:S // 128 + 2 * i + 2, :], in_=Vst2[sl]), s_v2[sl], reads=[B_V2[sl]], writes=[])
            for cc in range(4):
                k2 = cvi[0] % 2
                cvi[0] += 1
                banks = (0, 1, 4) if cc % 2 == 0 else (5, 2, 3)
                specs = ((banks[0], 0 + cc * 128, 2, 256), (banks[1], 512 + cc * 128, 0, 258), (banks[2], 1024 + cc * 128, 0, 258))
                for (bk, col, o0, n) in specs:
                    for c in range(8):
                        fw.op("pe", _L_matmul(psf[bk][:, 0:n], lhsT=w_in_bf[:, c, col:col + 128], rhs=xTo[sl][:, c, o0:o0 + n], start=(c == 0), stop=(c == 7)), reads=[B_xTo[sl][xg(c)], B_win[c]], writes=[PF[bk]])
                bcb, bcc, bch = banks
                fw.op("act", copy_op("act", ccs[k2], psf[bcc][:, 0:258]), reads=[PF[bcc]], writes=[B_cc[k2]])
                fw.op("dve", _L_tensor_tensor(out=uu[k2], in0=ccs[k2], in1=psf[bch][:, 0:258], op=ALU.mult), reads=[B_cc[k2], PF[bch]], writes=[B_uu[k2]])
                fw.op("dve", _L_tensor_scalar(out=tt_[k2], in0=uu[k2][:, 2:258], scalar1=wc[:, cc, 2:3], scalar2=None, op0=ALU.mult), reads=[B_uu[k2], B_const], writes=[B_tt[k2]])
                fw.op("dve", _L_scalar_tensor_tensor(out=tt_[k2], in0=uu[k2][:, 1:257], scalar=wc[:, cc, 1:2], in1=tt_[k2], op0=ALU.mult, op1=ALU.add), reads=[B_uu[k2], B_const, B_tt[k2]], writes=[B_tt[k2]])
                fw.op("dve", _L_scalar_tensor_tensor(out=tt_[k2], in0=uu[k2][:, 0:256], scalar=wc[:, cc, 0:1], in1=tt_[k2], op0=ALU.mult, op1=ALU.add), reads=[B_uu[k2], B_const, B_tt[k2]], writes=[B_tt[k2]])
                fw.op("dve", _L_tensor_tensor(out=yconvT[:, cc, c0:c0 + 256], in0=tt_[k2], in1=psf[bcb][:, 0:256], op=ALU.mult), reads=[B_tt[k2], PF[bcb]], writes=[B_yc[i]])
        fw.barrier()

        a2 = Alloc(R2, 20480)
        nm1 = a2(NSLOT * 256, F32, "p (i n) -> p i n", i=NSLOT)
        nm2 = a2(NSLOT * 256, F32, "p (i n) -> p i n", i=NSLOT)
        gm = [a2(256, F32, "p (h n) -> p h n", h=8) for _ in range(2)]
        m8 = [a2(64, F32, "p (h n) -> p h n", h=8) for _ in range(2)]
        a1 = Alloc(R1, 59392)
        bpad = [a1(8 * 128, BF16, "p (h n) -> p h n", h=8) for _ in range(2)]
        tA3 = [a1(256, F32, "p (h n) -> p h n", h=8) for _ in range(2)]
        tB3 = [a1(256, F32, "p (h n) -> p h n", h=8) for _ in range(2)]
        mm3 = [a1(24, F32, "p (r h) -> p r h", r=3) for _ in range(2)]
        B_nm = Buf("nm")
        B_gm = [Buf("gm%d" % i) for i in range(2)]
        B_m8 = [Buf("m8%d" % i) for i in range(2)]
        B_bp = [Buf("bp%d" % i) for i in range(2)]
        s_nm = fw.dsem("nm")
        s_kmh = fw.dsem("kmh")
        fw.dma("sp", _L_dma_start(out=nm1.rearrange("p i n -> p (i n)"), in_=nm1_d.partition_broadcast(128)), s_nm, writes=[B_nm])
        fw.dma("sp", _L_dma_start(out=nm2.rearrange("p i n -> p (i n)"), in_=nm2_d.partition_broadcast(128)), s_nm, writes=[B_nm])
        for i in range(2):
            fw.op("pool", _L_memset(bpad[i], 0.0), writes=[B_bp[i]])
        fw.op("dve", _L_tensor_copy(out=kmb, in_=km), reads=[B_km], writes=[B_km])
        kmh_v = kmh.rearrange("p (a b) n -> p a b n", b=2)
        fw.dma("sp", _L_dma_start(out=kmh_v[0:64, :, 0, :], in_=kmb[0:64, :, :]), s_kmh, reads=[B_km], writes=[B_kmh])
        fw.dma("sp", _L_dma_start(out=kmh_v[0:64, :, 1, :], in_=kmb[64:128, :, :]), s_kmh, reads=[B_km], writes=[B_kmh])
        for i in range(NSLOT):
            for t in range(2):
                k2 = (2 * i + t) % 2
                q0 = i * 256 + t * 128
                gb = k2
                for h in range(8):
                    fw.op("pe", _L_matmul(psf[gb][:, h * 32:(h + 1) * 32], lhsT=Qaug[0:64, h, q0:q0 + 128], rhs=kmh[0:64, h, :], start=True, stop=True), reads=[B_Q[i], B_kmh], writes=[PF[gb]])
                fw.op("dve", _L_tensor_tensor(out=gm[k2].rearrange("p h n -> p (h n)"), in0=psf[gb][:, 0:256], in1=nm1[:, i, :], op=ALU.add), reads=[PF[gb], B_nm], writes=[B_gm[k2]])
                G, A_, B_ = gm[k2], tA3[k2], tB3[k2]
                rd = [B_gm[k2], B_m8[k2]]
                wr_ = [B_m8[k2]]

                def bc(r):
                    return mm3[k2][:, r, :].unsqueeze(2).to_broadcast([128, 8, 32])
                fw.op("dve", _L_tensor_reduce(out=mm3[k2][:, 0, :], in_=G, axis=AX.X, op=ALU.max), reads=rd, writes=wr_)
                fw.op("dve", _L_tensor_tensor(out=A_, in0=G, in1=bc(0), op=ALU.is_ge), reads=rd, writes=wr_)
                fw.op("dve", _L_scalar_tensor_tensor(out=B_, in0=A_, scalar=-1.0e9, in1=G, op0=ALU.mult, op1=ALU.add), reads=rd, writes=wr_)
                fw.op("dve", _L_tensor_reduce(out=mm3[k2][:, 1, :], in_=B_, axis=AX.X, op=ALU.max), reads=rd, writes=wr_)
                fw.op("dve", _L_tensor_tensor(out=A_, in0=B_, in1=bc(1), op=ALU.is_ge), reads=rd, writes=wr_)
                fw.op("dve", _L_scalar_tensor_tensor(out=B_, in0=A_, scalar=-1.0e9, in1=B_, op0=ALU.mult, op1=ALU.add), reads=rd, writes=wr_)
                fw.op("dve", _L_tensor_reduce(out=mm3[k2][:, 2, :], in_=B_, axis=AX.X, op=ALU.max), reads=rd, writes=wr_)
                fw.op("dve", _L_tensor_tensor(out=A_, in0=G, in1=bc(2), op=ALU.is_lt), reads=rd, writes=wr_)
                fw.op("dve", _L_scalar_tensor_tensor(out=bpad[k2][:, :, 64:96], in0=A_, scalar=NEG, in1=nm2[:, i, :].rearrange("p (h n) -> p h n", h=8), op0=ALU.mult, op1=ALU.add), reads=rd + [B_nm], writes=[B_bp[k2]])
                pb = k2
                for h in range(8):
                    fw.op("pe", _L_transpose(out=psb[pb][:, h * 128:(h + 1) * 128], in_=bpad[k2][:, h, :], identity=ident_b), reads=[B_bp[k2], B_const], writes=[PB[pb]])
                fw.op("act", copy_op("act", Qaug[64:96, :, q0:q0 + 128], psb[pb][64:96, :].rearrange("p (h n) -> p h n", h=8)), reads=[PB[pb]], writes=[B_Q[i]])
        fw.barrier()

        if stop_after == "A":
            pass

        aB0 = Alloc(R0, 49152)
        KTaug = [aB0(TOT, BF16) for _ in range(2)]
        aB1 = Alloc(R1, 59392)
        yattnT = aB1(8 * NOWN, BF16, "p (h n) -> p h n", h=8)
        aB1.off = 32768
        Vh = [aB1(NKT * 65, BF16, "p (k e) -> p k e", e=65) for _ in range(2)]
        Pt = [aB1(1024, BF16) for _ in range(2)]
        aB2 = Alloc(R2, 20480)
        rcp = [aB2(256, F32) for _ in range(2)]
        bcs = [aB2(256, F32) for _ in range(2)]
        B_KTa = [Buf("KTa%d" % i) for i in range(2)]
        B_Vh = [Buf("Vh%d" % i) for i in range(2)]
        B_Pt = [Buf("Pt%d" % i) for i in range(2)]
        B_rcp = [Buf("rcp%d" % i) for i in range(2)]
        B_bcs = [Buf("bcs%d" % i) for i in range(2)]
        B_ya = [[Buf("ya%d_%d" % (h, i)) for i in range(NSLOT)] for h in range(8)]
        s_kta = [fw.dsem("kta%d" % i) for i in range(2)]
        s_vh = [fw.dsem("vh%d" % i) for i in range(2)]
        for s_ in range(2):
            fw.op("pool", _L_memset(KTaug[s_][64:128, :], 0.0), writes=[B_KTa[s_]])
            fw.op("pool", _L_memset(KTaug[s_][64:96, 0:S], 1.0), writes=[B_KTa[s_]])
            fw.op("pool", _L_affine_select(out=KTaug[s_][64:96, 0:S], in_=KTaug[s_][64:96, 0:S], pattern=[[1, S]], compare_op=ALU.is_ge, fill=0.0, base=0, channel_multiplier=-256), reads=[B_KTa[s_]], writes=[B_KTa[s_]])
            fw.op("pool", _L_affine_select(out=KTaug[s_][64:96, 0:S], in_=KTaug[s_][64:96, 0:S], pattern=[[-1, S]], compare_op=ALU.is_ge, fill=0.0, base=255, channel_multiplier=256), reads=[B_KTa[s_]], writes=[B_KTa[s_]])
        KTh = KT.rearrange("(h d) n -> h d n", d=64)

        def load_head(h):
            s_ = h % 2
            fw.dma("sp", _L_dma_start(out=KTaug[s_][0:64, :], in_=KTh[h]), s_kta[s_], reads=[B_KT], writes=[B_KTa[s_]])
            fw.dma("sp", _L_dma_start(out=Vh[s_], in_=VV[h]), s_vh[s_], reads=[B_VV], writes=[B_Vh[s_]])

        load_head(0)
        epi_pending = []
        sb_i = [0]
        ob_i = [0]
        pt_i = [0]
        for h in range(8):
            s_ = h % 2
            if h + 1 < 8:
                load_head(h + 1)
            for zt in range(h * (NSL // 128 // 8), (h + 1) * (NSL // 128 // 8)):
                fw.dma("sp", _L_dma_start(out=XSv[zt], in_=ztile), s_z, reads=[B_z], writes=[B_XS])
            for i in range(NSLOT):
                q0 = i * 256
                units = [256 * n for n in range(4 * i + 3)] + [S + q0]
                nu = len(units)
                ob = 4 + ob_i[0] % 2
                ob_i[0] += 1
                ndu = nu // 2

                def qk2(du):
                    j = sb_i[0] % 2
                    sb_i[0] += 1
                    for k in range(2):
                        u = 2 * du + k
                        kc = units[u]
                        own = (u == nu - 1)
                        for t in range(2):
                            o_ap = psw[j][:, k * 512 + t * 256:k * 512 + (t + 1) * 256]
                            fw.op("pe", _L_matmul(o_ap, lhsT=KTaug[s_][:, kc + t * 128:kc + (t + 1) * 128], rhs=Qaug[:, h, q0:q0 + 256], start=True, stop=(not own)), reads=[B_KTa[s_], B_Q[i]], writes=[PW[j]])
                            if own:
                                fw.op("pe", _L_matmul(o_ap, lhsT=ident_b, rhs=CBm[:, t, :], start=False, stop=True), reads=[B_const], writes=[PW[j]])
                    return j

                def expo2(j):
                    pi = pt_i[0] % 2
                    pt_i[0] += 1
                    fw.op("act", _L_activation(out=Pt[pi], in_=psw[j], func=AF.Exp, scale=0.125), reads=[PW[j]], writes=[B_Pt[pi]])
                    return pi

                def pv2(du, pi):
                    for k in range(2):
                        u = 2 * du + k
                        kc = units[u]
                        for t in range(2):
                            kt = kc // 128 + t
                            fw.op("pe", _L_matmul(psf[ob][0:65, 0:256], lhsT=Vh[s_][:, kt, :], rhs=Pt[pi][:, k * 512 + t * 256:k * 512 + (t + 1) * 256], start=(u == 0 and t == 0), stop=(u == nu - 1 and t == 1)), reads=[B_Vh[s_], B_Pt[pi]], writes=[PF[ob]])

                js = [qk2(0)]
                for du in range(ndu):
                    if du + 1 < ndu:
                        js.append(qk2(du + 1))
                    pi = expo2(js[du])
                    pv2(du, pi)
                    if du == 0 and epi_pending:
                        epi_pending.pop()()
                k2 = ob_i[0] % 2
                fw.op("dve", _L_reciprocal(out=rcp[k2][64:65, :], in_=psf[ob][64:65, 0:256]), reads=[PF[ob]], writes=[B_rcp[k2]])

                def epilogue(k2=k2, ob=ob, h=h, i=i, q0=q0):
                    fw.op("pe", _L_matmul(pse[0:64, 0:256], lhsT=ones_f[64:65, 0:64], rhs=rcp[k2][64:65, :], start=True, stop=True), reads=[B_rcp[k2], B_const], writes=[PB[0]])
                    fw.op("dve", copy_op("dve", bcs[k2][0:64, :], pse[0:64, 0:256]), reads=[PB[0]], writes=[B_bcs[k2]])
                    fw.op("dve", _L_tensor_tensor(out=yattnT[0:64, h, q0:q0 + 256], in0=psf[ob][0:64, 0:256], in1=bcs[k2][0:64, :], op=ALU.mult), reads=[PF[ob], B_bcs[k2]], writes=[B_ya[h][i]])
                epi_pending.append(epilogue)
        while epi_pending:
            epi_pending.pop()()
        fw.barrier()

        aC = Alloc(R0, 81920)
        wo_c = aC(4 * D, BF16, "p (c n) -> p c n", c=4)
        wo_a = aC(8 * D, BF16, "p (c n) -> p c n", c=8)
        wpg = aC(8 * D, BF16, "p (c n) -> p c n", c=8)
        wpp = aC(2 * D, BF16, "p (c n) -> p c n", c=2)
        wr = aC(8 * 36, F32, "p (c n) -> p c n", c=8)
        lng1 = aC(D, F32)
        lnb1 = aC(D, F32)
        cst = [aC(D, F32) for _ in range(4)]
        B_wC = Buf("wC")
        B_cst = [Buf("cst%d" % i) for i in range(4)]
        s_cst = [fw.dsem("cst%d" % i) for i in range(4)]
        s_wC = fw.dsem("wCs")
        fw.dma("sp", _L_dma_start(out=lng1, in_=ln1_g.partition_broadcast(128)), s_wC, writes=[B_wC])
        fw.dma("sp", _L_dma_start(out=lnb1, in_=ln1_b.partition_broadcast(128)), s_wC, writes=[B_wC])
        with nc.allow_non_contiguous_dma(reason="small router weights"):
            fw.dma("sp", _L_dma_start(out=wr[:, :, 0:4], in_=w_rg.rearrange("(c p) n -> p c n", p=128)), s_wC, writes=[B_wC])
            fw.dma("sp", _L_dma_start(out=wr[:, :, 4:36], in_=w_re.rearrange("(c p) n -> p c n", p=128)), s_wC, writes=[B_wC])
        pieces = []
        for c in range(4):
            pieces.append((w_out[c * 128:(c + 1) * 128, :], wo_c[:, c, :], 128))
        for hh in range(8):
            pieces.append((w_out[512 + hh * 64:512 + (hh + 1) * 64, :], wo_a[0:64, hh, :], 64))
        for c in range(8):
            pieces.append((w_pg[c * 128:(c + 1) * 128, :], wpg[:, c, :], 128))
        for c in range(2):
            pieces.append((w_pp[c * 128:(c + 1) * 128, :], wpp[:, c, :], 128))
        for k, (src, dst, npart) in enumerate(pieces):
            sl = k % 4
            fw.dma("sp", _L_dma_start(out=cst[sl][0:npart, :], in_=src), s_cst[sl], writes=[B_cst[sl]])
            eng = ("pool", "act", "dve")[k % 3]
            fw.op(eng, copy_op(eng, dst, cst[sl][0:npart, :]), reads=[B_cst[sl]], writes=[B_wC])

        aC1 = Alloc(R1 + 32768, 59392 - 32768)
        aC2 = Alloc(R2, 20480)
        xr = [aC1(D, F32) for _ in range(2)]
        h1 = [aC1(D, F32) for _ in range(2)]
        x1b = [aC1(D, BF16) for _ in range(2)]
        x1Tb = aC1(8 * 128, BF16, "p (c n) -> p c n", c=8)
        pTb = aC1(2 * 128, BF16, "p (c n) -> p c n", c=2)
        prow = [aC1(PLE, F32) for _ in range(2)]
        x1Tf = aC2(8 * 128, F32, "p (c n) -> p c n", c=8)
        sgt = aC2(D, F32)
        acc = [aC2(D, F32) for _ in range(2)]
        rs = aC2(512, F32)
        B_xr = [Buf("xr%d" % i) for i in range(2)]
        B_h1 = [Buf("h1_%d" % i) for i in range(2)]
        B_x1b = [Buf("x1b%d" % i) for i in range(2)]
        B_x1Tb = Buf("x1Tb")
        B_x1Tf = Buf("x1Tf")
        B_pTb = Buf("pTb")
        B_prow = [Buf("prow%d" % i) for i in range(2)]
        B_sgt = Buf("sgt")
        B_acc = [Buf("acc%d" % i) for i in range(2)]
        B_rs = Buf("rs")
        s_xr = [fw.dsem("xr%d" % i) for i in range(2)]
        s_pr = [fw.dsem("pr%d" % i) for i in range(2)]
        s_acc = [fw.dsem("accst%d" % i) for i in range(2)]
        s_sc = [fw.dsem("scat%d" % i) for i in range(2)]
        s_dbg = fw.dsem("dbg")
        B_ACCD = [Buf("ACCD%d" % t) for t in range(NT)]
        B_dbg = Buf("dbg")
        L36 = rs[:, 0:36]
        gmax = rs[:, 36:37]
        gsum = rs[:, 37:38]
        gp = rs[:, 38:39]
        oh = rs[:, 40:44]
        pen = rs[:, 44:48]
        junk4 = rs[:, 48:52]
        lem = rs[:, 64:96]
        r8 = rs[:, 96:104]
        sel1 = rs[:, 128:160]
        sel2 = rs[:, 160:192]
        selb = rs[:, 192:224]
        tmpa = rs[:, 224:256]
        tmpb = rs[:, 256:288]
        dd = rs[:, 288:289]
        rr = rs[:, 289:290]
        den = rs[:, 290:291]
        slf = rs[:, 292:294]
        stats = rs[:, 300:312]
        mv = rs[:, 312:314]
        rstd = rs[:, 314:315]

        dmy = rs[:, 320:324]
        B_dmy = Buf("dmy")
        fw.op("pool", _L_memset(rs[:, 316:324], 0.0), writes=[B_dmy])

        def prefetch_table(func):
            fw.op("act", _L_activation(out=dmy[:, 2:4], in_=dmy[:, 0:2], func=func), reads=[], writes=[B_dmy])

        def layer_norm(eng_list, h_ap, B_h, g_ap, b_ap, B_gb):
            fw.op("dve", _L_bn_stats(out=stats[:, 0:6], in_=h_ap[:, 0:512]), reads=[B_h], writes=[B_rs])
            fw.op("dve", _L_bn_stats(out=stats[:, 6:12], in_=h_ap[:, 512:1024]), reads=[B_h], writes=[B_rs])
            fw.op("dve", _L_bn_aggr(out=mv, in_=stats), reads=[B_rs], writes=[B_rs])
            fw.op("act", _L_activation(out=rstd, in_=mv[:, 1:2], func=AF.Sqrt, bias=EPS, scale=1.0), reads=[B_rs], writes=[B_rs])
            prefetch_table(AF.Exp)
            fw.op("dve", _L_reciprocal(out=rstd, in_=rstd), reads=[B_rs], writes=[B_rs])
            fw.op("dve", _L_tensor_scalar(out=h_ap, in0=h_ap, scalar1=mv[:, 0:1], scalar2=rstd, op0=ALU.subtract, op1=ALU.mult), reads=[B_h, B_rs], writes=[B_h])
            fw.op("dve", _L_tensor_tensor(out=h_ap, in0=h_ap, in1=g_ap, op=ALU.mult), reads=[B_h, B_gb], writes=[B_h])
            fw.op("dve", _L_tensor_tensor(out=h_ap, in0=h_ap, in1=b_ap, op=ALU.add), reads=[B_h, B_gb], writes=[B_h])

        def load_tok(t):
            k2 = t % 2
            i, hf = t // 2, t % 2
            fw.dma("sp", _L_dma_start(out=xr[k2], in_=xo[i, 2 + hf * 128:2 + (hf + 1) * 128, :]), s_xr[k2], writes=[B_xr[k2]])
            fw.dma("sp", _L_dma_start(out=prow[k2], in_=po[i, hf * 128:(hf + 1) * 128, :]), s_pr[k2], writes=[B_prow[k2]])

        def out_proj(t):
            i = t // 2
            tk = t * 128
            for half in range(2):
                bk = 2 + half
                n_mm = 12
                m = 0
                for cc in range(4):
                    fw.op("pe", _L_matmul(psf[bk][:, :], lhsT=yconvT[:, cc, tk:tk + 128], rhs=wo_c[:, cc, half * 512:(half + 1) * 512], start=(m == 0), stop=False), reads=[B_yc[i], B_wC], writes=[PF[bk]])
                    m += 1
                for hh in range(8):
                    fw.op("pe", _L_matmul(psf[bk][:, :], lhsT=yattnT[0:64, hh, tk:tk + 128], rhs=wo_a[0:64, hh, half * 512:(half + 1) * 512], start=False, stop=(m == n_mm - 1)), reads=[B_ya[hh][i], B_wC], writes=[PF[bk]])
                    m += 1

        load_tok(0)
        out_proj(0)
        prefetch_table(AF.Sqrt)
        for t in range(NT):
            k2 = t % 2
            i, hf = t // 2, t % 2
            tk = t * 128
            if t + 1 < NT:
                load_tok(t + 1)
            for half in range(2):
                bk = 2 + half
                fw.op("dve", _L_scalar_tensor_tensor(out=h1[k2][:, half * 512:(half + 1) * 512], in0=xr[k2][:, half * 512:(half + 1) * 512], scalar=ALPHA, in1=psf[bk][:, :], op0=ALU.mult, op1=ALU.add), reads=[B_xr[k2], PF[bk]], writes=[B_h1[k2]])
            if dbg:
                fw.dma("sp", _L_dma_start(out=dbg_mix[tk:tk + 128, :], in_=h1[k2]), s_dbg, reads=[B_h1[k2]], writes=[B_dbg])
            layer_norm(None, h1[k2], B_h1[k2], lng1, lnb1, B_wC)
            if dbg:
                fw.dma("sp", _L_dma_start(out=dbg_x1[tk:tk + 128, :], in_=h1[k2]), s_dbg, reads=[B_h1[k2]], writes=[B_dbg])
            for g in range(2):
                bk = 2 + g
                for q in range(4):
                    c = 4 * g + q
                    fw.op("pe", _L_transpose(out=psf[bk][:, q * 128:(q + 1) * 128], in_=h1[k2][:, c * 128:(c + 1) * 128], identity=ident_f), reads=[B_h1[k2], B_ident], writes=[PF[bk]])
                fw.op("act", copy_op("act", x1Tf[:, 4 * g:4 * g + 4, :], psf[bk][:, :].rearrange("p (a b) -> p a b", a=4)), reads=[PF[bk]], writes=[B_x1Tf])
                fw.op("dve", copy_op("dve", x1Tb[:, 4 * g:4 * g + 4, :], psf[bk][:, :].rearrange("p (a b) -> p a b", a=4)), reads=[PF[bk]], writes=[B_x1Tb])
            fw.op("act", copy_op("act", x1b[k2], h1[k2]), reads=[B_h1[k2]], writes=[B_x1b[k2]])
            for c in range(8):
                fw.op("pe", _L_matmul(psf[4][:, 0:36], lhsT=x1Tf[:, c, :], rhs=wr[:, c, :], start=(c == 0), stop=(c == 7)), reads=[B_x1Tf, B_wC], writes=[PF[4]])
            for half in range(2):
                bk = half
                for c in range(8):
                    fw.op("pe", _L_matmul(psf[bk][:, :], lhsT=x1Tb[:, c, :], rhs=wpg[:, c, half * 512:(half + 1) * 512], start=(c == 0), stop=(c == 7)), reads=[B_x1Tb, B_wC], writes=[PF[bk]])
            fw.op("dve", _L_tensor_tensor(out=L36, in0=psf[4][:, 0:36], in1=bias36, op=ALU.add), reads=[PF[4], B_const], writes=[B_rs])
            fw.op("dve", _L_tensor_reduce(out=gmax, in_=rs[:, 0:4], axis=AX.X, op=ALU.max), reads=[B_rs], writes=[B_rs])
            fw.op("dve", _L_tensor_scalar(out=dd, in0=gmax, scalar1=-1.0, scalar2=None, op0=ALU.mult), reads=[B_rs], writes=[B_rs])
            fw.op("act", _L_activation(out=junk4, in_=rs[:, 0:4], func=AF.Exp, bias=dd, scale=1.0, accum_out=gsum), reads=[B_rs], writes=[B_rs])
            fw.op("dve", _L_reciprocal(out=gp, in_=gsum), reads=[B_rs], writes=[B_rs])
            fw.op("dve", _L_tensor_scalar(out=oh, in0=rs[:, 0:4], scalar1=gmax, scalar2=None, op0=ALU.is_equal), reads=[B_rs], writes=[B_rs])
            fw.op("dve", _L_tensor_scalar(out=pen, in0=oh, scalar1=1.0, scalar2=1.0e30, op0=ALU.subtract, op1=ALU.mult), reads=[B_rs], writes=[B_rs])
            for g in range(4):
                fw.op("dve", _L_tensor_scalar(out=lem[:, 8 * g:8 * g + 8], in0=rs[:, 4 + 8 * g:12 + 8 * g], scalar1=pen[:, g:g + 1], scalar2=None, op0=ALU.add), reads=[B_rs], writes=[B_rs])
            fw.op("dve", _L_max(out=r8, in_=lem), reads=[B_rs], writes=[B_rs])
            fw.op("dve", _L_tensor_scalar(out=sel1, in0=lem, scalar1=r8[:, 0:1], scalar2=None, op0=ALU.is_equal), reads=[B_rs], writes=[B_rs])
            fw.op("dve", _L_tensor_scalar(out=sel2, in0=lem, scalar1=r8[:, 1:2], scalar2=None, op0=ALU.is_equal), reads=[B_rs], writes=[B_rs])
            fw.op("dve", _L_tensor_tensor(out=selb, in0=sel1, in1=sel2, op=ALU.add), reads=[B_rs], writes=[B_rs])
            fw.op("dve", _L_tensor_tensor(out=dd, in0=r8[:, 1:2], in1=r8[:, 0:1], op=ALU.subtract), reads=[B_rs], writes=[B_rs])
            fw.op("act", _L_activation(out=rr, in_=dd, func=AF.Exp), reads=[B_rs], writes=[B_rs])
            prefetch_table(AF.Sigmoid)
            fw.op("dve", _L_tensor_scalar(out=den, in0=rr, scalar1=1.0, scalar2=None, op0=ALU.add), reads=[B_rs], writes=[B_rs])
            fw.op("dve", _L_reciprocal(out=den, in_=den), reads=[B_rs], writes=[B_rs])
            fw.op("dve", _L_tensor_tensor(out=w12[:, t, 0:1], in0=gp, in1=den, op=ALU.mult), reads=[B_rs], writes=[B_w12[t]])
            fw.op("dve", _L_tensor_tensor(out=w12[:, t, 1:2], in0=w12[:, t, 0:1], in1=rr, op=ALU.mult), reads=[B_rs, B_w12[t]], writes=[B_w12[t]])
            fw.op("pe", _L_matmul(psf[5][:, 0:32], lhsT=Lst, rhs=selb, start=True, stop=False), reads=[B_rs, B_const], writes=[PF[5]])
            fw.op("pe", _L_matmul(psf[5][:, 0:32], lhsT=ones_f, rhs=Spre, start=False, stop=True), reads=[B_Spre, B_const], writes=[PF[5]])
            if t + 1 < NT:
                out_proj(t + 1)
            fw.op("dve", _L_tensor_tensor(out=tmpa, in0=psf[5][:, 0:32], in1=eC, op=ALU.add), reads=[PF[5], B_const], writes=[B_rs])
            fw.op("dve", _L_tensor_scalar(out=tmpb, in0=psf[5][:, 0:32], scalar1=float(CAP), scalar2=1.0e6, op0=ALU.is_ge, op1=ALU.mult), reads=[PF[5]], writes=[B_rs])
            fw.op("dve", _L_tensor_tensor(out=tmpa, in0=tmpa, in1=tmpb, op=ALU.add), reads=[B_rs], writes=[B_rs])
            fw.op("dve", _L_tensor_tensor(out=tmpb, in0=tmpa, in1=sel1, op=ALU.mult), reads=[B_rs], writes=[B_rs])
            fw.op("dve", _L_tensor_reduce(out=slf[:, 0:1], in_=tmpb, axis=AX.X, op=ALU.add), reads=[B_rs], writes=[B_rs])
            fw.op("dve", _L_tensor_tensor(out=tmpb, in0=tmpa, in1=sel2, op=ALU.mult), reads=[B_rs], writes=[B_rs])
            fw.op("dve", _L_tensor_reduce(out=slf[:, 1:2], in_=tmpb, axis=AX.X, op=ALU.add), reads=[B_rs], writes=[B_rs])
            fw.op("dve", _L_tensor_copy(out=slot_i[:, t, :], in_=slf), reads=[B_rs], writes=[B_slot[t]])
            fw.op("dve", _L_tensor_tensor(out=Spre, in0=Spre, in1=selb, op=ALU.add), reads=[B_rs, B_Spre], writes=[B_Spre])
            for k in range(2):
                fw.dma("pool", _L_indirect_dma_start(out=XS, out_offset=bass.IndirectOffsetOnAxis(ap=slot_i[:, t, k:k + 1], axis=0), in_=x1b[k2], in_offset=None, bounds_check=NSL - 1, oob_is_err=False), s_sc[k2], reads=[B_x1b[k2], B_slot[t], B_XS], writes=[B_XS])
            for half in range(2):
                fw.op("act", _L_activation(out=sgt[:, half * 512:(half + 1) * 512], in_=psf[half][:, :], func=AF.Sigmoid), reads=[PF[half]], writes=[B_sgt])
            prefetch_table(AF.Sqrt)
            for c in range(2):
                fw.op("pe", _L_transpose(out=psf[4][:, c * 128:(c + 1) * 128], in_=prow[k2][:, c * 128:(c + 1) * 128], identity=ident_f), reads=[B_prow[k2], B_ident], writes=[PF[4]])
            fw.op("act", copy_op("act", pTb, psf[4][:, 0:256].rearrange("p (a b) -> p a b", a=2)), reads=[PF[4]], writes=[B_pTb])
            for half in range(2):
                bk = half
                for c in range(2):
                    fw.op("pe", _L_matmul(psf[bk][:, :], lhsT=pTb[:, c, :], rhs=wpp[:, c, half * 512:(half + 1) * 512], start=(c == 0), stop=(c == 1)), reads=[B_pTb, B_wC], writes=[PF[bk]])
                fw.op("dve", _L_tensor_tensor(out=acc[k2][:, half * 512:(half + 1) * 512], in0=sgt[:, half * 512:(half + 1) * 512], in1=psf[bk][:, :], op=ALU.mult), reads=[B_sgt, PF[bk]], writes=[B_acc[k2]])
            fw.op("dve", _L_scalar_tensor_tensor(out=acc[k2], in0=h1[k2], scalar=ALPHA, in1=acc[k2], op0=ALU.mult, op1=ALU.add), reads=[B_h1[k2], B_acc[k2]], writes=[B_acc[k2]])
            fw.dma("sp", _L_dma_start(out=ACCD[tk:tk + 128, :], in_=acc[k2]), s_acc[k2], reads=[B_acc[k2]], writes=[B_ACCD[t]])
        fw.barrier()

        aD = Alloc(R0, 98304)
        wstg = [aD(8 * DE, F32) for _ in range(3)]
        wbf = [aD(8 * DE, BF16) for _ in range(6)]
        aD1 = Alloc(R1, 59392)
        Xe = [aD1(D, BF16) for _ in range(2)]
        XeT = [aD1(8 * 256, BF16, "p (c n) -> p c n", c=8) for _ in range(2)]
        hT = [aD1(4 * 256, BF16, "p (c n) -> p c n", c=4) for _ in range(2)]
        sgu = [aD1(256, F32) for _ in range(2)]
        Yst = [aD1(D, F32) for _ in range(2)]
        Y1 = [aD1(D, F32) for _ in range(2)]
        Y2 = [aD1(D, F32) for _ in range(2)]
        accr = [aD1(D, F32) for _ in range(3)]
        aD2 = Alloc(R2, 20480)
        lng2 = aD2(D, F32)
        lnb2 = aD2(D, F32)
        rs2 = aD2(64, F32)
        B_wstg = [Buf("wstg%d" % i) for i in range(3)]
        B_wbf = [Buf("wbf%d" % i) for i in range(6)]
        B_Xe = [Buf("Xe%d" % i) for i in range(2)]
        B_XeT = [Buf("XeT%d" % i) for i in range(2)]
        B_hT = [Buf("hT%d" % i) for i in range(2)]
        B_sgu = [Buf("sgu%d" % i) for i in range(2)]
        B_Yst = [Buf("Yst%d" % i) for i in range(2)]
        B_Y1 = [Buf("Y1_%d" % i) for i in range(2)]
        B_Y2 = [Buf("Y2_%d" % i) for i in range(2)]
        B_accr = [Buf("accr%d" % i) for i in range(3)]
        B_ln2 = Buf("ln2")
        B_YS = Buf("YS")
        s_wstg = [fw.dsem("wstg%d" % i) for i in range(3)]
        s_xe = [fw.dsem("xe%d" % i) for i in range(2)]
        s_yst = [fw.dsem("yst%d" % i) for i in range(2)]
        s_g = [fw.dsem("gath%d" % i) for i in range(2)]
        s_accr = [fw.dsem("accr%d" % i) for i in range(3)]
        s_out = [fw.dsem("out%d" % i) for i in range(3)]
        s_ln2 = fw.dsem("ln2")
        B_out = Buf("out")
        fw.dma("sp", _L_dma_start(out=lng2, in_=ln2_g.partition_broadcast(128)), s_ln2, writes=[B_ln2])
        fw.dma("sp", _L_dma_start(out=lnb2, in_=ln2_b.partition_broadcast(128)), s_ln2, writes=[B_ln2])

        mats = []
        for ex in range(NE):
            mats.append(("g", ex))
            mats.append(("u", ex))
            mats.append(("d", ex))

        CAST_ENG = ("pool", "act", "dve", "dve", "pool", "act", "dve", "act", "pool", "dve", "act", "dve")

        pending = {"act": [], "dve": []}

        def load_mat(mi):
            kind, ex = mats[mi]
            sl = mi % 3
            bs = mi % 6
            if kind in ("g", "u"):
                src = (w_gate if kind == "g" else w_up)[ex].rearrange("(p c) f -> p c f", c=8)
                dstv = wstg[sl].rearrange("p (c f) -> p c f", c=8)
            else:
                src = w_down[ex].rearrange("(c p) n -> p c n", p=128)
                dstv = wstg[sl].rearrange("p (c n) -> p c n", c=4)
            fw.dma("sp", _L_dma_start(out=dstv, in_=src), s_wstg[sl], writes=[B_wstg[sl]])
            for k in range(4):
                eng = CAST_ENG[(4 * mi + k) % 12]
                o, i_ = wbf[bs][:, k * 1024:(k + 1) * 1024], wstg[sl][:, k * 1024:(k + 1) * 1024]

                def emit(eng=eng, o=o, i_=i_, sl=sl, bs=bs):
                    fw.op(eng, copy_op(eng, o, i_), reads=[B_wstg[sl]], writes=[B_wbf[bs]])
                if eng == "pool":
                    emit()
                else:
                    pending[eng].append(emit)

        def drain(eng, n):
            for _ in range(n):
                if pending[eng]:
                    pending[eng].pop(0)()

        XSe = XS.rearrange("(e s p) d -> e s p d", s=2, p=128)
        YSe = YS.rearrange("(e s p) d -> e s p d", s=2, p=128)

        def load_xe(ex):
            for s2 in range(2):
                fw.dma("act", _L_dma_start(out=Xe[s2], in_=XSe[ex, s2]), s_xe[s2], reads=[B_XS], writes=[B_Xe[s2]])

        for mi in range(3):
            load_mat(mi)
        drain("act", 99)
        drain("dve", 99)
        load_xe(0)
        yi = [0]
        for ex in range(NE):
            k2 = ex % 2
            bb = (3 * ex) % 6
            wg = wbf[bb].rearrange("p (c f) -> p c f", c=8)
            wu = wbf[bb + 1].rearrange("p (c f) -> p c f", c=8)
            wd = wbf[bb + 2].rearrange("p (c n) -> p c n", c=4)
            if ex + 1 < NE:
                for q in range(3):
                    load_mat(3 * (ex + 1) + q)
            for s2 in range(2):
                pb = s2
                for c in range(8):
                    fw.op("pe", _L_transpose(out=psb[pb][:, c * 128:(c + 1) * 128], in_=Xe[s2].rearrange("t (p c) -> t c p", c=8)[:, c, :], identity=ident_b), reads=[B_Xe[s2], B_const], writes=[PB[pb]])
                eng = alt(s2)
                fw.op(eng, copy_op(eng, XeT[k2][:, :, s2 * 128:(s2 + 1) * 128], psb[pb][:, :].rearrange("p (c n) -> p c n", c=8)), reads=[PB[pb]], writes=[B_XeT[k2]])
            if ex + 1 < NE:
                load_xe(ex + 1)
            for fo in range(4):
                bg, bu = (0, 1) if fo % 2 == 0 else (2, 3)
                for (bk, wm, bi) in ((bg, wg, bb), (bu, wu, bb + 1)):
                    for c in range(8):
                        fw.op("pe", _L_matmul(psf[bk][:, 0:256], lhsT=wm[:, c, fo * 128:(fo + 1) * 128], rhs=XeT[k2][:, c, :], start=(c == 0), stop=(c == 7)), reads=[B_XeT[k2], B_wbf[bi]], writes=[PF[bk]])
                sq = fo % 2
                fw.op("act", _L_activation(out=sgu[sq], in_=psf[bg][:, 0:256], func=AF.Silu), reads=[PF[bg]], writes=[B_sgu[sq]])
                fw.op("dve", _L_tensor_tensor(out=hT[k2][:, fo, :], in0=sgu[sq], in1=psf[bu][:, 0:256], op=ALU.mult), reads=[B_sgu[sq], PF[bu]], writes=[B_hT[k2]])
                drain("act", 1)
                drain("dve", 1)
            for s2 in range(2):
                ys = yi[0] % 2
                yi[0] += 1
                for half in range(2):
                    bk = 4 + half
                    for fo in range(4):
                        fw.op("pe", _L_matmul(psf[bk][:, :], lhsT=hT[k2][:, fo, s2 * 128:(s2 + 1) * 128], rhs=wd[:, fo, half * 512:(half + 1) * 512], start=(fo == 0), stop=(fo == 3)), reads=[B_hT[k2], B_wbf[bb + 2]], writes=[PF[bk]])
                    eng = alt(half)
                    fw.op(eng, copy_op(eng, Yst[ys][:, half * 512:(half + 1) * 512], psf[bk][:, :]), reads=[PF[bk]], writes=[B_Yst[ys]])
                fw.dma("act", _L_dma_start(out=YSe[ex, s2], in_=Yst[ys]), s_yst[ys], reads=[B_Yst[ys]], writes=[])
            drain("act", 99)
            drain("dve", 99)

        fw.barrier()

        st2 = rs2[:, 0:12]
        mv2 = rs2[:, 12:14]
        rstd2 = rs2[:, 14:15]

        def fin_issue(t):
            k2, k3 = t % 2, t % 3
            tk = t * 128
            fw.op("pool", _L_memset(Y1[k2], 0.0), writes=[B_Y1[k2]])
            fw.op("pool", _L_memset(Y2[k2], 0.0), writes=[B_Y2[k2]])
            fw.dma("sp", _L_dma_start(out=accr[k3], in_=ACCD[tk:tk + 128, :]), s_accr[k3], reads=[B_ACCD[t]], writes=[B_accr[k3]])
            fw.dma("pool", _L_indirect_dma_start(out=Y1[k2], out_offset=None, in_=YS, in_offset=bass.IndirectOffsetOnAxis(ap=slot_i[:, t, 0:1], axis=0), bounds_check=NSL - 1, oob_is_err=False), s_g[k2], reads=[B_YS, B_slot[t]], writes=[B_Y1[k2]])
            fw.dma("pool", _L_indirect_dma_start(out=Y2[k2], out_offset=None, in_=YS, in_offset=bass.IndirectOffsetOnAxis(ap=slot_i[:, t, 1:2], axis=0), bounds_check=NSL - 1, oob_is_err=False), s_g[k2], reads=[B_YS, B_slot[t]], writes=[B_Y2[k2]])

        def fin_front(t):
            k2, k3 = t % 2, t % 3
            h_ap, B_h, B_r = accr[k3], B_accr[k3], B_rs
            fw.op("dve", _L_scalar_tensor_tensor(out=h_ap, in0=Y1[k2], scalar=w12[:, t, 0:1], in1=h_ap, op0=ALU.mult, op1=ALU.add), reads=[B_Y1[k2], B_w12[t], B_h], writes=[B_h])
            fw.op("dve", _L_scalar_tensor_tensor(out=h_ap, in0=Y2[k2], scalar=w12[:, t, 1:2], in1=h_ap, op0=ALU.mult, op1=ALU.add), reads=[B_Y2[k2], B_w12[t], B_h], writes=[B_h])
            fw.op("dve", _L_bn_stats(out=st2[:, 0:6], in_=h_ap[:, 0:512]), reads=[B_h], writes=[B_r])
            fw.op("dve", _L_bn_stats(out=st2[:, 6:12], in_=h_ap[:, 512:1024]), reads=[B_h], writes=[B_r])
            fw.op("dve", _L_bn_aggr(out=mv2, in_=st2), reads=[B_r], writes=[B_r])
            fw.op("act", _L_activation(out=rstd2, in_=mv2[:, 1:2], func=AF.Sqrt, bias=EPS, scale=1.0), reads=[B_r], writes=[B_r])
            fw.op("dve", _L_reciprocal(out=rstd2, in_=rstd2), reads=[B_r], writes=[B_r])
            fw.op("dve", _L_tensor_scalar(out=h_ap, in0=h_ap, scalar1=mv2[:, 0:1], scalar2=rstd2, op0=ALU.subtract, op1=ALU.mult), reads=[B_h, B_r], writes=[B_h])
            fw.op("dve", _L_tensor_tensor(out=h_ap, in0=h_ap, in1=lng2, op=ALU.mult), reads=[B_h, B_ln2], writes=[B_h])

        def fin_back(t):
            k3 = t % 3
            tk = t * 128
            fw.op("dve", _L_tensor_tensor(out=accr[k3], in0=accr[k3], in1=lnb2, op=ALU.add), reads=[B_accr[k3], B_ln2], writes=[B_accr[k3]])
            fw.dma("sp", _L_dma_start(out=out[tk:tk + 128, :], in_=accr[k3]), s_out[k3], reads=[B_accr[k3]], writes=[])

        fin_issue(0)
        for t in range(NT):
            if t + 1 < NT:
                fin_issue(t + 1)
            fin_front(t)
            if t >= 1:
                fin_back(t - 1)
        fin_back(NT - 1)
        fw.barrier()
        with nc.allow_non_contiguous_dma(reason="tiny strided parameter loads"):
            fw.replay()
    nc._fw_names = fw.names
    return nc


def own_blocks(r, NB):
    NSLOT = NB // 4
    js = []
    for i in range(NSLOT):
        if i < NSLOT // 2:
            js.append(r + 4 * i)
        else:
            js.append(NB - 1 - r - 4 * (NSLOT - 1 - i))
    return js


def make_in_maps(inputs, S):
    NB = S // 256
    x = np.asarray(inputs["x"], dtype=np.float32)
    p = np.asarray(inputs["p"], dtype=np.float32)
    nbatch = x.shape[0]
    shared = {
        "w_in": inputs["w_in"][0], "w_conv": inputs["w_conv"][0], "w_out": inputs["w_out"][0],
        "ln1_g": inputs["ln1_g"][0], "ln1_b": inputs["ln1_b"][0],
        "w_rg": inputs["w_router_g"][0], "b_rg": inputs["b_router_g"][0],
        "w_re": inputs["w_router_e"][0], "b_re": inputs["b_router_e"][0],
        "w_gate": inputs["w_gate"][0], "w_up": inputs["w_up"][0], "w_down": inputs["w_down"][0],
        "w_pg": inputs["w_ple_gate"][0], "w_pp": inputs["w_ple_proj"][0],
        "ln2_g": inputs["ln2_g"][0], "ln2_b": inputs["ln2_b"][0],
    }
    shared = {k: np.ascontiguousarray(np.asarray(v, dtype=np.float32)) for k, v in shared.items()}
    maps = []
    for b in range(nbatch):
        for r in range(4):
            js = own_blocks(r, NB)
            xo = np.zeros((len(js), 258, D), np.float32)
            po = np.zeros((len(js), 256, PLE), np.float32)
            nm1 = np.zeros((len(js), 8, 32), np.float32)
            nm2 = np.zeros((len(js), 8, 32), np.float32)
            for i, j in enumerate(js):
                lo = 256 * j - 2
                if lo >= 0:
                    xo[i] = x[b, lo:lo + 258]
                else:
                    xo[i, 2:] = x[b, 0:256]
                po[i] = p[0, b, 256 * j:256 * j + 256]
                nm1[i, :, j:] = NEGINF
                nm2[i, :, j:] = NEG
            m = dict(shared)
            m["xfT"] = np.ascontiguousarray(x[b].T)
            m["xoT"] = np.ascontiguousarray(xo.transpose(0, 2, 1))
            m["xo"] = xo
            m["po"] = po
            m["nm1"] = nm1.reshape(-1)
            m["nm2"] = nm2.reshape(-1)
            maps.append(m)
    return maps


_NC_CACHE = {}


def kernel(**inputs):
    x = np.asarray(inputs["x"])
    nbatch, S, _ = x.shape
    NB = S // 256
    if S not in _NC_CACHE:
        _NC_CACHE[S] = build_nc(S)
    nc = _NC_CACHE[S]
    maps = make_in_maps(inputs, S)
    res = run_bass_kernel_spmd(nc, maps, core_ids=list(range(len(maps))))
    outp = np.zeros((nbatch, S, D), np.float32)
    k = 0
    for b in range(nbatch):
        for r in range(4):
            o = np.asarray(res.results[k]["out"], dtype=np.float32)
            for i, j in enumerate(own_blocks(r, NB)):
                outp[b, 256 * j:256 * j + 256] = o[256 * i:256 * i + 256]
            k += 1
    return outp
```

```python
import numpy as np
from contextlib import ExitStack

import concourse.bass as bass
import concourse.mybir as mybir
from concourse.bass_utils import run_bass_kernel_spmd

F32 = mybir.dt.float32
BF16 = mybir.dt.bfloat16
U8 = mybir.dt.uint8
I32 = mybir.dt.int32
AF = mybir.ActivationFunctionType
ALU = mybir.AluOpType
AX = mybir.AxisListType

D = 1024
DIN = 3072
NH = 8
HD = 64
NE = 32
DE = 512
PLE = 256
CAP = 256
ALPHA = float(2 ** 0.25)
EPS = 1e-5
NEG = -30000.0
NEGINF = -1.0e30


class Eng:
    def __init__(self, name, sem, same_wait):
        self.name = name
        self.sem = sem
        self.count = 0
        self.waited = {}
        self.ops = []
        self.same_wait = same_wait


class Buf:
    __slots__ = ("name", "w", "r", "excl")

    def __init__(self, name, excl=False):
        self.name = name
        self.w = None
        self.r = []
        self.excl = excl


class FW:
    def __init__(self, nc, stack):
        self.nc = nc
        self.stack = stack
        self.eng = {}
        for n, sw in (("pe", False), ("act", True), ("dve", True), ("pool", True), ("sp", False)):
            s = stack.enter_context(nc.semaphore("sem_" + n))
            self.eng[n] = Eng(n, s, sw)
        self.dsems = []
        self.names = {}
        self.bc_reg = None

    def dsem(self, name):
        s = [self.stack.enter_context(self.nc.semaphore(name)), 0]
        self.dsems.append(s)
        return s

    def _wait(self, e, s, v):
        if isinstance(s, list):
            s, v = s[0], s[1]
        if v <= 0:
            return
        k = id(s)
        if e.waited.get(k, 0) < v:
            e.waited[k] = v
            e.ops.append(lambda en, s=s, v=v: en.wait_ge(s, v))

    def _deps(self, e, reads, writes):
        for b in reads:
            if b.w is not None:
                s, v = b.w
                if not (s is e.sem and not e.same_wait):
                    self._wait(e, s, v)
            if b.excl:
                for (s, v) in b.r:
                    if not (s is e.sem and not e.same_wait):
                        self._wait(e, s, v)
        for b in writes:
            if b.w is not None:
                s, v = b.w
                if not (s is e.sem and not e.same_wait):
                    self._wait(e, s, v)
            for (s, v) in b.r:
                if not (s is e.sem and not e.same_wait):
                    self._wait(e, s, v)

    def _mark(self, tok, reads, writes):
        for b in reads:
            if b.excl:
                b.r = [tok]
            else:
                b.r.append(tok)
                if len(b.r) > 64:
                    b.r = b.r[-64:]
        for b in writes:
            b.w = tok
            b.r = []

    def op(self, engname, fn, reads=(), writes=()):
        e = self.eng[engname]
        self._deps(e, reads, writes)
        e.count += 1
        tok = (e.sem, e.count)
        import sys as _sys
        line = _sys._getframe(1).f_lineno

        def run(en, fn=fn, s=e.sem, line=line):
            ins = fn(en)
            try:
                self.names[ins.ins.name] = line
            except Exception:
                pass
            return ins.then_inc(s, 1)
        e.ops.append(run)
        self._mark(tok, reads, writes)
        return tok

    def dma(self, engname, fn, sem_state, reads=(), writes=()):
        e = self.eng[engname]
        saved = []
        for b in writes:
            if b.w is not None and b.w[0] is sem_state and not b.r:
                saved.append((b, b.w))
                b.w = None
        self._deps(e, reads, writes)
        for b, w in saved:
            b.w = w
        sem_state[1] += 16
        tok = (sem_state, None)
        e.ops.append(lambda en, fn=fn, s=sem_state[0]: fn(en).then_inc(s, 16))
        self._mark(tok, reads, writes)
        return tok

    def barrier(self):
        names = ["pe", "act", "dve", "pool", "sp"]
        snap = [(self.eng[n].sem, self.eng[n].count) for n in names]
        dsnap = [(s[0], s[1]) for s in self.dsems]
        for n in names:
            e = self.eng[n]
            for (s, v) in snap:
                if s is e.sem:
                    continue
                self._wait(e, s, v)
            for (s, v) in dsnap:
                self._wait(e, s, v)

    def replay(self):
        nc = self.nc
        with nc.Block() as block:
            @block.tensor
            def _(en):
                for f in self.eng["pe"].ops:
                    f(en)

            @block.scalar
            def _(en):
                for f in self.eng["act"].ops:
                    f(en)

            @block.vector
            def _(en):
                for f in self.eng["dve"].ops:
                    f(en)

            @block.gpsimd
            def _(en):
                for f in self.eng["pool"].ops:
                    f(en)

            @block.sync
            def _(en):
                for f in self.eng["sp"].ops:
                    f(en)


def build_nc(S=8192, dbg=False, stop_after=None):
    NB = S // 256
    NSLOT = NB // 4
    NOWN = NSLOT * 256
    TOT = S + NOWN
    NT = NOWN // 128
    NCH = S // 512
    NKT = TOT // 128
    NSL = NE * CAP

    nc = bass.Bass("TRN2", target_bir_lowering=False)

    def din(name, shape, dt=F32):
        return nc.dram_tensor(name, list(shape), dt, kind="ExternalInput").ap()

    xfT = din("xfT", [D, S])
    xoT = din("xoT", [NSLOT, D, 258])
    xo = din("xo", [NSLOT, 258, D])
    po = din("po", [NSLOT, 256, PLE])
    nm1_d = din("nm1", [NSLOT * 256])
    nm2_d = din("nm2", [NSLOT * 256])
    w_in = din("w_in", [D, DIN])
    w_conv = din("w_conv", [3, 512])
    w_out = din("w_out", [D, D])
    ln1_g = din("ln1_g", [D])
    ln1_b = din("ln1_b", [D])
    w_rg = din("w_rg", [D, 4])
    b_rg = din("b_rg", [4])
    w_re = din("w_re", [D, NE])
    b_re = din("b_re", [NE])
    w_gate = din("w_gate", [NE, D, DE])
    w_up = din("w_up", [NE, D, DE])
    w_down = din("w_down", [NE, DE, D])
    w_pg = din("w_pg", [D, D])
    w_pp = din("w_pp", [PLE, D])
    ln2_g = din("ln2_g", [D])
    ln2_b = din("ln2_b", [D])
    out = nc.dram_tensor("out", [NOWN, D], F32, kind="ExternalOutput").ap()
    if dbg:
        dbg_x1 = nc.dram_tensor("dbg_x1", [NOWN, D], F32, kind="ExternalOutput").ap()
        dbg_mix = nc.dram_tensor("dbg_mix", [NOWN, D], F32, kind="ExternalOutput").ap()

    KT = nc.dram_tensor("KT_scr", [NH * HD, TOT], BF16).ap()
    VV = nc.dram_tensor("VV_scr", [NH, 128, NKT, 65], BF16).ap()
    XS = nc.dram_tensor("XS_scr", [NSL, D], BF16).ap()
    YS = nc.dram_tensor("YS_scr", [NSL, D], F32).ap()
    ACCD = nc.dram_tensor("ACC_scr", [NOWN, D], F32).ap()

    st = ExitStack()
    with st:
        fw = FW(nc, st)
        ARENA = 98304 + 59392 + 20480 + 8192
        arena = st.enter_context(nc.sbuf_tensor("arena", [128, ARENA], U8))
        R0, R1, R2, RC = 0, 98304, 98304 + 59392, 98304 + 59392 + 20480

        class Alloc:
            def __init__(self, base, size):
                self.base, self.size, self.off = base, size, 0

            def __call__(self, nelem, dtype, pat=None, **kw):
                esz = 4 if dtype in (F32, I32) else 2
                nbytes = nelem * esz
                o = self.base + self.off
                self.off += (nbytes + 63) // 64 * 64
                assert self.off <= self.size, ("arena overflow", self.base, self.off, self.size)
                a = arena[:, o:o + nbytes].bitcast(dtype)
                if pat:
                    a = a.rearrange(pat, **kw)
                return a

        psall = st.enter_context(nc.psum_tensor("psall", [128, 4096], F32))
        psf = [psall[:, i * 512:(i + 1) * 512] for i in range(6)]
        psb = [psall[:, (6 + i) * 512:(7 + i) * 512].bitcast(BF16) for i in range(2)]
        psw = [psall[:, j * 1024:(j + 1) * 1024] for j in range(2)]
        pse = psall[:, 6 * 512:7 * 512]
        PW = [Buf("psw%d" % j, excl=True) for j in range(2)]
        PF = [Buf("psf%d" % i, excl=True) for i in range(6)]
        PB = [Buf("psb%d" % i, excl=True) for i in range(2)]

        def alt(i):
            return "act" if i % 2 == 0 else "dve"


        def MM(o, l, r, st_, sp_):
            return lambda e: e.matmul(o, lhsT=l, rhs=r, start=st_, stop=sp_)

        def TR(o, i_, idn):
            return lambda e: e.transpose(out=o, in_=i_, identity=idn)

        def ACTF(o, i_, f, **kw):
            return lambda e: e.activation(out=o, in_=i_, func=f, **kw)

        def TT(o, a, b, op):
            return lambda e: e.tensor_tensor(out=o, in0=a, in1=b, op=op)

        def TS(o, a, s1, s2, op0, op1=None):
            if op1 is None:
                return lambda e: e.tensor_scalar(out=o, in0=a, scalar1=s1, scalar2=None, op0=op0)
            return lambda e: e.tensor_scalar(out=o, in0=a, scalar1=s1, scalar2=s2, op0=op0, op1=op1)

        def STT(o, a, sc, b, op0, op1):
            return lambda e: e.scalar_tensor_tensor(out=o, in0=a, scalar=sc, in1=b, op0=op0, op1=op1)

        def MS(ap, v):
            return lambda e: e.memset(ap, v)

        def RD(o, i_, op):
            return lambda e: e.tensor_reduce(out=o, in_=i_, axis=AX.X, op=op)

        def MX(o, i_):
            return lambda e: e.max(out=o, in_=i_)

        def RCP(o, i_):
            return lambda e: e.reciprocal(out=o, in_=i_)

        def DM(o, i_):
            return lambda e: e.dma_start(out=o, in_=i_)

        def CPY(o, i_):
            return lambda e: e.tensor_copy(out=o, in_=i_)

        def ASEL(o, i_, pattern, cmp, fill, base, cm):
            return lambda e: e.affine_select(out=o, in_=i_, pattern=pattern, compare_op=cmp, fill=fill, base=base, channel_multiplier=cm)

        def BNS(o, i_):
            return lambda e: e.bn_stats(out=o, in_=i_)

        def BNA(o, i_):
            return lambda e: e.bn_aggr(out=o, in_=i_)

        def SCAT(dst, idx, src, bc):
            return lambda e: e.indirect_dma_start(out=dst, out_offset=bass.IndirectOffsetOnAxis(ap=idx, axis=0), in_=src, in_offset=None, bounds_check=bc, oob_is_err=False)

        def GATH(dst, src, idx, bc):
            return lambda e: e.indirect_dma_start(out=dst, out_offset=None, in_=src, in_offset=bass.IndirectOffsetOnAxis(ap=idx, axis=0), bounds_check=bc, oob_is_err=False)


        def _L_matmul(o, lhsT=None, rhs=None, start=None, stop=None):
            return lambda e: e.matmul(o, lhsT=lhsT, rhs=rhs, start=start, stop=stop)

        def _mk(meth):
            def f(*a, **kw):
                return lambda e: getattr(e, meth)(*a, **kw)
            return f

        _L_transpose = _mk("transpose")
        _L_activation = _mk("activation")
        _L_tensor_tensor = _mk("tensor_tensor")
        _L_tensor_scalar = _mk("tensor_scalar")
        _L_scalar_tensor_tensor = _mk("scalar_tensor_tensor")
        _L_memset = _mk("memset")
        _L_tensor_reduce = _mk("tensor_reduce")
        _L_max = _mk("max")
        _L_reciprocal = _mk("reciprocal")
        _L_dma_start = _mk("dma_start")
        _L_tensor_copy = _mk("tensor_copy")
        _L_affine_select = _mk("affine_select")
        _L_bn_stats = _mk("bn_stats")
        _L_bn_aggr = _mk("bn_aggr")
        def _L_indirect_dma_start(*a, **kw):
            def f(e):
                if fw.bc_reg is None:
                    fw.bc_reg = e.to_reg(kw["bounds_check"])
                kw2 = dict(kw)
                kw2["bounds_check"] = fw.bc_reg
                return e.indirect_dma_start(*a, **kw2)
            return f
        _L_iota = _mk("iota")

        def copy_op(eng, out_ap, in_ap):
            if eng == "act":
                return lambda e: e.activation(out=out_ap, in_=in_ap, func=AF.Copy)
            return lambda e: e.tensor_copy(out=out_ap, in_=in_ap)

        ac = Alloc(RC, 8192)
        ident_f = ac(128, F32)
        ident_b = ac(128, BF16)
        ones_f = ac(128, F32)
        CBm = ac(512, BF16, "p (t q) -> p t q", t=2)
        wc = ac(12, F32, "p (c k) -> p c k", c=4)
        km = ac(4 * 32, F32, "p (c n) -> p c n", c=4)
        kmb = ac(4 * 32, BF16, "p (c n) -> p c n", c=4)
        kmh = ac(8 * 32, BF16, "p (h n) -> p h n", h=8)
        slot_i = ac(NT * 2, I32, "p (t k) -> p t k", k=2)
        w12 = ac(NT * 2, F32, "p (t k) -> p t k", k=2)
        Lst = ac(128, F32)
        eC = ac(32, F32)
        Spre = ac(32, F32)
        bias36 = ac(36, F32)
        ztile = ac(1024, BF16)
        B_ident = Buf("ident")
        B_const = Buf("const")
        B_km = Buf("km")
        B_kmh = Buf("kmh")
        B_slot = [Buf("slot%d" % t) for t in range(NT)]
        B_w12 = [Buf("w12_%d" % t) for t in range(NT)]
        B_Spre = Buf("Spre")

        s_setup = fw.dsem("setup")

        fw.op("pool", _L_memset(ident_f, 0.0), writes=[B_ident])
        fw.op("pool", _L_affine_select(out=ident_f, in_=ident_f, pattern=[[-1, 128]], compare_op=ALU.not_equal, fill=1.0, base=0, channel_multiplier=1), reads=[B_ident], writes=[B_ident])
        fw.op("pool", _L_tensor_copy(out=ident_b, in_=ident_f), reads=[B_ident], writes=[B_const])
        fw.op("pool", _L_memset(ones_f, 1.0), writes=[B_const])
        fw.op("pool", _L_memset(CBm, 0.0), writes=[B_const])
        fw.op("pool", _L_affine_select(out=CBm, in_=CBm, pattern=[[-128, 2], [1, 256]], compare_op=ALU.is_ge, fill=NEG, base=0, channel_multiplier=-1), reads=[B_const], writes=[B_const])
        fw.op("pool", _L_memset(Lst, 1.0), writes=[B_const])
        fw.op("pool", _L_affine_select(out=Lst, in_=Lst, pattern=[[1, 128]], compare_op=ALU.is_ge, fill=0.0, base=-1, channel_multiplier=-1), reads=[B_const], writes=[B_const])
        fw.op("pool", _L_iota(eC, pattern=[[CAP, 32]], base=0, channel_multiplier=0, allow_small_or_imprecise_dtypes=True), writes=[B_const])
        fw.op("pool", _L_memset(Spre, 0.0), writes=[B_Spre])
        for c_ in range(4):
            for k_ in range(3):
                fw.dma("sp", _L_dma_start(out=wc[:, c_, k_:k_ + 1], in_=w_conv[k_, c_ * 128:(c_ + 1) * 128].rearrange("(p o) -> p o", o=1)), s_setup, writes=[B_const])
        fw.dma("sp", _L_dma_start(out=bias36[:, 0:4], in_=b_rg.partition_broadcast(128)), s_setup, writes=[B_const])
        fw.dma("sp", _L_dma_start(out=bias36[:, 4:36], in_=b_re.partition_broadcast(128)), s_setup, writes=[B_const])

        a0 = Alloc(R0, 98304)
        w_in_bf = a0(8 * DIN, BF16, "p (c n) -> p c n", c=8)
        Qaug = Alloc(R0 + 49152, 32768)(8 * NOWN, BF16, "p (h n) -> p h n", h=8)
        yconvT = Alloc(R0 + 81920, 16384)(4 * NOWN, BF16, "p (c n) -> p c n", c=4)
        B_win = [Buf("win%d" % c) for c in range(8)]
        B_Q = [Buf("Q%d" % i) for i in range(NSLOT)]
        B_yc = [Buf("yc%d" % i) for i in range(NSLOT)]

        a1 = Alloc(R1, 59392)
        wst = [a1(DIN, F32) for _ in range(2)]
        B_wst = [Buf("wst%d" % i) for i in range(2)]
        s_wst = [fw.dsem("wst%d" % i) for i in range(2)]
        B_z = Buf("ztile")
        s_z = fw.dsem("zfill")
        B_XS = Buf("XS")

        fw.op("pool", _L_memset(ztile, 0.0), writes=[B_z])
        XSv = XS.rearrange("(t p) d -> t p d", p=128)
        for c in range(8):
            sl = c % 2
            fw.dma("sp", _L_dma_start(out=wst[sl], in_=w_in[c * 128:(c + 1) * 128, :]), s_wst[sl], writes=[B_wst[sl]])
            for k, eng in enumerate(("pool", "act", "dve")):
                o, i_ = w_in_bf[:, c, k * 1024:(k + 1) * 1024], wst[sl][:, k * 1024:(k + 1) * 1024]
                fw.op(eng, copy_op(eng, o, i_), reads=[B_wst[sl]], writes=[B_win[c]])
        fw.barrier()

        a1 = Alloc(R1, 59392)
        xs = [a1(8 * 512, F32, "p (c n) -> p c n", c=8) for _ in range(2)]
        xT = [a1(8 * 512, BF16, "p (c n) -> p c n", c=8) for _ in range(2)]
        KTst = [a1(4 * 512, BF16, "p (c n) -> p c n", c=4) for _ in range(2)]
        a2 = Alloc(R2, 20480)
        Vst = [a2(8 * 4 * 65, BF16, "p (h k e) -> p h k e", h=8, k=4) for _ in range(2)]
        B_xs = [Buf("xs%d" % i) for i in range(2)]
        B_xT = [[Buf("xT%d_%d" % (i, g)) for g in range(3)] for i in range(2)]
        XG = ((("act", 0, 3), ("dve", 3, 6), ("pool", 6, 8)))

        def xg(c):
            return min(c // 3, 2)
        B_KTst = [Buf("KTst%d" % i) for i in range(2)]
        B_Vst = [Buf("Vst%d" % i) for i in range(2)]
        s_xs = [fw.dsem("xs%d" % i) for i in range(2)]
        s_kst = [fw.dsem("kst%d" % i) for i in range(2)]
        s_vst = [fw.dsem("vst%d" % i) for i in range(2)]
        B_KT = Buf("KT")
        B_VV = Buf("VV")
        for i in range(2):
            fw.op("pool", _L_memset(Vst[i], 1.0), writes=[B_Vst[i]])
        fw.op("pool", _L_memset(km, 0.0), writes=[B_km])
        fw.op("pool", _L_memset(Qaug[64:128], 0.0), writes=B_Q)
        ZF_PER = (NSL // 128 + NCH - 1) // NCH

        KTv = KT.rearrange("(c p) n -> p c n", p=128)
        VVv = VV.rearrange("h p k e -> p h k e")
        xfTv = xfT.rearrange("(c p) n -> p c n", p=128)

        def load_chunk(t):
            sl = t % 2
            fw.dma("sp", _L_dma_start(out=xs[sl], in_=xfTv[:, :, t * 512:(t + 1) * 512]), s_xs[sl], writes=[B_xs[sl]])

        def cast_chunk(t):
            sl_ = t % 2
            for g, (eng, c0, c1) in enumerate(XG):
                fw.op(eng, copy_op(eng, xT[sl_][:, c0:c1, :], xs[sl_][:, c0:c1, :]), reads=[B_xs[sl_]], writes=[B_xT[sl_][g]])

        ev = [0]
        load_chunk(0)
        load_chunk(1)
        cast_chunk(0)
        for t in range(NCH):
            sl = t % 2
            if t + 2 < NCH:
                load_chunk(t + 2)
            for zt in range(t * ZF_PER, min((t + 1) * ZF_PER, NSL // 128)):
                fw.dma("act", _L_dma_start(out=XSv[zt], in_=ztile), s_z, reads=[B_z], writes=[B_XS])
            for pr in range(4):
                bk = 2 + pr % 2
                for c in range(8):
                    fw.op("pe", _L_matmul(psf[bk][:, :], lhsT=w_in_bf[:, c, 2048 + pr * 128:2048 + (pr + 1) * 128], rhs=xT[sl][:, c, :], start=(c == 0), stop=(c == 7)), reads=[B_xT[sl][xg(c)], B_win[c]], writes=[PF[bk]])
                fw.op("act", copy_op("act", KTst[sl][:, pr, :], psf[bk][:, :]), reads=[PF[bk]], writes=[B_KTst[sl]])
                fw.op("dve", _L_tensor_reduce(out=km[:, pr, 2 * t:2 * t + 2], in_=psf[bk][:, :].rearrange("p (a b) -> p a b", a=2), axis=AX.X, op=ALU.add), reads=[PF[bk]], writes=[B_km])
            fw.dma("sp", _L_dma_start(out=KTv[:, :, t * 512:(t + 1) * 512], in_=KTst[sl]), s_kst[sl], reads=[B_KTst[sl]], writes=[])
            for q in range(4):
                bk = 4 + q % 2
                for c in range(8):
                    fw.op("pe", _L_matmul(psf[bk][:, :], lhsT=xT[sl][:, c, q * 128:(q + 1) * 128], rhs=w_in_bf[:, c, 2560:3072], start=(c == 0), stop=(c == 7)), reads=[B_xT[sl][xg(c)], B_win[c]], writes=[PF[bk]])
                eng = alt(ev[0]); ev[0] += 1
                fw.op(eng, copy_op(eng, Vst[sl][:, :, q, 0:64], psf[bk][:, :].rearrange("p (h e) -> p h e", h=8)), reads=[PF[bk]], writes=[B_Vst[sl]])
                if q == 1 and t + 1 < NCH:
                    cast_chunk(t + 1)
            fw.dma("sp", _L_dma_start(out=VVv[:, :, 4 * t:4 * t + 4, :], in_=Vst[sl]), s_vst[sl], reads=[B_Vst[sl]], writes=[])
        fw.barrier()

        a1 = Alloc(R1, 59392)
        xos = [a1(8 * 258, F32, "p (c n) -> p c n", c=8) for _ in range(2)]
        xTo = [a1(8 * 258, BF16, "p (c n) -> p c n", c=8) for _ in range(2)]
        KTst2 = [a1(4 * 256, BF16, "p (c n) -> p c n", c=4) for _ in range(2)]
        Vst2 = [a1(8 * 2 * 65, BF16, "p (h k e) -> p h k e", h=8, k=2) for _ in range(2)]
        ccs = [a1(258, F32) for _ in range(2)]
        uu = [a1(258, F32) for _ in range(2)]
        tt_ = [a1(256, F32) for _ in range(2)]
        B_xos = [Buf("xos%d" % i) for i in range(2)]
        B_xTo = [[Buf("xTo%d_%d" % (i, g)) for g in range(3)] for i in range(2)]
        B_K2 = [Buf("K2_%d" % i) for i in range(2)]
        B_V2 = [Buf("V2_%d" % i) for i in range(2)]
        B_cc = [Buf("cc%d" % i) for i in range(2)]
        B_uu = [Buf("uu%d" % i) for i in range(2)]
        B_tt = [Buf("tt%d" % i) for i in range(2)]
        s_xos = [fw.dsem("xos%d" % i) for i in range(2)]
        s_k2 = [fw.dsem("k2_%d" % i) for i in range(2)]
        s_v2 = [fw.dsem("v2_%d" % i) for i in range(2)]
        for i in range(2):
            fw.op("pool", _L_memset(Vst2[i], 1.0), writes=[B_V2[i]])

        def load_own(i):
            sl = i % 2
            fw.dma("sp", _L_dma_start(out=xos[sl], in_=xoT[i].rearrange("(c p) n -> p c n", p=128)), s_xos[sl], writes=[B_xos[sl]])

        def cast_own(i_):
            sl_ = i_ % 2
            for g, (eng, c0_, c1_) in enumerate(XG):
                fw.op(eng, copy_op(eng, xTo[sl_][:, c0_:c1_, :], xos[sl_][:, c0_:c1_, :]), reads=[B_xos[sl_]], writes=[B_xTo[sl_][g]])

        load_own(0)
        cvi = [0]
        for i in range(NSLOT):
            sl = i % 2
            if i + 1 < NSLOT:
                load_own(i + 1)
            c0 = i * 256
            if i == 0:
                cast_own(0)
            for hp in range(4):
                bk = 2 + hp % 2
                for hh in range(2):
                    h = 2 * hp + hh
                    for c in range(8):
                        fw.op("pe", _L_matmul(psf[bk][0:64, hh * 256:(hh + 1) * 256], lhsT=w_in_bf[:, c, 1536 + h * 64:1536 + (h + 1) * 64], rhs=xTo[sl][:, c, 2:258], start=(c == 0), stop=(c == 7)), reads=[B_xTo[sl][xg(c)], B_win[c]], writes=[PF[bk]])
                eng = alt(ev[0]); ev[0] += 1
                fw.op(eng, copy_op(eng, Qaug[0:64, 2 * hp:2 * hp + 2, c0:c0 + 256], psf[bk][0:64, :].rearrange("p (a b) -> p a b", a=2)), reads=[PF[bk]], writes=[B_Q[i]])
            for pp in range(2):
                bk = 4 + pp % 2
                for q in range(2):
                    pr = 2 * pp + q
                    for c in range(8):
                        fw.op("pe", _L_matmul(psf[bk][:, q * 256:(q + 1) * 256], lhsT=w_in_bf[:, c, 2048 + pr * 128:2048 + (pr + 1) * 128], rhs=xTo[sl][:, c, 2:258], start=(c == 0), stop=(c == 7)), reads=[B_xTo[sl][xg(c)], B_win[c]], writes=[PF[bk]])
                eng = alt(ev[0]); ev[0] += 1
                fw.op(eng, copy_op(eng, KTst2[sl][:, 2 * pp:2 * pp + 2, :], psf[bk][:, :].rearrange("p (a b) -> p a b", a=2)), reads=[PF[bk]], writes=[B_K2[sl]])
            fw.dma("sp", _L_dma_start(out=KTv[:, :, S + c0:S + c0 + 256], in_=KTst2[sl]), s_k2[sl], reads=[B_K2[sl]], writes=[])
            if i + 1 < NSLOT:
                cast_own(i + 1)
            for q in range(2):
                bk = 2 + q % 2
                for c in range(8):
                    fw.op("pe", _L_matmul(psf[bk][:, :], lhsT=xTo[sl][:, c, 2 + q * 128:2 + (q + 1) * 128], rhs=w_in_bf[:, c, 2560:3072], start=(c == 0), stop=(c == 7)), reads=[B_xTo[sl][xg(c)], B_win[c]], writes=[PF[bk]])
                eng = alt(ev[0]); ev[0] += 1
                fw.op(eng, copy_op(eng, Vst2[sl][:, :, q, 0:64], psf[bk][:, :].rearrange("p (h e) -> p h e", h=8)), reads=[PF[bk]], writes=[B_V2[sl]])
            fw.dma("sp", _L_dma_start(out=VVv[:, :, S // 128 + 2 * i:S // 128 + 2 * i + 2, :], in_=Vst2[sl]), s_v2[sl], reads=[B_V2[sl]], writes=[])
            for cc in range(4):
                k2 = cvi[0] % 2
                cvi[0] += 1
                banks = (0, 1, 4) if cc % 2 == 0 else (5, 2, 3)
                specs = ((banks[0], 0 + cc * 128, 2, 256), (banks[1], 512 + cc * 128, 0, 258), (banks[2], 1024 + cc * 128, 0, 258))
                for (bk, col, o0, n) in specs:
                    for c in range(8):
                        fw.op("pe", _L_matmul(psf[bk][:, 0:n], lhsT=w_in_bf[:, c, col:col + 128], rhs=xTo[sl][:, c, o0:o0 + n], start=(c == 0), stop=(c == 7)), reads=[B_xTo[sl][xg(c)], B_win[c]], writes=[PF[bk]])
                bcb, bcc, bch = banks
                fw.op("act", copy_op("act", ccs[k2], psf[bcc][:, 0:258]), reads=[PF[bcc]], writes=[B_cc[k2]])
                fw.op("dve", _L_tensor_tensor(out=uu[k2], in0=ccs[k2], in1=psf[bch][:, 0:258], op=ALU.mult), reads=[B_cc[k2], PF[bch]], writes=[B_uu[k2]])
                fw.op("dve", _L_tensor_scalar(out=tt_[k2], in0=uu[k2][:, 2:258], scalar1=wc[:, cc, 2:3], scalar2=None, op0=ALU.mult), reads=[B_uu[k2], B_const], writes=[B_tt[k2]])
                fw.op("dve", _L_scalar_tensor_tensor(out=tt_[k2], in0=uu[k2][:, 1:257], scalar=wc[:, cc, 1:2], in1=tt_[k2], op0=ALU.mult, op1=ALU.add), reads=[B_uu[k2], B_const, B_tt[k2]], writes=[B_tt[k2]])
                fw.op("dve", _L_scalar_tensor_tensor(out=tt_[k2], in0=uu[k2][:, 0:256], scalar=wc[:, cc, 0:1], in1=tt_[k2], op0=ALU.mult, op1=ALU.add), reads=[B_uu[k2], B_const, B_tt[k2]], writes=[B_tt[k2]])
                fw.op("dve", _L_tensor_tensor(out=yconvT[:, cc, c0:c0 + 256], in0=tt_[k2], in1=psf[bcb][:, 0:256], op=ALU.mult), reads=[B_tt[k2], PF[bcb]], writes=[B_yc[i]])
        fw.barrier()

        a2 = Alloc(R2, 20480)
        nm1 = a2(NSLOT * 256, F32, "p (i n) -> p i n", i=NSLOT)
        nm2 = a2(NSLOT * 256, F32, "p (i n) -> p i n", i=NSLOT)
        gm = [a2(256, F32, "p (h n) -> p h n", h=8) for _ in range(2)]
        m8 = [a2(64, F32, "p (h n) -> p h n", h=8) for _ in range(2)]
        a1 = Alloc(R1, 59392)
        bpad = [a1(8 * 128, BF16, "p (h n) -> p h n", h=8) for _ in range(2)]
        tA3 = [a1(256, F32, "p (h n) -> p h n", h=8) for _ in range(2)]
        tB3 = [a1(256, F32, "p (h n) -> p h n", h=8) for _ in range(2)]
        mm3 = [a1(24, F32, "p (r h) -> p r h", r=3) for _ in range(2)]
        B_nm = Buf("nm")
        B_gm = [Buf("gm%d" % i) for i in range(2)]
        B_m8 = [Buf("m8%d" % i) for i in range(2)]
        B_bp = [Buf("bp%d" % i) for i in range(2)]
        s_nm = fw.dsem("nm")
        s_kmh = fw.dsem("kmh")
        fw.dma("sp", _L_dma_start(out=nm1.rearrange("p i n -> p (i n)"), in_=nm1_d.partition_broadcast(128)), s_nm, writes=[B_nm])
        fw.dma("sp", _L_dma_start(out=nm2.rearrange("p i n -> p (i n)"), in_=nm2_d.partition_broadcast(128)), s_nm, writes=[B_nm])
        for i in range(2):
            fw.op("pool", _L_memset(bpad[i], 0.0), writes=[B_bp[i]])
        fw.op("dve", _L_tensor_copy(out=kmb, in_=km), reads=[B_km], writes=[B_km])
        kmh_v = kmh.rearrange("p (a b) n -> p a b n", b=2)
        fw.dma("sp", _L_dma_start(out=kmh_v[0:64, :, 0, :], in_=kmb[0:64, :, :]), s_kmh, reads=[B_km], writes=[B_kmh])
        fw.dma("sp", _L_dma_start(out=kmh_v[0:64, :, 1, :], in_=kmb[64:128, :, :]), s_kmh, reads=[B_km], writes=[B_kmh])
        for i in range(NSLOT):
            for t in range(2):
                k2 = (2 * i + t) % 2
                q0 = i * 256 + t * 128
                gb = k2
                for h in range(8):
                    fw.op("pe", _L_matmul(psf[gb][:, h * 32:(h + 1) * 32], lhsT=Qaug[0:64, h, q0:q0 + 128], rhs=kmh[0:64, h, :], start=True, stop=True), reads=[B_Q[i], B_kmh], writes=[PF[gb]])
                fw.op("dve", _L_tensor_tensor(out=gm[k2].rearrange("p h n -> p (h n)"), in0=psf[gb][:, 0:256], in1=nm1[:, i, :], op=ALU.add), reads=[PF[gb], B_nm], writes=[B_gm[k2]])
                G, A_, B_ = gm[k2], tA3[k2], tB3[k2]
                rd = [B_gm[k2], B_m8[k2]]
                wr_ = [B_m8[k2]]

                def bc(r):
                    return mm3[k2][:, r, :].unsqueeze(2).to_broadcast([128, 8, 32])
                fw.op("dve", _L_tensor_reduce(out=mm3[k2][:, 0, :], in_=G, axis=AX.X, op=ALU.max), reads=rd, writes=wr_)
                fw.op("dve", _L_tensor_tensor(out=A_, in0=G, in1=bc(0), op=ALU.is_ge), reads=rd, writes=wr_)
                fw.op("dve", _L_scalar_tensor_tensor(out=B_, in0=A_, scalar=-1.0e9, in1=G, op0=ALU.mult, op1=ALU.add), reads=rd, writes=wr_)
                fw.op("dve", _L_tensor_reduce(out=mm3[k2][:, 1, :], in_=B_, axis=AX.X, op=ALU.max), reads=rd, writes=wr_)
                fw.op("dve", _L_tensor_tensor(out=A_, in0=B_, in1=bc(1), op=ALU.is_ge), reads=rd, writes=wr_)
                fw.op("dve", _L_scalar_tensor_tensor(out=B_, in0=A_, scalar=-1.0e9, in1=B_, op0=ALU.mult, op1=ALU.add), reads=rd, writes=wr_)
                fw.op("dve", _L_tensor_reduce(out=mm3[k2][:, 2, :], in_=B_, axis=AX.X, op=ALU.max), reads=rd, writes=wr_)
                fw.op("dve", _L_tensor_tensor(out=A_, in0=G, in1=bc(2), op=ALU.is_lt), reads=rd, writes=wr_)
                fw.op("dve", _L_scalar_tensor_tensor(out=bpad[k2][:, :, 64:96], in0=A_, scalar=NEG, in1=nm2[:, i, :].rearrange("p (h n) -> p h n", h=8), op0=ALU.mult, op1=ALU.add), reads=rd + [B_nm], writes=[B_bp[k2]])
                pb = k2
                for h in range(8):
                    fw.op("pe", _L_transpose(out=psb[pb][:, h * 128:(h + 1) * 128], in_=bpad[k2][:, h, :], identity=ident_b), reads=[B_bp[k2], B_const], writes=[PB[pb]])
                fw.op("act", copy_op("act", Qaug[64:96, :, q0:q0 + 128], psb[pb][64:96, :].rearrange("p (h n) -> p h n", h=8)), reads=[PB[pb]], writes=[B_Q[i]])
        fw.barrier()

        if stop_after == "A":
            pass

        aB0 = Alloc(R0, 49152)
        KTaug = [aB0(TOT, BF16) for _ in range(2)]
        aB1 = Alloc(R1, 59392)
        yattnT = aB1(8 * NOWN, BF16, "p (h n) -> p h n", h=8)
        aB1.off = 32768
        Vh = [aB1(NKT * 65, BF16, "p (k e) -> p k e", e=65) for _ in range(2)]
        Pt = [aB1(1024, BF16) for _ in range(2)]
        aB2 = Alloc(R2, 20480)
        rcp = [aB2(256, F32) for _ in range(2)]
        bcs = [aB2(256, F32) for _ in range(2)]
        B_KTa = [Buf("KTa%d" % i) for i in range(2)]
        B_Vh = [Buf("Vh%d" % i) for i in range(2)]
        B_Pt = [Buf("Pt%d" % i) for i in range(2)]
        B_rcp = [Buf("rcp%d" % i) for i in range(2)]
        B_bcs = [Buf("bcs%d" % i) for i in range(2)]
        B_ya = [[Buf("ya%d_%d" % (h, i)) for i in range(NSLOT)] for h in range(8)]
        s_kta = [fw.dsem("kta%d" % i) for i in range(2)]
        s_vh = [fw.dsem("vh%d" % i) for i in range(2)]
        for s_ in range(2):
            fw.op("pool", _L_memset(KTaug[s_][64:128, :], 0.0), writes=[B_KTa[s_]])
            fw.op("pool", _L_memset(KTaug[s_][64:96, 0:S], 1.0), writes=[B_KTa[s_]])
            fw.op("pool", _L_affine_select(out=KTaug[s_][64:96, 0:S], in_=KTaug[s_][64:96, 0:S], pattern=[[1, S]], compare_op=ALU.is_ge, fill=0.0, base=0, channel_multiplier=-256), reads=[B_KTa[s_]], writes=[B_KTa[s_]])
            fw.op("pool", _L_affine_select(out=KTaug[s_][64:96, 0:S], in_=KTaug[s_][64:96, 0:S], pattern=[[-1, S]], compare_op=ALU.is_ge, fill=0.0, base=255, channel_multiplier=256), reads=[B_KTa[s_]], writes=[B_KTa[s_]])
        KTh = KT.rearrange("(h d) n -> h d n", d=64)

        def load_head(h):
            s_ = h % 2
            fw.dma("sp", _L_dma_start(out=KTaug[s_][0:64, :], in_=KTh[h]), s_kta[s_], reads=[B_KT], writes=[B_KTa[s_]])
            fw.dma("sp", _L_dma_start(out=Vh[s_], in_=VV[h]), s_vh[s_], reads=[B_VV], writes=[B_Vh[s_]])

        load_head(0)
        epi_pending = []
        sb_i = [0]
        ob_i = [0]
        pt_i = [0]
        for h in range(8):
            s_ = h % 2
            if h + 1 < 8:
                load_head(h + 1)
            for i in range(NSLOT):
                q0 = i * 256
                units = [256 * n for n in range(4 * i + 3)] + [S + q0]
                nu = len(units)
                ob = 4 + ob_i[0] % 2
                ob_i[0] += 1
                ndu = nu // 2

                def qk2(du):
                    j = sb_i[0] % 2
                    sb_i[0] += 1
                    for k in range(2):
                        u = 2 * du + k
                        kc = units[u]
                        own = (u == nu - 1)
                        for t in range(2):
                            o_ap = psw[j][:, k * 512 + t * 256:k * 512 + (t + 1) * 256]
                            fw.op("pe", _L_matmul(o_ap, lhsT=KTaug[s_][:, kc + t * 128:kc + (t + 1) * 128], rhs=Qaug[:, h, q0:q0 + 256], start=True, stop=(not own)), reads=[B_KTa[s_], B_Q[i]], writes=[PW[j]])
                            if own:
                                fw.op("pe", _L_matmul(o_ap, lhsT=ident_b, rhs=CBm[:, t, :], start=False, stop=True), reads=[B_const], writes=[PW[j]])
                    return j

                def expo2(j):
                    pi = pt_i[0] % 2
                    pt_i[0] += 1
                    fw.op("act", _L_activation(out=Pt[pi], in_=psw[j], func=AF.Exp, scale=0.125), reads=[PW[j]], writes=[B_Pt[pi]])
                    return pi

                def pv2(du, pi):
                    for k in range(2):
                        u = 2 * du + k
                        kc = units[u]
                        for t in range(2):
                            kt = kc // 128 + t
                            fw.op("pe", _L_matmul(psf[ob][0:65, 0:256], lhsT=Vh[s_][:, kt, :], rhs=Pt[pi][:, k * 512 + t * 256:k * 512 + (t + 1) * 256], start=(u == 0 and t == 0), stop=(u == nu - 1 and t == 1)), reads=[B_Vh[s_], B_Pt[pi]], writes=[PF[ob]])

                js = [qk2(0)]
                for du in range(ndu):
                    if du + 1 < ndu:
                        js.append(qk2(du + 1))
                    pi = expo2(js[du])
                    pv2(du, pi)
                    if du == 0 and epi_pending:
                        epi_pending.pop()()
                k2 = ob_i[0] % 2
                fw.op("dve", _L_reciprocal(out=rcp[k2][64:65, :], in_=psf[ob][64:65, 0:256]), reads=[PF[ob]], writes=[B_rcp[k2]])

                def epilogue(k2=k2, ob=ob, h=h, i=i, q0=q0):
                    fw.op("pe", _L_matmul(pse[0:64, 0:256], lhsT=ones_f[64:65, 0:64], rhs=rcp[k2][64:65, :], start=True, stop=True), reads=[B_rcp[k2], B_const], writes=[PB[0]])
                    fw.op("dve", copy_op("dve", bcs[k2][0:64, :], pse[0:64, 0:256]), reads=[PB[0]], writes=[B_bcs[k2]])
                    fw.op("dve", _L_tensor_tensor(out=yattnT[0:64, h, q0:q0 + 256], in0=psf[ob][0:64, 0:256], in1=bcs[k2][0:64, :], op=ALU.mult), reads=[PF[ob], B_bcs[k2]], writes=[B_ya[h][i]])
                epi_pending.append(epilogue)
        while epi_pending:
            epi_pending.pop()()
        fw.barrier()

        aC = Alloc(R0, 81920)
        wo_c = aC(4 * D, BF16, "p (c n) -> p c n", c=4)
        wo_a = aC(8 * D, BF16, "p (c n) -> p c n", c=8)
        wpg = aC(8 * D, BF16, "p (c n) -> p c n", c=8)
        wpp = aC(2 * D, BF16, "p (c n) -> p c n", c=2)
        wr = aC(8 * 36, F32, "p (c n) -> p c n", c=8)
        lng1 = aC(D, F32)
        lnb1 = aC(D, F32)
        cst = [aC(D, F32) for _ in range(4)]
        B_wC = Buf("wC")
        B_cst = [Buf("cst%d" % i) for i in range(4)]
        s_cst = [fw.dsem("cst%d" % i) for i in range(4)]
        s_wC = fw.dsem("wCs")
        fw.dma("sp", _L_dma_start(out=lng1, in_=ln1_g.partition_broadcast(128)), s_wC, writes=[B_wC])
        fw.dma("sp", _L_dma_start(out=lnb1, in_=ln1_b.partition_broadcast(128)), s_wC, writes=[B_wC])
        with nc.allow_non_contiguous_dma(reason="small router weights"):
            fw.dma("sp", _L_dma_start(out=wr[:, :, 0:4], in_=w_rg.rearrange("(c p) n -> p c n", p=128)), s_wC, writes=[B_wC])
            fw.dma("sp", _L_dma_start(out=wr[:, :, 4:36], in_=w_re.rearrange("(c p) n -> p c n", p=128)), s_wC, writes=[B_wC])
        pieces = []
        for c in range(4):
            pieces.append((w_out[c * 128:(c + 1) * 128, :], wo_c[:, c, :], 128))
        for hh in range(8):
            pieces.append((w_out[512 + hh * 64:512 + (hh + 1) * 64, :], wo_a[0:64, hh, :], 64))
        for c in range(8):
            pieces.append((w_pg[c * 128:(c + 1) * 128, :], wpg[:, c, :], 128))
        for c in range(2):
            pieces.append((w_pp[c * 128:(c + 1) * 128, :], wpp[:, c, :], 128))
        for k, (src, dst, npart) in enumerate(pieces):
            sl = k % 4
            fw.dma("sp", _L_dma_start(out=cst[sl][0:npart, :], in_=src), s_cst[sl], writes=[B_cst[sl]])
            eng = ("pool", "act", "dve")[k % 3]
            fw.op(eng, copy_op(eng, dst, cst[sl][0:npart, :]), reads=[B_cst[sl]], writes=[B_wC])

        aC1 = Alloc(R1 + 32768, 59392 - 32768)
        aC2 = Alloc(R2, 20480)
        xr = [aC1(D, F32) for _ in range(2)]
        h1 = [aC1(D, F32) for _ in range(2)]
        x1b = [aC1(D, BF16) for _ in range(2)]
        x1Tb = aC1(8 * 128, BF16, "p (c n) -> p c n", c=8)
        pTb = aC1(2 * 128, BF16, "p (c n) -> p c n", c=2)
        prow = [aC1(PLE, F32) for _ in range(2)]
        x1Tf = aC2(8 * 128, F32, "p (c n) -> p c n", c=8)
        sgt = aC2(D, F32)
        acc = [aC2(D, F32) for _ in range(2)]
        rs = aC2(512, F32)
        B_xr = [Buf("xr%d" % i) for i in range(2)]
        B_h1 = [Buf("h1_%d" % i) for i in range(2)]
        B_x1b = [Buf("x1b%d" % i) for i in range(2)]
        B_x1Tb = Buf("x1Tb")
        B_x1Tf = Buf("x1Tf")
        B_pTb = Buf("pTb")
        B_prow = [Buf("prow%d" % i) for i in range(2)]
        B_sgt = Buf("sgt")
        B_acc = [Buf("acc%d" % i) for i in range(2)]
        B_rs = Buf("rs")
        s_xr = [fw.dsem("xr%d" % i) for i in range(2)]
        s_pr = [fw.dsem("pr%d" % i) for i in range(2)]
        s_acc = [fw.dsem("accst%d" % i) for i in range(2)]
        s_sc = [fw.dsem("scat%d" % i) for i in range(2)]
        s_dbg = fw.dsem("dbg")
        B_ACCD = [Buf("ACCD%d" % t) for t in range(NT)]
        B_dbg = Buf("dbg")
        L36 = rs[:, 0:36]
        gmax = rs[:, 36:37]
        gsum = rs[:, 37:38]
        gp = rs[:, 38:39]
        oh = rs[:, 40:44]
        pen = rs[:, 44:48]
        junk4 = rs[:, 48:52]
        lem = rs[:, 64:96]
        r8 = rs[:, 96:104]
        sel1 = rs[:, 128:160]
        sel2 = rs[:, 160:192]
        selb = rs[:, 192:224]
        tmpa = rs[:, 224:256]
        tmpb = rs[:, 256:288]
        dd = rs[:, 288:289]
        rr = rs[:, 289:290]
        den = rs[:, 290:291]
        slf = rs[:, 292:294]
        stats = rs[:, 300:312]
        mv = rs[:, 312:314]
        rstd = rs[:, 314:315]

        dmy = rs[:, 320:324]
        B_dmy = Buf("dmy")
        fw.op("pool", _L_memset(rs[:, 316:324], 0.0), writes=[B_dmy])

        def prefetch_table(func):
            fw.op("act", _L_activation(out=dmy[:, 2:4], in_=dmy[:, 0:2], func=func), reads=[], writes=[B_dmy])

        def layer_norm(eng_list, h_ap, B_h, g_ap, b_ap, B_gb):
            fw.op("dve", _L_bn_stats(out=stats[:, 0:6], in_=h_ap[:, 0:512]), reads=[B_h], writes=[B_rs])
            fw.op("dve", _L_bn_stats(out=stats[:, 6:12], in_=h_ap[:, 512:1024]), reads=[B_h], writes=[B_rs])
            fw.op("dve", _L_bn_aggr(out=mv, in_=stats), reads=[B_rs], writes=[B_rs])
            fw.op("act", _L_activation(out=rstd, in_=mv[:, 1:2], func=AF.Sqrt, bias=EPS, scale=1.0), reads=[B_rs], writes=[B_rs])
            prefetch_table(AF.Exp)
            fw.op("dve", _L_reciprocal(out=rstd, in_=rstd), reads=[B_rs], writes=[B_rs])
            fw.op("dve", _L_tensor_scalar(out=h_ap, in0=h_ap, scalar1=mv[:, 0:1], scalar2=rstd, op0=ALU.subtract, op1=ALU.mult), reads=[B_h, B_rs], writes=[B_h])
            fw.op("dve", _L_tensor_tensor(out=h_ap, in0=h_ap, in1=g_ap, op=ALU.mult), reads=[B_h, B_gb], writes=[B_h])
            fw.op("dve", _L_tensor_tensor(out=h_ap, in0=h_ap, in1=b_ap, op=ALU.add), reads=[B_h, B_gb], writes=[B_h])

        def load_tok(t):
            k2 = t % 2
            i, hf = t // 2, t % 2
            fw.dma("sp", _L_dma_start(out=xr[k2], in_=xo[i, 2 + hf * 128:2 + (hf + 1) * 128, :]), s_xr[k2], writes=[B_xr[k2]])
            fw.dma("sp", _L_dma_start(out=prow[k2], in_=po[i, hf * 128:(hf + 1) * 128, :]), s_pr[k2], writes=[B_prow[k2]])

        def out_proj(t):
            i = t // 2
            tk = t * 128
            for half in range(2):
                bk = 2 + half
                n_mm = 12
                m = 0
                for cc in range(4):
                    fw.op("pe", _L_matmul(psf[bk][:, :], lhsT=yconvT[:, cc, tk:tk + 128], rhs=wo_c[:, cc, half * 512:(half + 1) * 512], start=(m == 0), stop=False), reads=[B_yc[i], B_wC], writes=[PF[bk]])
                    m += 1
                for hh in range(8):
                    fw.op("pe", _L_matmul(psf[bk][:, :], lhsT=yattnT[0:64, hh, tk:tk + 128], rhs=wo_a[0:64, hh, half * 512:(half + 1) * 512], start=False, stop=(m == n_mm - 1)), reads=[B_ya[hh][i], B_wC], writes=[PF[bk]])
                    m += 1

        load_tok(0)
        out_proj(0)
        prefetch_table(AF.Sqrt)
        for t in range(NT):
            k2 = t % 2
            i, hf = t // 2, t % 2
            tk = t * 128
            if t + 1 < NT:
                load_tok(t + 1)
            for half in range(2):
                bk = 2 + half
                fw.op("dve", _L_scalar_tensor_tensor(out=h1[k2][:, half * 512:(half + 1) * 512], in0=xr[k2][:, half * 512:(half + 1) * 512], scalar=ALPHA, in1=psf[bk][:, :], op0=ALU.mult, op1=ALU.add), reads=[B_xr[k2], PF[bk]], writes=[B_h1[k2]])
            if dbg:
                fw.dma("sp", _L_dma_start(out=dbg_mix[tk:tk + 128, :], in_=h1[k2]), s_dbg, reads=[B_h1[k2]], writes=[B_dbg])
            layer_norm(None, h1[k2], B_h1[k2], lng1, lnb1, B_wC)
            if dbg:
                fw.dma("sp", _L_dma_start(out=dbg_x1[tk:tk + 128, :], in_=h1[k2]), s_dbg, reads=[B_h1[k2]], writes=[B_dbg])
            for g in range(2):
                bk = 2 + g
                for q in range(4):
                    c = 4 * g + q
                    fw.op("pe", _L_transpose(out=psf[bk][:, q * 128:(q + 1) * 128], in_=h1[k2][:, c * 128:(c + 1) * 128], identity=ident_f), reads=[B_h1[k2], B_ident], writes=[PF[bk]])
                fw.op("act", copy_op("act", x1Tf[:, 4 * g:4 * g + 4, :], psf[bk][:, :].rearrange("p (a b) -> p a b", a=4)), reads=[PF[bk]], writes=[B_x1Tf])
                fw.op("dve", copy_op("dve", x1Tb[:, 4 * g:4 * g + 4, :], psf[bk][:, :].rearrange("p (a b) -> p a b", a=4)), reads=[PF[bk]], writes=[B_x1Tb])
            fw.op("act", copy_op("act", x1b[k2], h1[k2]), reads=[B_h1[k2]], writes=[B_x1b[k2]])
            for c in range(8):
                fw.op("pe", _L_matmul(psf[4][:, 0:36], lhsT=x1Tf[:, c, :], rhs=wr[:, c, :], start=(c == 0), stop=(c == 7)), reads=[B_x1Tf, B_wC], writes=[PF[4]])
            for half in range(2):
                bk = half
                for c in range(8):
                    fw.op("pe", _L_matmul(psf[bk][:, :], lhsT=x1Tb[:, c, :], rhs=wpg[:, c, half * 512:(half + 1) * 512], start=(c == 0), stop=(c == 7)), reads=[B_x1Tb, B_wC], writes=[PF[bk]])
            fw.op("dve", _L_tensor_tensor(out=L36, in0=psf[4][:, 0:36], in1=bias36, op=ALU.add), reads=[PF[4], B_const], writes=[B_rs])
            fw.op("dve", _L_tensor_reduce(out=gmax, in_=rs[:, 0:4], axis=AX.X, op=ALU.max), reads=[B_rs], writes=[B_rs])
            fw.op("dve", _L_tensor_scalar(out=dd, in0=gmax, scalar1=-1.0, scalar2=None, op0=ALU.mult), reads=[B_rs], writes=[B_rs])
            fw.op("act", _L_activation(out=junk4, in_=rs[:, 0:4], func=AF.Exp, bias=dd, scale=1.0, accum_out=gsum), reads=[B_rs], writes=[B_rs])
            fw.op("dve", _L_reciprocal(out=gp, in_=gsum), reads=[B_rs], writes=[B_rs])
            fw.op("dve", _L_tensor_scalar(out=oh, in0=rs[:, 0:4], scalar1=gmax, scalar2=None, op0=ALU.is_equal), reads=[B_rs], writes=[B_rs])
            fw.op("dve", _L_tensor_scalar(out=pen, in0=oh, scalar1=1.0, scalar2=1.0e30, op0=ALU.subtract, op1=ALU.mult), reads=[B_rs], writes=[B_rs])
            for g in range(4):
                fw.op("dve", _L_tensor_scalar(out=lem[:, 8 * g:8 * g + 8], in0=rs[:, 4 + 8 * g:12 + 8 * g], scalar1=pen[:, g:g + 1], scalar2=None, op0=ALU.add), reads=[B_rs], writes=[B_rs])
            fw.op("dve", _L_max(out=r8, in_=lem), reads=[B_rs], writes=[B_rs])
            fw.op("dve", _L_tensor_scalar(out=sel1, in0=lem, scalar1=r8[:, 0:1], scalar2=None, op0=ALU.is_equal), reads=[B_rs], writes=[B_rs])
            fw.op("dve", _L_tensor_scalar(out=sel2, in0=lem, scalar1=r8[:, 1:2], scalar2=None, op0=ALU.is_equal), reads=[B_rs], writes=[B_rs])
            fw.op("dve", _L_tensor_tensor(out=selb, in0=sel1, in1=sel2, op=ALU.add), reads=[B_rs], writes=[B_rs])
            fw.op("dve", _L_tensor_tensor(out=dd, in0=r8[:, 1:2], in1=r8[:, 0:1], op=ALU.subtract), reads=[B_rs], writes=[B_rs])
            fw.op("act", _L_activation(out=rr, in_=dd, func=AF.Exp), reads=[B_rs], writes=[B_rs])
            prefetch_table(AF.Sigmoid)
            fw.op("dve", _L_tensor_scalar(out=den, in0=rr, scalar1=1.0, scalar2=None, op0=ALU.add), reads=[B_rs], writes=[B_rs])
            fw.op("dve", _L_reciprocal(out=den, in_=den), reads=[B_rs], writes=[B_rs])
            fw.op("dve", _L_tensor_tensor(out=w12[:, t, 0:1], in0=gp, in1=den, op=ALU.mult), reads=[B_rs], writes=[B_w12[t]])
            fw.op("dve", _L_tensor_tensor(out=w12[:, t, 1:2], in0=w12[:, t, 0:1], in1=rr, op=ALU.mult), reads=[B_rs, B_w12[t]], writes=[B_w12[t]])
            fw.op("pe", _L_matmul(psf[5][:, 0:32], lhsT=Lst, rhs=selb, start=True, stop=False), reads=[B_rs, B_const], writes=[PF[5]])
            fw.op("pe", _L_matmul(psf[5][:, 0:32], lhsT=ones_f, rhs=Spre, start=False, stop=True), reads=[B_Spre, B_const], writes=[PF[5]])
            if t + 1 < NT:
                out_proj(t + 1)
            fw.op("dve", _L_tensor_tensor(out=tmpa, in0=psf[5][:, 0:32], in1=eC, op=ALU.add), reads=[PF[5], B_const], writes=[B_rs])
            fw.op("dve", _L_tensor_scalar(out=tmpb, in0=psf[5][:, 0:32], scalar1=float(CAP), scalar2=1.0e6, op0=ALU.is_ge, op1=ALU.mult), reads=[PF[5]], writes=[B_rs])
            fw.op("dve", _L_tensor_tensor(out=tmpa, in0=tmpa, in1=tmpb, op=ALU.add), reads=[B_rs], writes=[B_rs])
            fw.op("dve", _L_tensor_tensor(out=tmpb, in0=tmpa, in1=sel1, op=ALU.mult), reads=[B_rs], writes=[B_rs])
            fw.op("dve", _L_tensor_reduce(out=slf[:, 0:1], in_=tmpb, axis=AX.X, op=ALU.add), reads=[B_rs], writes=[B_rs])
            fw.op("dve", _L_tensor_tensor(out=tmpb, in0=tmpa, in1=sel2, op=ALU.mult), reads=[B_rs], writes=[B_rs])
            fw.op("dve", _L_tensor_reduce(out=slf[:, 1:2], in_=tmpb, axis=AX.X, op=ALU.add), reads=[B_rs], writes=[B_rs])
            fw.op("dve", _L_tensor_copy(out=slot_i[:, t, :], in_=slf), reads=[B_rs], writes=[B_slot[t]])
            fw.op("dve", _L_tensor_tensor(out=Spre, in0=Spre, in1=selb, op=ALU.add), reads=[B_rs, B_Spre], writes=[B_Spre])
            for k in range(2):
                fw.dma("pool", _L_indirect_dma_start(out=XS, out_offset=bass.IndirectOffsetOnAxis(ap=slot_i[:, t, k:k + 1], axis=0), in_=x1b[k2], in_offset=None, bounds_check=NSL - 1, oob_is_err=False), s_sc[k2], reads=[B_x1b[k2], B_slot[t], B_XS], writes=[B_XS])
            for half in range(2):
                fw.op("act", _L_activation(out=sgt[:, half * 512:(half + 1) * 512], in_=psf[half][:, :], func=AF.Sigmoid), reads=[PF[half]], writes=[B_sgt])
            prefetch_table(AF.Sqrt)
            for c in range(2):
                fw.op("pe", _L_transpose(out=psf[4][:, c * 128:(c + 1) * 128], in_=prow[k2][:, c * 128:(c + 1) * 128], identity=ident_f), reads=[B_prow[k2], B_ident], writes=[PF[4]])
            fw.op("act", copy_op("act", pTb, psf[4][:, 0:256].rearrange("p (a b) -> p a b", a=2)), reads=[PF[4]], writes=[B_pTb])
            for half in range(2):
                bk = half
                for c in range(2):
                    fw.op("pe", _L_matmul(psf[bk][:, :], lhsT=pTb[:, c, :], rhs=wpp[:, c, half * 512:(half + 1) * 512], start=(c == 0), stop=(c == 1)), reads=[B_pTb, B_wC], writes=[PF[bk]])
                fw.op("dve", _L_tensor_tensor(out=acc[k2][:, half * 512:(half + 1) * 512], in0=sgt[:, half * 512:(half + 1) * 512], in1=psf[bk][:, :], op=ALU.mult), reads=[B_sgt, PF[bk]], writes=[B_acc[k2]])
            fw.op("dve", _L_scalar_tensor_tensor(out=acc[k2], in0=h1[k2], scalar=ALPHA, in1=acc[k2], op0=ALU.mult, op1=ALU.add), reads=[B_h1[k2], B_acc[k2]], writes=[B_acc[k2]])
            fw.dma("sp", _L_dma_start(out=ACCD[tk:tk + 128, :], in_=acc[k2]), s_acc[k2], reads=[B_acc[k2]], writes=[B_ACCD[t]])
        fw.barrier()

        aD = Alloc(R0, 98304)
        wstg = [aD(8 * DE, F32) for _ in range(3)]
        wbf = [aD(8 * DE, BF16) for _ in range(6)]
        aD1 = Alloc(R1, 59392)
        Xe = [aD1(D, BF16) for _ in range(2)]
        XeT = [aD1(8 * 256, BF16, "p (c n) -> p c n", c=8) for _ in range(2)]
        hT = [aD1(4 * 256, BF16, "p (c n) -> p c n", c=4) for _ in range(2)]
        sgu = [aD1(256, F32) for _ in range(2)]
        Yst = [aD1(D, F32) for _ in range(2)]
        Y1 = [aD1(D, F32) for _ in range(2)]
        Y2 = [aD1(D, F32) for _ in range(2)]
        accr = [aD1(D, F32) for _ in range(3)]
        aD2 = Alloc(R2, 20480)
        lng2 = aD2(D, F32)
        lnb2 = aD2(D, F32)
        rs2 = aD2(64, F32)
        B_wstg = [Buf("wstg%d" % i) for i in range(3)]
        B_wbf = [Buf("wbf%d" % i) for i in range(6)]
        B_Xe = [Buf("Xe%d" % i) for i in range(2)]
        B_XeT = [Buf("XeT%d" % i) for i in range(2)]
        B_hT = [Buf("hT%d" % i) for i in range(2)]
        B_sgu = [Buf("sgu%d" % i) for i in range(2)]
        B_Yst = [Buf("Yst%d" % i) for i in range(2)]
        B_Y1 = [Buf("Y1_%d" % i) for i in range(2)]
        B_Y2 = [Buf("Y2_%d" % i) for i in range(2)]
        B_accr = [Buf("accr%d" % i) for i in range(3)]
        B_ln2 = Buf("ln2")
        B_YS = Buf("YS")
        s_wstg = [fw.dsem("wstg%d" % i) for i in range(3)]
        s_xe = [fw.dsem("xe%d" % i) for i in range(2)]
        s_yst = [fw.dsem("yst%d" % i) for i in range(2)]
        s_g = [fw.dsem("gath%d" % i) for i in range(2)]
        s_accr = [fw.dsem("accr%d" % i) for i in range(3)]
        s_out = [fw.dsem("out%d" % i) for i in range(3)]
        s_ln2 = fw.dsem("ln2")
        B_out = Buf("out")
        fw.dma("sp", _L_dma_start(out=lng2, in_=ln2_g.partition_broadcast(128)), s_ln2, writes=[B_ln2])
        fw.dma("sp", _L_dma_start(out=lnb2, in_=ln2_b.partition_broadcast(128)), s_ln2, writes=[B_ln2])

        mats = []
        for ex in range(NE):
            mats.append(("g", ex))
            mats.append(("u", ex))
            mats.append(("d", ex))

        CAST_ENG = ("pool", "act", "dve", "dve", "pool", "act", "dve", "act", "pool", "dve", "act", "dve")

        pending = {"act": [], "dve": []}

        def load_mat(mi):
            kind, ex = mats[mi]
            sl = mi % 3
            bs = mi % 6
            if kind in ("g", "u"):
                src = (w_gate if kind == "g" else w_up)[ex].rearrange("(p c) f -> p c f", c=8)
                dstv = wstg[sl].rearrange("p (c f) -> p c f", c=8)
            else:
                src = w_down[ex].rearrange("(c p) n -> p c n", p=128)
                dstv = wstg[sl].rearrange("p (c n) -> p c n", c=4)
            fw.dma("sp", _L_dma_start(out=dstv, in_=src), s_wstg[sl], writes=[B_wstg[sl]])
            for k in range(4):
                eng = CAST_ENG[(4 * mi + k) % 12]
                o, i_ = wbf[bs][:, k * 1024:(k + 1) * 1024], wstg[sl][:, k * 1024:(k + 1) * 1024]

                def emit(eng=eng, o=o, i_=i_, sl=sl, bs=bs):
                    fw.op(eng, copy_op(eng, o, i_), reads=[B_wstg[sl]], writes=[B_wbf[bs]])
                if eng == "pool":
                    emit()
                else:
                    pending[eng].append(emit)

        def drain(eng, n):
            for _ in range(n):
                if pending[eng]:
                    pending[eng].pop(0)()

        XSe = XS.rearrange("(e s p) d -> e s p d", s=2, p=128)
        YSe = YS.rearrange("(e s p) d -> e s p d", s=2, p=128)

        def load_xe(ex):
            for s2 in range(2):
                fw.dma("act", _L_dma_start(out=Xe[s2], in_=XSe[ex, s2]), s_xe[s2], reads=[B_XS], writes=[B_Xe[s2]])

        for mi in range(3):
            load_mat(mi)
        drain("act", 99)
        drain("dve", 99)
        load_xe(0)
        yi = [0]
        for ex in range(NE):
            k2 = ex % 2
            bb = (3 * ex) % 6
            wg = wbf[bb].rearrange("p (c f) -> p c f", c=8)
            wu = wbf[bb + 1].rearrange("p (c f) -> p c f", c=8)
            wd = wbf[bb + 2].rearrange("p (c n) -> p c n", c=4)
            if ex + 1 < NE:
                for q in range(3):
                    load_mat(3 * (ex + 1) + q)
            for s2 in range(2):
                pb = s2
                for c in range(8):
                    fw.op("pe", _L_transpose(out=psb[pb][:, c * 128:(c + 1) * 128], in_=Xe[s2].rearrange("t (p c) -> t c p", c=8)[:, c, :], identity=ident_b), reads=[B_Xe[s2], B_const], writes=[PB[pb]])
                eng = alt(s2)
                fw.op(eng, copy_op(eng, XeT[k2][:, :, s2 * 128:(s2 + 1) * 128], psb[pb][:, :].rearrange("p (c n) -> p c n", c=8)), reads=[PB[pb]], writes=[B_XeT[k2]])
            if ex + 1 < NE:
                load_xe(ex + 1)
            for fo in range(4):
                bg, bu = (0, 1) if fo % 2 == 0 else (2, 3)
                for (bk, wm, bi) in ((bg, wg, bb), (bu, wu, bb + 1)):
                    for c in range(8):
                        fw.op("pe", _L_matmul(psf[bk][:, 0:256], lhsT=wm[:, c, fo * 128:(fo + 1) * 128], rhs=XeT[k2][:, c, :], start=(c == 0), stop=(c == 7)), reads=[B_XeT[k2], B_wbf[bi]], writes=[PF[bk]])
                sq = fo % 2
                fw.op("act", _L_activation(out=sgu[sq], in_=psf[bg][:, 0:256], func=AF.Silu), reads=[PF[bg]], writes=[B_sgu[sq]])
                fw.op("dve", _L_tensor_tensor(out=hT[k2][:, fo, :], in0=sgu[sq], in1=psf[bu][:, 0:256], op=ALU.mult), reads=[B_sgu[sq], PF[bu]], writes=[B_hT[k2]])
                drain("act", 1)
                drain("dve", 1)
            for s2 in range(2):
                ys = yi[0] % 2
                yi[0] += 1
                for half in range(2):
                    bk = 4 + half
                    for fo in range(4):
                        fw.op("pe", _L_matmul(psf[bk][:, :], lhsT=hT[k2][:, fo, s2 * 128:(s2 + 1) * 128], rhs=wd[:, fo, half * 512:(half + 1) * 512], start=(fo == 0), stop=(fo == 3)), reads=[B_hT[k2], B_wbf[bb + 2]], writes=[PF[bk]])
                    eng = alt(half)
                    fw.op(eng, copy_op(eng, Yst[ys][:, half * 512:(half + 1) * 512], psf[bk][:, :]), reads=[PF[bk]], writes=[B_Yst[ys]])
                fw.dma("act", _L_dma_start(out=YSe[ex, s2], in_=Yst[ys]), s_yst[ys], reads=[B_Yst[ys]], writes=[])
            drain("act", 99)
            drain("dve", 99)

        fw.barrier()

        st2 = rs2[:, 0:12]
        mv2 = rs2[:, 12:14]
        rstd2 = rs2[:, 14:15]

        def fin_issue(t):
            k2, k3 = t % 2, t % 3
            tk = t * 128
            fw.op("pool", _L_memset(Y1[k2], 0.0), writes=[B_Y1[k2]])
            fw.op("pool", _L_memset(Y2[k2], 0.0), writes=[B_Y2[k2]])
            fw.dma("sp", _L_dma_start(out=accr[k3], in_=ACCD[tk:tk + 128, :]), s_accr[k3], reads=[B_ACCD[t]], writes=[B_accr[k3]])
            fw.dma("pool", _L_indirect_dma_start(out=Y1[k2], out_offset=None, in_=YS, in_offset=bass.IndirectOffsetOnAxis(ap=slot_i[:, t, 0:1], axis=0), bounds_check=NSL - 1, oob_is_err=False), s_g[k2], reads=[B_YS, B_slot[t]], writes=[B_Y1[k2]])
            fw.dma("pool", _L_indirect_dma_start(out=Y2[k2], out_offset=None, in_=YS, in_offset=bass.IndirectOffsetOnAxis(ap=slot_i[:, t, 1:2], axis=0), bounds_check=NSL - 1, oob_is_err=False), s_g[k2], reads=[B_YS, B_slot[t]], writes=[B_Y2[k2]])

        def fin_front(t):
            k2, k3 = t % 2, t % 3
            h_ap, B_h, B_r = accr[k3], B_accr[k3], B_rs
            fw.op("dve", _L_scalar_tensor_tensor(out=h_ap, in0=Y1[k2], scalar=w12[:, t, 0:1], in1=h_ap, op0=ALU.mult, op1=ALU.add), reads=[B_Y1[k2], B_w12[t], B_h], writes=[B_h])
            fw.op("dve", _L_scalar_tensor_tensor(out=h_ap, in0=Y2[k2], scalar=w12[:, t, 1:2], in1=h_ap, op0=ALU.mult, op1=ALU.add), reads=[B_Y2[k2], B_w12[t], B_h], writes=[B_h])
            fw.op("dve", _L_bn_stats(out=st2[:, 0:6], in_=h_ap[:, 0:512]), reads=[B_h], writes=[B_r])
            fw.op("dve", _L_bn_stats(out=st2[:, 6:12], in_=h_ap[:, 512:1024]), reads=[B_h], writes=[B_r])
            fw.op("dve", _L_bn_aggr(out=mv2, in_=st2), reads=[B_r], writes=[B_r])
            fw.op("act", _L_activation(out=rstd2, in_=mv2[:, 1:2], func=AF.Sqrt, bias=EPS, scale=1.0), reads=[B_r], writes=[B_r])
            fw.op("dve", _L_reciprocal(out=rstd2, in_=rstd2), reads=[B_r], writes=[B_r])
            fw.op("dve", _L_tensor_scalar(out=h_ap, in0=h_ap, scalar1=mv2[:, 0:1], scalar2=rstd2, op0=ALU.subtract, op1=ALU.mult), reads=[B_h, B_r], writes=[B_h])
            fw.op("dve", _L_tensor_tensor(out=h_ap, in0=h_ap, in1=lng2, op=ALU.mult), reads=[B_h, B_ln2], writes=[B_h])

        def fin_back(t):
            k3 = t % 3
            tk = t * 128
            fw.op("dve", _L_tensor_tensor(out=accr[k3], in0=accr[k3], in1=lnb2, op=ALU.add), reads=[B_accr[k3], B_ln2], writes=[B_accr[k3]])
            fw.dma("sp", _L_dma_start(out=out[tk:tk + 128, :], in_=accr[k3]), s_out[k3], reads=[B_accr[k3]], writes=[])

        fin_issue(0)
        for t in range(NT):
            if t + 1 < NT:
                fin_issue(t + 1)
            fin_front(t)
            if t >= 1:
                fin_back(t - 1)
        fin_back(NT - 1)
        fw.barrier()
        with nc.allow_non_contiguous_dma(reason="tiny strided parameter loads"):
            fw.replay()
    nc._fw_names = fw.names
    return nc


def own_blocks(r, NB):
    NSLOT = NB // 4
    js = []
    for i in range(NSLOT):
        if i < NSLOT // 2:
            js.append(r + 4 * i)
        else:
            js.append(NB - 1 - r - 4 * (NSLOT - 1 - i))
    return js


def make_in_maps(inputs, S):
    NB = S // 256
    x = np.asarray(inputs["x"], dtype=np.float32)
    p = np.asarray(inputs["p"], dtype=np.float32)
    nbatch = x.shape[0]
    shared = {
        "w_in": inputs["w_in"][0], "w_conv": inputs["w_conv"][0], "w_out": inputs["w_out"][0],
        "ln1_g": inputs["ln1_g"][0], "ln1_b": inputs["ln1_b"][0],
        "w_rg": inputs["w_router_g"][0], "b_rg": inputs["b_router_g"][0],
        "w_re": inputs["w_router_e"][0], "b_re": inputs["b_router_e"][0],
        "w_gate": inputs["w_gate"][0], "w_up": inputs["w_up"][0], "w_down": inputs["w_down"][0],
        "w_pg": inputs["w_ple_gate"][0], "w_pp": inputs["w_ple_proj"][0],
        "ln2_g": inputs["ln2_g"][0], "ln2_b": inputs["ln2_b"][0],
    }
    shared = {k: np.ascontiguousarray(np.asarray(v, dtype=np.float32)) for k, v in shared.items()}
    maps = []
    for b in range(nbatch):
        for r in range(4):
            js = own_blocks(r, NB)
            xo = np.zeros((len(js), 258, D), np.float32)
            po = np.zeros((len(js), 256, PLE), np.float32)
            nm1 = np.zeros((len(js), 8, 32), np.float32)
            nm2 = np.zeros((len(js), 8, 32), np.float32)
            for i, j in enumerate(js):
                lo = 256 * j - 2
                if lo >= 0:
                    xo[i] = x[b, lo:lo + 258]
                else:
                    xo[i, 2:] = x[b, 0:256]
                po[i] = p[0, b, 256 * j:256 * j + 256]
                nm1[i, :, j:] = NEGINF
                nm2[i, :, j:] = NEG
            m = dict(shared)
            m["xfT"] = np.ascontiguousarray(x[b].T)
            m["xoT"] = np.ascontiguousarray(xo.transpose(0, 2, 1))
            m["xo"] = xo
            m["po"] = po
            m["nm1"] = nm1.reshape(-1)
            m["nm2"] = nm2.reshape(-1)
            maps.append(m)
    return maps


_NC_CACHE = {}


def kernel(**inputs):
    x = np.asarray(inputs["x"])
    nbatch, S, _ = x.shape
    NB = S // 256
    if S not in _NC_CACHE:
        _NC_CACHE[S] = build_nc(S)
    nc = _NC_CACHE[S]
    maps = make_in_maps(inputs, S)
    res = run_bass_kernel_spmd(nc, maps, core_ids=list(range(len(maps))))
    outp = np.zeros((nbatch, S, D), np.float32)
    k = 0
    for b in range(nbatch):
        for r in range(4):
            o = np.asarray(res.results[k]["out"], dtype=np.float32)
            for i, j in enumerate(own_blocks(r, NB)):
                outp[b, 256 * j:256 * j + 256] = o[256 * i:256 * i + 256]
            k += 1
    return outp
```
